# Optimizing a Trainium2 kernel written in Bass

```python
import jax, jax.numpy as jnp
from jax import lax
import numpy as np

D_MODEL = 1024
BATCH = 32
SEQ = 2048
DEPTH = 1

HEAD_DIM = 64
NSA_HEADS = 8
NSA_KV = 2
DSA_HEADS = 8
IDX_HEADS = 4
IDX_DIM = 64
CMP_LEN = 32
CMP_STRIDE = 16
CMP_HIDDEN = 2 * HEAD_DIM
SLC_LEN = 64
SLC_TOPN = 16
WIN = 512
DSA_TOPK = 256
D_FF = 4 * D_MODEL
D_MIX = (NSA_HEADS + DSA_HEADS) * HEAD_DIM
ROPE_THETA = 10000.0
EPS = 1e-6
Q_BLOCK = 128
SLC_Q_BLOCK = 16
NEG = -1e30
NSA_COLS = [NSA_HEADS * HEAD_DIM] + [NSA_KV * HEAD_DIM] * 6 + [NSA_HEADS * 3]
DSA_COLS = [DSA_HEADS * HEAD_DIM, HEAD_DIM, HEAD_DIM, IDX_HEADS * IDX_DIM, IDX_DIM, IDX_HEADS]
IN_COLS = NSA_COLS + DSA_COLS
D_IN = sum(IN_COLS)

kernel_name = 'hybrid_nsa_dsa_parallel_block'


def rms_norm(x, g):
    xf = x.astype(jnp.float32)
    y = xf * lax.rsqrt(jnp.mean(xf * xf, axis=-1, keepdims=True) + EPS)
    return (y * g.astype(jnp.float32)).astype(x.dtype)


def rope(x, pos):
    half = x.shape[-1] // 2
    inv = ROPE_THETA ** (-jnp.arange(half, dtype=jnp.float32) / half)
    ang = pos.astype(jnp.float32)[:, :, None] * inv
    cos = jnp.cos(ang)[:, :, None, :]
    sin = jnp.sin(ang)[:, :, None, :]
    xf = x.astype(jnp.float32)
    x1, x2 = xf[..., :half], xf[..., half:]
    return jnp.concatenate([x1 * cos - x2 * sin, x2 * cos + x1 * sin], axis=-1).astype(x.dtype)


def masked_softmax(s, mask):
    s = jnp.where(mask, s.astype(jnp.float32), NEG)
    return jax.nn.softmax(s, axis=-1) * mask.astype(jnp.float32)


def compress(kv, pe, w1, w2):
    B, T, G, dh = kv.shape
    n_cmp = (T - CMP_LEN) // CMP_STRIDE + 1
    blk = np.arange(n_cmp)[:, None] * CMP_STRIDE + np.arange(CMP_LEN)[None, :]
    blocks = kv[:, blk] + pe[None, None, :, None, :]
    flat = blocks.transpose(0, 1, 3, 2, 4).reshape(B, n_cmp, G, CMP_LEN * dh)
    return jax.nn.gelu(flat @ w1) @ w2


def nsa_attention(q, kc, vc, ks, vs, kw, vw, gate_logits, pe_k, pe_v, w1k, w2k, w1v, w2v):
    B, T, H, dh = q.shape
    G = kc.shape[2]
    R = H // G
    scale = dh ** -0.5
    q_t = q.reshape(B, T, G, R, dh).transpose(0, 2, 3, 1, 4)
    t_idx = np.arange(T)

    k_cmp = compress(kc, pe_k, w1k, w2k)
    v_cmp = compress(vc, pe_v, w1v, w2v)
    n_cmp = k_cmp.shape[1]
    cmp_start = np.arange(n_cmp) * CMP_STRIDE
    cmp_end = cmp_start + CMP_LEN - 1
    mask_c = jnp.asarray(cmp_end[None, :] <= t_idx[:, None])
    s_c = jnp.einsum('bgrtd,bngd->bgrtn', q_t, k_cmp) * scale
    p_c = masked_softmax(s_c, mask_c)
    o_cmp = jnp.einsum('bgrtn,bngd->bgrtd', p_c.astype(v_cmp.dtype), v_cmp)

    n_slc = T // SLC_LEN
    n_sel = min(SLC_TOPN, n_slc)
    slc_start = np.arange(n_slc) * SLC_LEN
    slc_end = slc_start + SLC_LEN - 1
    overlap = ((cmp_start[:, None] <= slc_end[None, :]) & (cmp_end[:, None] >= slc_start[None, :])).astype(np.float32)
    imp = jnp.einsum('bgrtn,nj->bgtj', p_c, jnp.asarray(overlap))
    j = np.arange(n_slc)[None, :]
    cur = (t_idx // SLC_LEN)[:, None]
    valid = jnp.asarray(j <= cur)
    forced = jnp.asarray((j == 0) | (j == cur) | (j == cur - 1))
    sel_score = jnp.where(valid, jnp.where(forced, jnp.inf, imp), -jnp.inf)
    _, sel = lax.top_k(sel_score, n_sel)

    kb = ks.reshape(B, n_slc, SLC_LEN, G, dh).transpose(0, 3, 1, 2, 4)
    vb = vs.reshape(B, n_slc, SLC_LEN, G, dh).transpose(0, 3, 1, 2, 4)
    nq = T // SLC_Q_BLOCK
    n_key = n_sel * SLC_LEN
    q_ch = q_t.reshape(B, G, R, nq, SLC_Q_BLOCK, dh).transpose(3, 0, 1, 2, 4, 5)
    i_ch = sel.reshape(B, G, nq, SLC_Q_BLOCK, n_sel).transpose(2, 0, 1, 3, 4)
    t_ch = jnp.arange(T, dtype=jnp.int32).reshape(nq, SLC_Q_BLOCK)
    bi = jnp.arange(B)[:, None, None, None]
    gi = jnp.arange(G)[None, :, None, None]

    def sel_chunk(args):
        qc, ic, tc = args
        kg = kb[bi, gi, ic].reshape(B, G, SLC_Q_BLOCK, n_key, dh)
        vg = vb[bi, gi, ic].reshape(B, G, SLC_Q_BLOCK, n_key, dh)
        kpos = (ic[..., None] * SLC_LEN + jnp.arange(SLC_LEN)).reshape(B, G, SLC_Q_BLOCK, n_key)
        mask = (kpos <= tc[None, None, :, None])[:, :, None]
        s = jnp.einsum('bgrqd,bgqkd->bgrqk', qc, kg) * scale
        p = masked_softmax(s, mask)
        return jnp.einsum('bgrqk,bgqkd->bgrqd', p.astype(vg.dtype), vg)

    o_slc = lax.map(sel_chunk, (q_ch, i_ch, t_ch))
    o_slc = o_slc.transpose(1, 2, 3, 0, 4, 5).reshape(B, G, R, T, dh)

    kp = jnp.pad(kw, ((0, 0), (WIN, 0), (0, 0), (0, 0)))
    vp = jnp.pad(vw, ((0, 0), (WIN, 0), (0, 0), (0, 0)))
    nb = T // Q_BLOCK
    span = WIN + Q_BLOCK
    q_b = q_t.reshape(B, G, R, nb, Q_BLOCK, dh).transpose(3, 0, 1, 2, 4, 5)

    def win_block(args):
        qb, bidx = args
        s0 = bidx * Q_BLOCK
        kk = lax.dynamic_slice_in_dim(kp, s0, span, axis=1)
        vv = lax.dynamic_slice_in_dim(vp, s0, span, axis=1)
        kpos = s0 - WIN + jnp.arange(span)
        tq = s0 + jnp.arange(Q_BLOCK)
        mask = (kpos[None, :] <= tq[:, None]) & (kpos[None, :] > tq[:, None] - WIN) & (kpos[None, :] >= 0)
        s = jnp.einsum('bgrqd,bsgd->bgrqs', qb, kk) * scale
        p = masked_softmax(s, mask)
        return jnp.einsum('bgrqs,bsgd->bgrqd', p.astype(vv.dtype), vv)

    o_win = lax.map(win_block, (q_b, jnp.arange(nb)))
    o_win = o_win.transpose(1, 2, 3, 0, 4, 5).reshape(B, G, R, T, dh)

    g = jax.nn.sigmoid(gate_logits.astype(jnp.float32)).reshape(B, T, G, R, 3)
    g = g.transpose(0, 2, 3, 1, 4).astype(q.dtype)
    o = g[..., 0:1] * o_cmp + g[..., 1:2] * o_slc + g[..., 2:3] * o_win
    return o.transpose(0, 3, 1, 2, 4).reshape(B, T, H * dh)


def dsa_attention(q, k, v, q_idx, k_idx, w_idx):
    B, T, H, dh = q.shape
    k_top = min(DSA_TOPK, T // 4)
    scale = dh ** -0.5
    idx_scale = (IDX_HEADS ** -0.5) * (IDX_DIM ** -0.5)
    nb = T // Q_BLOCK
    bi = jnp.arange(B)[:, None, None]
    key_pos = jnp.arange(T)

    def blocks(a):
        return a.reshape((B, nb, Q_BLOCK) + a.shape[2:]).swapaxes(0, 1)

    def dsa_block(args):
        qb, qib, wb, bidx = args
        tq = bidx * Q_BLOCK + jnp.arange(Q_BLOCK)
        logits = jnp.einsum('bqhd,bsd->bqhs', qib, k_idx).astype(jnp.float32)
        score = jnp.einsum('bqh,bqhs->bqs', wb.astype(jnp.float32) * idx_scale, jax.nn.relu(logits))
        score = jnp.where(key_pos[None, None, :] <= tq[None, :, None], score, -jnp.inf)
        _, sel = lax.top_k(score, k_top)
        kg = k[bi, sel]
        vg = v[bi, sel]
        mask = (sel <= tq[None, :, None])[:, None]
        s = jnp.einsum('bqhd,bqkd->bhqk', qb, kg) * scale
        p = masked_softmax(s, mask)
        return jnp.einsum('bhqk,bqkd->bqhd', p.astype(vg.dtype), vg)

    o = lax.map(dsa_block, (blocks(q), blocks(q_idx), blocks(w_idx), jnp.arange(nb)))
    return o.swapaxes(0, 1).reshape(B, T, H * dh)


def setup_inputs(seed: int = 0) -> dict:
    key = jax.random.key(seed)
    ks = jax.random.split(key, 20)

    def nrm(k, shape, s):
        return jax.random.normal(k, shape, jnp.float32) * s

    x = nrm(ks[0], (BATCH, SEQ, D_MODEL), 1.0)
    c = nrm(ks[1], (BATCH, D_MODEL), 1.0)
    start = jax.random.randint(ks[2], (BATCH, 1), 0, 4096, dtype=jnp.int32)
    positions = start + jnp.arange(SEQ, dtype=jnp.int32)[None, :]
    return {
        'x': x,
        'c': c,
        'positions': positions,
        'w_ada': nrm(ks[3], (DEPTH, D_MODEL, 6 * D_MODEL), 0.1 * D_MODEL ** -0.5),
        'b_ada': nrm(ks[4], (DEPTH, 6 * D_MODEL), 0.01),
        'g_pre_mix': 1.0 + nrm(ks[5], (DEPTH, D_MODEL), 0.05),
        'g_post_mix': 1.0 + nrm(ks[6], (DEPTH, D_MODEL), 0.05),
        'g_pre_ffn': 1.0 + nrm(ks[7], (DEPTH, D_MODEL), 0.05),
        'g_post_ffn': 1.0 + nrm(ks[8], (DEPTH, D_MODEL), 0.05),
        'w_in': nrm(ks[9], (DEPTH, D_MODEL, D_IN), D_MODEL ** -0.5),
        'cmp_pe_k': nrm(ks[10], (DEPTH, CMP_LEN, HEAD_DIM), 0.02),
        'cmp_pe_v': nrm(ks[11], (DEPTH, CMP_LEN, HEAD_DIM), 0.02),
        'cmp_w1_k': nrm(ks[12], (DEPTH, CMP_LEN * HEAD_DIM, CMP_HIDDEN), (CMP_LEN * HEAD_DIM) ** -0.5),
        'cmp_w2_k': nrm(ks[13], (DEPTH, CMP_HIDDEN, HEAD_DIM), CMP_HIDDEN ** -0.5),
        'cmp_w1_v': nrm(ks[14], (DEPTH, CMP_LEN * HEAD_DIM, CMP_HIDDEN), (CMP_LEN * HEAD_DIM) ** -0.5),
        'cmp_w2_v': nrm(ks[15], (DEPTH, CMP_HIDDEN, HEAD_DIM), CMP_HIDDEN ** -0.5),
        'w_out': nrm(ks[16], (DEPTH, D_MIX, D_MODEL), D_MIX ** -0.5),
        'w_up': nrm(ks[17], (DEPTH, D_MODEL, D_FF), D_MODEL ** -0.5),
        'w_down': nrm(ks[18], (DEPTH, D_FF, D_MODEL), D_FF ** -0.5),
    }


def reference(x, c, positions, w_ada, b_ada, g_pre_mix, g_post_mix, g_pre_ffn, g_post_ffn,
              w_in, cmp_pe_k, cmp_pe_v, cmp_w1_k, cmp_w2_k, cmp_w1_v, cmp_w2_v,
              w_out, w_up, w_down):
    B, T, _ = x.shape
    splits = np.cumsum(IN_COLS)[:-1].tolist()

    def heads(a, n):
        return a.reshape(B, T, n, -1)

    for l in range(DEPTH):
        mod = c @ w_ada[l] + b_ada[l]
        sh1, sc1, gt1, sh2, sc2, gt2 = jnp.split(mod, 6, axis=-1)

        h = rms_norm(x, g_pre_mix[l]) * (1.0 + sc1[:, None]) + sh1[:, None]
        proj = h @ w_in[l]
        (q_n, kc, vc, ksl, vsl, kw, vw, gl,
         q_d, k_d, v_d, qi, ki, wi) = jnp.split(proj, splits, axis=-1)

        o_nsa = nsa_attention(
            rope(heads(q_n, NSA_HEADS), positions),
            rope(heads(kc, NSA_KV), positions), heads(vc, NSA_KV),
            rope(heads(ksl, NSA_KV), positions), heads(vsl, NSA_KV),
            rope(heads(kw, NSA_KV), positions), heads(vw, NSA_KV),
            gl, cmp_pe_k[l], cmp_pe_v[l], cmp_w1_k[l], cmp_w2_k[l], cmp_w1_v[l], cmp_w2_v[l])

        o_dsa = dsa_attention(
            rope(heads(q_d, DSA_HEADS), positions),
            rope(k_d[:, :, None, :], positions)[:, :, 0],
            v_d,
            rope(heads(qi, IDX_HEADS), positions),
            rope(ki[:, :, None, :], positions)[:, :, 0],
            wi)

        o = jnp.concatenate([o_nsa, o_dsa], axis=-1) @ w_out[l]
        x = x + gt1[:, None] * rms_norm(o, g_post_mix[l])

        h = rms_norm(x, g_pre_ffn[l]) * (1.0 + sc2[:, None]) + sh2[:, None]
        y = jnp.square(jax.nn.relu(h @ w_up[l])) @ w_down[l]
        x = x + gt2[:, None] * rms_norm(y, g_post_ffn[l])
    return x
```

```python
import math
from contextlib import ExitStack

import numpy as np
import ml_dtypes

import concourse.bass as bass
import concourse.mybir as mybir
from concourse.bass_utils import run_bass_kernel_spmd

F32 = mybir.dt.float32
BF16 = mybir.dt.bfloat16
I32 = mybir.dt.int32
AF = mybir.ActivationFunctionType
ALU = mybir.AluOpType
AX = mybir.AxisListType

NCORES = 8
SEQ_PER_CORE = 4
T = 2048
D = 1024
NT = T // 128
DFF = 4096
D_IN = 2268
NEGB = -240000.0
EPS = 1e-6
IDX_SCALE = (4 ** -0.5) * (64 ** -0.5)
GELU_C = math.sqrt(2.0 / math.pi)
NBIS = 16


class Sched:
    COMPUTE = ("pe", "act", "dve", "pool")

    def __init__(self, nc, n_dma_sems=12):
        self.nc = nc
        self.ops = []
        self.lastw = {}
        self.readers = {}
        self.n_dma_sems = n_dma_sems
        self.dma_rr = {}
        self.dma_last = {}

    def add(self, eng, fn, r=(), w=(), dma=False, extra_deps=()):
        if getattr(self, "stopped", False):
            return -1
        idx = len(self.ops)
        deps = set(extra_deps)
        if eng != "pe":
            w = list(w) + [x for x in r if isinstance(x, str) and x.startswith("pb") and x not in w]
        pb_ = getattr(self, "pending_bar", None)
        if pb_ and eng in pb_:
            deps.add(pb_.pop(eng))
        for x in r:
            if x in self.lastw:
                deps.add(self.lastw[x])
        for x in w:
            if x in self.lastw:
                deps.add(self.lastw[x])
            for y in self.readers.get(x, ()):
                deps.add(y)
        for x in w:
            self.lastw[x] = idx
            self.readers[x] = []
        for x in r:
            if x not in w:
                self.readers.setdefault(x, []).append(idx)
        op = dict(eng=eng, fn=fn, deps=deps, dma=dma)
        if dma:
            k = self.dma_rr.get(eng, 0)
            self.dma_rr[eng] = k + 1
            slot = (eng, k % self.n_dma_sems)
            prev = self.dma_last.get(slot)
            if prev is not None:
                deps.add(prev)
            self.dma_last[slot] = idx
            op["slot"] = slot
        deps.discard(idx)
        self.ops.append(op)
        return idx

    def barrier(self):
        if getattr(self, "stopped", False):
            return
        live = set(self.lastw.values())
        for v in self.readers.values():
            live.update(v)
        for v in self.dma_last.values():
            live.add(v)
        b = self.add("sp", self.bar_fn, dma=True, extra_deps=live)
        self.lastw = {}
        self.readers = {}
        self.pending_bar = {e: b for e in self.COMPUTE}

    def emit(self):
        nc = self.nc
        ops = self.ops
        n = len(ops)
        es = ExitStack()
        sems = {}
        for e in self.COMPUTE + ("sp",):
            sems[e] = es.enter_context(nc.semaphore("s_" + e))
        dma_slots = sorted({op["slot"] for op in ops if op["dma"]})
        for s in dma_slots:
            sems[s] = es.enter_context(nc.semaphore("d_%s_%d" % s))

        def pe_pe(a, b):
            return a["eng"] == "pe" and b["eng"] == "pe" and not a["dma"] and not b["dma"]

        signaled = [False] * n
        for op in ops:
            for d in op["deps"]:
                if pe_pe(ops[d], op):
                    continue
                signaled[d] = True
        cnt = {}
        sig = [None] * n
        for i, op in enumerate(ops):
            if op["dma"]:
                key = op["slot"]
                cnt[key] = cnt.get(key, 0) + 16
                sig[i] = (key, cnt[key], 16)
            elif signaled[i]:
                key = op["eng"]
                cnt[key] = cnt.get(key, 0) + 1
                sig[i] = (key, cnt[key], 1)
        know = {}
        vc = [None] * n
        waits = [None] * n
        nw = 0
        for i, op in enumerate(ops):
            E = op["eng"]
            K = know.setdefault(E, {})
            best = {}
            for d in sorted(op["deps"], reverse=True):
                if pe_pe(ops[d], op):
                    continue
                key, val, _ = sig[d]
                if K.get(key, 0) >= val:
                    continue
                if best.get(key, 0) < val:
                    best[key] = val
                for k2, v2 in vc[d].items():
                    if K.get(k2, 0) < v2:
                        K[k2] = v2
            waits[i] = list(best.items())
            nw += len(waits[i])
            v = dict(K)
            if sig[i] is not None:
                key, val, _ = sig[i]
                v[key] = val
            vc[i] = v
        self.stats = dict(n_ops=n, n_waits=nw, n_sig=sum(1 for s in sig if s))
        per = {}
        for i, op in enumerate(ops):
            per.setdefault(op["eng"], []).append(i)
        self.stats["per_engine"] = {k: len(v) for k, v in per.items()}

        def run(engobj, name):
            for i in per.get(name, ()):
                op = ops[i]
                for key, val in waits[i]:
                    engobj.wait_ge(sems[key], val)
                ins = op["fn"](engobj)
                if sig[i] is not None:
                    key, val, inc = sig[i]
                    ins.then_inc(sems[key], inc)

        with nc.Block() as block:
            @block.sync
            def _(e):
                run(e, "sp")

            @block.tensor
            def _(e):
                run(e, "pe")

            @block.scalar
            def _(e):
                run(e, "act")

            @block.vector
            def _(e):
                run(e, "dve")

            @block.gpsimd
            def _(e):
                run(e, "pool")
        es.close()


def _win_perm():
    off = dict(q_n=0, kc=512, vc=640, ksl=768, vsl=896, kw=1024, vw=1152, gl=1280,
               q_d=1304, k_d=1816, v_d=1880, qi=1944, ki=2200, wi=2264)

    def head(name, i):
        return list(range(off[name] + 64 * i, off[name] + 64 * (i + 1)))

    nsa = [("q_n", i) for i in range(8)] + [("kc", 0), ("kc", 1), ("ksl", 0), ("ksl", 1), ("kw", 0), ("kw", 1)]
    dsa = [("q_d", i) for i in range(8)] + [("k_d", 0), ("ki", 0)] + [("qi", i) for i in range(4)]
    cols = []
    for p in range(14):
        cols += head(*nsa[p]) + head(*dsa[p])
    cols += head("vc", 0) + head("vc", 1) + head("vsl", 0) + head("vsl", 1) + head("vw", 0) + head("vw", 1)
    cols += head("v_d", 0)
    cols += list(range(off["gl"], off["gl"] + 24)) + list(range(off["wi"], off["wi"] + 4))
    assert len(cols) == D_IN and len(set(cols)) == D_IN
    return np.array(cols)


def _consts():
    bf = ml_dtypes.bfloat16
    c = {}
    eye = np.eye(128, dtype=np.float32)
    c["c_i4"] = np.tile(eye, (1, 4)).astype(bf)
    q = np.arange(128)[:, None]
    k = np.arange(128)[None, :]
    c["c_causal"] = np.where(k <= q, 0.0, NEGB).astype(bf)
    c["c_band"] = np.where(k > q, 0.0, NEGB).astype(bf)
    c["c_causalf"] = np.where(k <= q, 0.0, -1e30).astype(np.float32)
    t = (np.arange(NT)[None, :, None] * 128 + np.arange(128)[:, None, None])
    n = np.arange(128)[None, None, :]
    c["c_cmpbias"] = np.where((16 * n + 31 <= t) & (n < 127), 0.0, NEGB).astype(bf)
    j = np.arange(32)[None, None, :]
    cur = t // 64
    forced = (j == 0) | (j == cur) | (j == cur - 1)
    fb = np.where(j > cur, -100.0, np.where(forced, 100.0, 0.0))
    c["c_forceb"] = fb.astype(np.float32)
    cs = np.arange(128) * 16
    ce = cs + 31
    ss_ = np.arange(32) * 64
    se = ss_ + 63
    ov = ((cs[:, None] <= se[None, :]) & (ce[:, None] >= ss_[None, :]) & (np.arange(128)[:, None] < 127))
    vca = np.zeros((128, 2, 112), np.float32)
    vca[:, :, 64] = 1.0
    vca[:, :, 65:97] = ov[:, None, :]
    c["c_vca"] = vca.astype(bf)
    inv = (np.float32(10000.0) ** (-np.arange(32, dtype=np.float32) / np.float32(32))).astype(np.float32)
    misc = np.zeros((128, 64), np.float32)
    misc[:, 0:32] = inv[None, :]
    misc[:, 32] = -math.pi
    misc[:, 33:33 + NBIS + 1] = (2.0 ** -(np.arange(NBIS + 1) + 1.0))[None, :]
    misc[:, 50] = 1e-30
    c["c_misc"] = misc
    return c


class _Stop(Exception):
    pass


def build_program(nseq=SEQ_PER_CORE, debug=None, stop=None, stop_tile=0, dumps=()):
    nc = bass.Bass("TRN2", target_bir_lowering=False)
    S = Sched(nc)
    add = S.add

    def cut(name, tile=None):
        if stop == name and (tile is None or tile == stop_tile):
            S.stopped = True

    def dump(name, ap, keys, tile=None):
        if name in dumps and (tile is None or tile == stop_tile):
            shp = list(ap.shape)
            d_ = nc.dram_tensor("dbg_" + name, shp, ap.dtype, kind="ExternalOutput").ap()
            add("sp", lambda e: e.dma_start(out=d_, in_=ap), r=list(keys), w=["dbg_" + name], dma=True)

    def din(name, shape, dt=F32):
        return nc.dram_tensor(name, list(shape), dt, kind="ExternalInput").ap()

    x_d = din("x", [SEQ_PER_CORE, T, D])
    cT_d = din("cT", [128, 8, 4])
    pos_d = din("posT", [128, 4, NT], I32)
    wada_d = din("w_ada", [D, 6 * D])
    bada_d = din("b_ada", [1, 6 * D])
    badac_d = din("b_adac", [128, 48])
    gcol_d = din("gcol", [128, 2, 8])
    grow_d = din("grow", [1, 2, D])
    win_d = din("w_in", [D, D_IN])
    pe_d = din("peT", [128, 2, 32])
    w1k_d = din("w1k", [64, 32, 128])
    w1v_d = din("w1v", [128, 32, 128])
    w2_d = din("w2", [128, 2, 64])
    wout_d = din("w_out", [D, D])
    wup_d = din("w_up", [D, DFF])
    wdn_d = din("w_down", [DFF, D])
    consts = _consts()
    cd = {k: din(k, v.shape, BF16 if v.dtype == ml_dtypes.bfloat16 else F32) for k, v in consts.items()}
    out_d = nc.dram_tensor("out", [SEQ_PER_CORE, T, D], F32, kind="ExternalOutput").ap()
    S.bar_fn = lambda e: e.dma_start(out=bar_d[1:2, :], in_=cd["c_misc"][0:1, :])
    gscr_d = nc.dram_tensor("gscr", [4, 2, D], F32, kind="Internal").ap()
    bar_d = nc.dram_tensor("bar_scr", [2, 64], F32, kind="Internal").ap()
    dbg_d = {}
    if debug:
        for name, shape in debug.items():
            dbg_d[name] = nc.dram_tensor("dbg_" + name, list(shape), F32, kind="ExternalOutput").ap()

    top = ExitStack()

    def sbuf(es, name, shape, dt):
        return es.enter_context(nc.sbuf_tensor("s_" + name, list(shape), dt))

    pb = [top.enter_context(nc.psum_tensor("pb%d" % i, [128, 512], F32)) for i in range(8)]

    def pbf(i):
        return pb[i][:].bitcast(BF16)

    def bc(ap, shape):
        return ap.to_broadcast(list(shape))

    P = top
    i4 = sbuf(P, "i4", [128, 512], BF16)
    causal = sbuf(P, "causal", [128, 128], BF16)
    band = sbuf(P, "band", [128, 128], BF16)
    causalf = sbuf(P, "causalf", [128, 128], F32)
    misc = sbuf(P, "misc", [128, 64], F32)
    AB = sbuf(P, "AB", [128, 4, 4, 8], F32)
    for name, t_ in (("c_i4", i4), ("c_causal", causal), ("c_band", band), ("c_causalf", causalf),
                     ("c_misc", misc)):
        add("sp", lambda e, t_=t_, name=name: e.dma_start(out=t_[:], in_=cd[name]), w=[name], dma=True)
    CONST_R = ["c_i4", "c_causal", "c_band", "c_causalf", "c_misc"]
    invf = misc[:, 0:32]
    negpi = misc[:, 32:33]

    def _build_body():
        with ExitStack() as es0:
            cT = sbuf(es0, "cT", [128, 8, 4], F32)
            badar = sbuf(es0, "badar", [4, 6 * D], F32)
            badac = sbuf(es0, "badac", [128, 48], F32)
            gcol = sbuf(es0, "gcol", [128, 2, 8], F32)
            growb = sbuf(es0, "growb", [4, 2, D], F32)
            modT = sbuf(es0, "modT", [128, 4, 8, 4], F32)
            wst = [sbuf(es0, "wst%d" % i, [128, 8, 512], F32) for i in range(2)]
            add("sp", lambda e: e.dma_start(out=cT[:], in_=cT_d), w=["cT"], dma=True)
            add("sp", lambda e: e.dma_start(out=badar[:], in_=bc(bada_d, [4, 6 * D])), w=["badar"], dma=True)
            add("sp", lambda e: e.dma_start(out=badac[:], in_=badac_d), w=["badac"], dma=True)
            add("sp", lambda e: e.dma_start(out=gcol[:], in_=gcol_d), w=["gcol"], dma=True)
            add("sp", lambda e: e.dma_start(out=growb[:], in_=bc(grow_d, [4, 2, D])), w=["growb"], dma=True)
            wada_v = wada_d.rearrange("(k p) n -> p k n", p=128)
            colmap = {0: 0, 1: 0, 2: 1, 3: 1, 6: 2, 7: 2, 8: 3, 9: 3}
            rowmap = {4: (0, 0), 5: (0, 1), 10: (1, 0), 11: (1, 1)}
            for cc in range(12):
                st = wst[cc % 2]
                key = "wst%d" % (cc % 2)
                add("sp", lambda e, st=st, cc=cc: e.dma_start(out=st[:], in_=wada_v[:, :, cc * 512:(cc + 1) * 512]),
                    w=[key], dma=True)
                if cc in colmap:
                    for q in range(4):
                        jj = colmap[cc] * 8 + (cc % 2) * 4 + q
                        for k in range(8):
                            add("pe", lambda e, st=st, k=k, q=q, jj=jj: e.matmul(pb[2][:, jj * 4:(jj + 1) * 4], lhsT=st[:, k, q * 128:(q + 1) * 128],
                                                                                 rhs=cT[:, k, :], start=(k == 0), stop=(k == 7)),
                                r=["cT", key], w=["pb2"])
                else:
                    gi, half = rowmap[cc]
                    for k in range(8):
                        add("pe", lambda e, st=st, k=k: e.matmul(pb[0][0:4, :], lhsT=cT[:, k, :], rhs=st[:, k, :],
                                                                 start=(k == 0), stop=(k == 7)),
                            r=["cT", key], w=["pb0"])
                    hs_ = slice(half * 512, (half + 1) * 512)
                    add("dve", lambda e, cc=cc: e.tensor_tensor(out=badar[:, cc * 512:(cc + 1) * 512], in0=pb[0][0:4, :],
                                                                 in1=badar[:, cc * 512:(cc + 1) * 512], op=ALU.add),
                        r=["pb0", "badar"], w=["badar"])
                    add("dve", lambda e, cc=cc, gi=gi, hs_=hs_: e.tensor_tensor(out=growb[:, gi, hs_], in0=growb[:, gi, hs_],
                                                                                  in1=badar[:, cc * 512:(cc + 1) * 512], op=ALU.mult),
                        r=["badar", "growb"], w=["growb"])
            for a_, ch in enumerate((0, 1, 3, 4)):
                add("dve", lambda e, a_=a_, ch=ch: e.tensor_tensor(out=modT[:, a_, :, :], in0=pb[2][:, a_ * 32:(a_ + 1) * 32].rearrange("p (k b) -> p k b", b=4),
                                                                    in1=bc(badac[:, ch * 8:(ch + 1) * 8].unsqueeze(2), [128, 8, 4]), op=ALU.add),
                    r=["pb2", "badac"], w=["modT"])
            for b in range(4):
                for (dst, src, gi) in ((0, 1, 0), (2, 3, 1)):
                    add("dve", lambda e, b=b, dst=dst, src=src: e.tensor_scalar(out=AB[:, dst, b, :], in0=modT[:, src, :, b],
                                                                                 scalar1=1.0, scalar2=None, op0=ALU.add),
                        r=["modT"], w=["AB"])
                    add("dve", lambda e, b=b, dst=dst, gi=gi: e.tensor_tensor(out=AB[:, dst, b, :], in0=AB[:, dst, b, :],
                                                                               in1=gcol[:, gi, :], op=ALU.mult),
                        r=["gcol"], w=["AB"])
                for (dst, src) in ((1, 0), (3, 2)):
                    add("dve", lambda e, b=b, dst=dst, src=src: e.tensor_copy(out=AB[:, dst, b, :], in_=modT[:, src, :, b]),
                        r=["modT"], w=["AB"])
            add("sp", lambda e: e.dma_start(out=gscr_d, in_=growb[:]), r=["growb"], w=["gscr"], dma=True)
            dump("growb", growb[:].rearrange("p a d -> p (a d)"), ["growb"])
            dump("AB", AB[:].rearrange("p a b k -> p (a b k)"), ["AB"])
            S.barrier()
        cut("setup")

        with ExitStack() as es1:
            M = es1
            Win = sbuf(M, "Win", [128, 8, D_IN], BF16)
            Wout = sbuf(M, "Wout", [128, 8, D], BF16)
            W1k = sbuf(M, "W1k", [64, 32, 128], BF16)
            W1v = sbuf(M, "W1v", [128, 32, 128], BF16)
            W2 = sbuf(M, "W2", [128, 2, 64], BF16)
            peT = sbuf(M, "peT", [128, 2, 32], BF16)
            bT = sbuf(M, "bT", [128, 2], F32)
            cmpbias = sbuf(M, "cmpbias", [128, NT, 128], BF16)
            forceb = sbuf(M, "forceb", [128, NT, 32], F32)
            VCa = sbuf(M, "VCa", [128, 2, 112], BF16)
            add("sp", lambda e: e.dma_start(out=cmpbias[:], in_=cd["c_cmpbias"]), w=["c_cmpbias"], dma=True)
            add("sp", lambda e: e.dma_start(out=forceb[:], in_=cd["c_forceb"]), w=["c_forceb"], dma=True)
            add("sp", lambda e: e.dma_start(out=VCa[:], in_=cd["c_vca"]), w=["VCa"], dma=True)

            with ExitStack() as es_w:
                wst = [sbuf(es_w, "wstm%d" % i, [128, 8, 512], F32) for i in range(2)]
                cnt = [0]

                def load_cast(dst_fn, src_ap, shape, eng="pool"):
                    i = cnt[0] % 2
                    cnt[0] += 1
                    st = wst[i]
                    key = "wstm%d" % i
                    a, n_ = shape[1], shape[2]
                    view = st[0:shape[0], :, :].rearrange("p a n -> p (a n)")[:, 0:a * n_].rearrange("p (a n) -> p a n", a=a)
                    add("sp", lambda e: e.dma_start(out=view, in_=src_ap), w=[key], dma=True)
                    add(eng, lambda e: e.tensor_copy(out=dst_fn, in_=view), r=[key], w=["weights"])

                win_v = win_d.rearrange("(k p) n -> p k n", p=128)
                c0 = 0
                while c0 < D_IN:
                    cw = min(512, D_IN - c0)
                    load_cast(Win[:, :, c0:c0 + cw], win_v[:, :, c0:c0 + cw], [128, 8, cw], eng="pool" if (c0 // 512) % 2 else "dve")
                    c0 += cw
                wout_v = wout_d.rearrange("(k p) n -> p k n", p=128)
                for c0 in range(0, D, 512):
                    load_cast(Wout[:, :, c0:c0 + 512], wout_v[:, :, c0:c0 + 512], [128, 8, 512])
                for l0 in range(0, 32, 16):
                    load_cast(W1k[:, l0:l0 + 16, :], w1k_d[:, l0:l0 + 16, :], [64, 16, 128])
                    load_cast(W1v[:, l0:l0 + 16, :], w1v_d[:, l0:l0 + 16, :], [128, 16, 128])
                load_cast(W2[:], w2_d, [128, 2, 64])
                load_cast(peT[:], pe_d, [128, 2, 32])
                for l in range(32):
                    add("pe", lambda e, l=l: e.matmul(pb[0][:, 0:1], lhsT=W1k[0:64, l, :], rhs=peT[0:64, 0, l:l + 1],
                                                      start=(l == 0), stop=(l == 31)), r=["weights"], w=["pb0"])
                for l in range(32):
                    add("pe", lambda e, l=l: e.matmul(pb[1][:, 0:1], lhsT=W1v[0:64, l, :], rhs=peT[0:64, 1, l:l + 1],
                                                      start=(l == 0), stop=(l == 31)), r=["weights"], w=["pb1"])
                add("dve", lambda e: e.tensor_copy(out=bT[:, 0:1], in_=pb[0][:, 0:1]), r=["pb0"], w=["bT"])
                add("dve", lambda e: e.tensor_copy(out=bT[:, 1:2], in_=pb[1][:, 0:1]), r=["pb1"], w=["bT"])
                dump("bT", bT[:], ["bT"])
                S.barrier()
            cut("weights")

            KT = sbuf(M, "KT", [128, 6, T], BF16)
            VA = sbuf(M, "VA", [128, NT, 5, 80], BF16)
            VCT = sbuf(M, "VCT", [128, T], BF16)
            GH = sbuf(M, "GH", [128, 2, 2, 128], BF16)
            KCT = sbuf(M, "KCT", [64, 2, 128], BF16)
            cosT = sbuf(M, "cosT", [128, NT, 32], F32)
            sinT = sbuf(M, "sinT", [128, NT, 32], F32)
            posf = sbuf(M, "posf", [128, NT], F32)
            posi = sbuf(M, "posi", [128, 4, NT], I32)
            G1 = sbuf(M, "G1", [128, D], F32)
            xt = [sbuf(M, "xt%d" % i, [128, D], F32) for i in range(2)]
            xn = sbuf(M, "xn", [128, D], BF16)
            st4 = sbuf(M, "st4", [128, 8], F32)
            hT = sbuf(M, "hT", [128, 8, 128], BF16)
            rq = sbuf(M, "rq", [128, 28, 2, 32], BF16)
            rtmp = sbuf(M, "rtmp", [128, 4, 8, 32], F32)
            vct = sbuf(M, "vct", [128, 128], BF16)
            qT = sbuf(M, "qT", [128, 14, 128], BF16)
            gate = sbuf(M, "gate", [128, 8, 3], F32)
            WA = sbuf(M, "WA", [128, 4], F32)
            WS = sbuf(M, "WS", [128, 4], F32)
            hb = sbuf(M, "hb", [128, 4, 8], F32)
            Ygk = sbuf(M, "Ygk", [64, 2, 32, 8], BF16)
            Ygv = sbuf(M, "Ygv", [128, 32, 8], BF16)
            hu = sbuf(M, "hu", [128, 4, 8], F32)
            PT = sbuf(M, "PT", [128, 8, 512], BF16)
            PTc = sbuf(M, "PTc", [128, 512], BF16)
            rc = sbuf(M, "rc", [128, 8], F32)
            imp = sbuf(M, "imp", [128, 2, 32], F32)
            tk = sbuf(M, "tk", [128, 80], F32)
            selb = sbuf(M, "selb", [128, 2, 32], BF16)
            selx = sbuf(M, "selx", [128, T], BF16)
            score = sbuf(M, "score", [128, T], F32)
            junk2 = sbuf(M, "junk2", [128, T], BF16)
            atmp = [sbuf(M, "atmp%d" % i, [128, 512], F32) for i in range(2)]
            maskb = sbuf(M, "maskb", [128, T], BF16)
            bis = sbuf(M, "bis", [128, 32], F32)
            wtab = sbuf(M, "wtab", [128, NBIS + 1], F32)
            Ot = sbuf(M, "Ot", [128, D], F32)
            otmp = sbuf(M, "otmp", [128, 256], F32)
            Otb = sbuf(M, "Otb", [128, D], BF16)
            oT = sbuf(M, "oT", [128, 8, 128], BF16)
            x1t = sbuf(M, "x1t", [128, D], F32)

            add("sp", lambda e: e.dma_start(out=posi[:], in_=pos_d), w=["posi"], dma=True)
            add("pool", lambda e: e.memset(VA[:, :, :, 64:65], 1.0), w=[("VA", t_) for t_ in range(NT)])
            add("pool", lambda e: e.memset(qT[:], 0.0), w=["qT"])
            add("pool", lambda e: e.memset(KT[:], 0.0), w=[("KT", t_) for t_ in range(NT)] + [("KTd", t_) for t_ in range(NT)])

            def rstd_from_ss(ss_ap, out_ap, rkeys, wkey):
                add("dve", lambda e: e.tensor_scalar(out=out_ap, in0=ss_ap, scalar1=1.0 / D, scalar2=EPS,
                                                     op0=ALU.mult, op1=ALU.add), r=rkeys, w=[wkey])
                add("act", lambda e: e.activation(out=out_ap, in_=out_ap, func=AF.Ln), r=[wkey], w=[wkey])
                add("act", lambda e: e.activation(out=out_ap, in_=out_ap, func=AF.Exp, scale=-0.5), r=[wkey], w=[wkey])

            def pv_evac(bank, bkey, width, dst_cols, gate_br, g, first):
                v = bank[:, 0:512].rearrange("p (h w) -> p h w", h=4)
                add("act", lambda e: e.activation(out=rc[:, 0:4], in_=v[:, :, 64], func=AF.Ln, bias=misc[:, 50:51], scale=1.0),
                    r=[bkey, "c_misc"], w=["rc"])
                add("act", lambda e: e.activation(out=rc[:, 0:4], in_=rc[:, 0:4], func=AF.Exp, scale=-1.0), r=["rc"], w=["rc"])
                if gate_br is not None:
                    add("pool", lambda e: e.tensor_tensor(out=rc[:, 4:8], in0=rc[:, 0:4], in1=gate[:, 4 * g:4 * g + 4, gate_br],
                                                          op=ALU.mult), r=["rc", "gate"], w=["rcg"])
                    rcs, rkey = rc[:, 4:8], "rcg"
                else:
                    rcs, rkey = rc[:, 0:4], "rc"
                if first:
                    for h in range(4):
                        add("act", lambda e, h=h: e.activation(out=Ot[:, dst_cols + 64 * h:dst_cols + 64 * (h + 1)], in_=v[:, h, 0:64], func=AF.Copy,
                                                               scale=rcs[:, h:h + 1]), r=[bkey, rkey], w=["Ot"])
                else:
                    for h in range(4):
                        add("act", lambda e, h=h: e.activation(out=otmp[:, 64 * h:64 * (h + 1)], in_=v[:, h, 0:64], func=AF.Copy,
                                                               scale=rcs[:, h:h + 1]), r=[bkey, rkey], w=["otmp"])
                    add("pool", lambda e: e.tensor_tensor(out=Ot[:, dst_cols:dst_cols + 256], in0=Ot[:, dst_cols:dst_cols + 256], in1=otmp[:], op=ALU.add),
                        r=["otmp"], w=["Ot"])

            pt_i = [0]
            sbank = [4, 5]
            sb_i = [0]
            obank = [6, 7]
            ob_i = [0]

            def attention(t, kts, kslice_fn, q_rhs, mask_fn, v_fn, width=65):
                ob = obank[ob_i[0] % 2]
                ob_i[0] += 1
                okey = "pb%d" % ob
                first_pv = [True]
                for b0 in range(0, len(kts), 4):
                    blk = kts[b0:b0 + 4]
                    pt0 = (pt_i[0] % 2) * 4
                    pt_i[0] += 1
                    for j0, kt in enumerate(blk):
                        j = pt0 + j0
                        sbk = sbank[sb_i[0] % 2]
                        sb_i[0] += 1
                        skey = "pb%d" % sbk
                        lhs, kkeys = kslice_fn(kt)
                        m = mask_fn(kt)
                        add("pe", lambda e, sbk=sbk, lhs=lhs, m=m: e.matmul(pb[sbk][:, :], lhsT=lhs, rhs=q_rhs, start=True, stop=(m is None)),
                            r=kkeys + ["qT"], w=[skey])
                        if m is not None:
                            mlhs, mkeys = m
                            add("pe", lambda e, sbk=sbk, mlhs=mlhs: e.matmul(pb[sbk][:, :], lhsT=mlhs, rhs=i4[:, :], start=False, stop=True),
                                r=mkeys + ["c_i4"], w=[skey])
                        add("act", lambda e, sbk=sbk, j=j: e.activation(out=PT[:, j, :], in_=pb[sbk][:, :], func=AF.Exp, scale=0.125),
                            r=[skey], w=[("PT", j)])
                    for h in range(4):
                        for j0, kt in enumerate(blk):
                            j = pt0 + j0
                            rhs, vkeys = v_fn(kt)
                            st_flag = first_pv[0]
                            first_pv[0] = False
                            add("pe", lambda e, ob=ob, h=h, j=j, rhs=rhs, st_flag=st_flag: e.matmul(
                                pb[ob][:, h * 128:h * 128 + width], lhsT=PT[:, j, h * 128:(h + 1) * 128], rhs=rhs,
                                start=st_flag, stop=False, skip_group_check=True),
                                r=[("PT", j)] + vkeys, w=[okey])
                return pb[ob], okey

            for b in range(nseq):
                add("dve", lambda e, b=b: e.tensor_copy(out=posf[:], in_=posi[:, b, :]), r=["posi"], w=["posf"])
                ang = score[:, 0:NT * 32].rearrange("p (t j) -> p t j", t=NT)
                angk = score[:, 512:512 + NT * 32].rearrange("p (t j) -> p t j", t=NT)
                angi = junk2[:, 0:NT * 64].bitcast(I32).rearrange("p (t j) -> p t j", t=NT)
                for (dstT, shift) in ((sinT, 0.5), (cosT, 0.75)):
                    add("dve", lambda e: e.tensor_tensor(out=ang, in0=bc(invf.unsqueeze(1), [128, NT, 32]),
                                                         in1=bc(posf[:].unsqueeze(2), [128, NT, 32]), op=ALU.mult),
                        r=["posf", "c_misc"], w=["score"])
                    add("dve", lambda e, shift=shift: e.tensor_scalar(out=ang, in0=ang, scalar1=1.0 / (2.0 * math.pi), scalar2=shift,
                                                                       op0=ALU.mult, op1=ALU.add), r=["score"], w=["score"])
                    add("dve", lambda e: e.tensor_copy(out=angi, in_=ang), r=["score"], w=["junk2"])
                    add("dve", lambda e: e.tensor_copy(out=angk, in_=angi), r=["junk2"], w=["score"])
                    add("dve", lambda e: e.tensor_tensor(out=ang, in0=ang, in1=angk, op=ALU.subtract), r=["score"], w=["score"])
                    add("dve", lambda e: e.scalar_tensor_tensor(out=ang, in0=ang, scalar=0.0, in1=ang, op0=ALU.is_lt, op1=ALU.add),
                        r=["score"], w=["score"])
                    add("act", lambda e, dstT=dstT: e.activation(out=dstT[:], in_=ang, func=AF.Sin, bias=negpi, scale=2.0 * math.pi),
                        r=["score", "c_misc"], w=["rope_tab"])
                add("sp", lambda e, b=b: e.dma_start(out=G1[:], in_=bc(gscr_d[b:b + 1, 0, :], [128, D])), r=["gscr"], w=["G1"], dma=True)
                add("pool", lambda e: e.memset(GH[:], 0.0), w=["GH"])

                for t in range(NT):
                    xb = xt[t % 2]
                    xkey = "xt%d" % (t % 2)
                    tsl = slice(t * 128, (t + 1) * 128)
                    add("sp", lambda e, b=b, tsl=tsl, xb=xb: e.dma_start(out=xb[:], in_=x_d[b, tsl, :]), w=[xkey], dma=True)
                    add("dve", lambda e: e.memset(st4[:, 0:1], 0.0), w=["ss0"])
                    add("act", lambda e, xb=xb: e.activation(out=xn[:], in_=xb[:], func=AF.Square, accum_out=st4[:, 0:1]),
                        r=[xkey], w=["xn", "ss0"])
                    rstd_from_ss(st4[:, 0:1], st4[:, 1:2], ["ss0"], "rstd")
                    add("act", lambda e, xb=xb: e.activation(out=xn[:], in_=xb[:], func=AF.Copy, scale=st4[:, 1:2]),
                        r=[xkey, "rstd"], w=["xn"])
                    for k in range(8):
                        add("pe", lambda e, k=k: e.transpose(out=pbf(0)[:, k * 128:(k + 1) * 128], in_=xn[:, k * 128:(k + 1) * 128],
                                                             identity=i4[:, 0:128]), r=["xn", "c_i4"], w=["pb0"])
                    for k in range(8):
                        add("dve", lambda e, k=k, b=b: e.tensor_scalar(out=hT[:, k, :], in0=pbf(0)[:, k * 128:(k + 1) * 128],
                                                                        scalar1=AB[:, 0, b, k:k + 1], scalar2=AB[:, 1, b, k:k + 1],
                                                                        op0=ALU.mult, op1=ALU.add), r=["pb0", "AB"], w=["hT"])
                    chunks = [(0, 512), (512, 512), (1024, 512), (1536, 256), (1792, 476)]
                    for ci, (c0, cw) in enumerate(chunks):
                        bk = 1 + (ci % 2)
                        bkey = "pb%d" % bk
                        for k in range(8):
                            add("pe", lambda e, k=k, bk=bk, c0=c0, cw=cw: e.matmul(pb[bk][:, 0:cw], lhsT=hT[:, k, :], rhs=Win[:, k, c0:c0 + cw],
                                                                                   start=(k == 0), stop=(k == 7)), r=["hT", "weights"], w=[bkey])
                        if ci < 4:
                            nh = cw // 64
                            h0 = c0 // 64
                            pv = pb[bk][:, 0:cw].rearrange("p (h two j) -> p h two j", two=2, j=32)
                            cs_ = bc(cosT[:, t, :].unsqueeze(1), [128, nh, 32])
                            sn_ = bc(sinT[:, t, :].unsqueeze(1), [128, nh, 32])
                            add("dve", lambda e, pv=pv, cs_=cs_, nh=nh: e.tensor_tensor(out=rtmp[:, 0, 0:nh, :], in0=pv[:, :, 0, :], in1=cs_, op=ALU.mult),
                                r=[bkey, "rope_tab"], w=["rt0"])
                            add("dve", lambda e, pv=pv, sn_=sn_, nh=nh: e.tensor_tensor(out=rtmp[:, 1, 0:nh, :], in0=pv[:, :, 1, :], in1=sn_, op=ALU.mult),
                                r=[bkey, "rope_tab"], w=["rt1"])
                            add("dve", lambda e, pv=pv, cs_=cs_, nh=nh: e.tensor_tensor(out=rtmp[:, 2, 0:nh, :], in0=pv[:, :, 1, :], in1=cs_, op=ALU.mult),
                                r=[bkey, "rope_tab"], w=["rt2"])
                            add("dve", lambda e, pv=pv, sn_=sn_, nh=nh: e.tensor_tensor(out=rtmp[:, 3, 0:nh, :], in0=pv[:, :, 0, :], in1=sn_, op=ALU.mult),
                                r=[bkey, "rope_tab"], w=["rt3"])
                            add("pool", lambda e, nh=nh, h0=h0: e.tensor_tensor(out=rq[:, h0:h0 + nh, 0, :], in0=rtmp[:, 0, 0:nh, :], in1=rtmp[:, 1, 0:nh, :],
                                                                                 op=ALU.subtract), r=["rt0", "rt1"], w=["rq"])
                            add("pool", lambda e, nh=nh, h0=h0: e.tensor_tensor(out=rq[:, h0:h0 + nh, 1, :], in0=rtmp[:, 2, 0:nh, :], in1=rtmp[:, 3, 0:nh, :],
                                                                                 op=ALU.add), r=["rt2", "rt3"], w=["rq"])
                        else:
                            pvv = pb[bk]
                            add("act", lambda e, pvv=pvv: e.activation(out=vct[:], in_=pvv[:, 0:128], func=AF.Copy), r=[bkey], w=["vct"])
                            add("act", lambda e, pvv=pvv, t=t: e.activation(out=VA[:, t, :, 0:64], in_=pvv[:, 128:448].rearrange("p (s d) -> p s d", s=5),
                                                                             func=AF.Copy), r=[bkey], w=[("VA", t)])
                            g24 = gate[:].rearrange("p h c -> p (h c)")
                            add("act", lambda e, pvv=pvv: e.activation(out=g24, in_=pvv[:, 448:472], func=AF.Exp, scale=-1.0), r=[bkey], w=["gate"])
                            add("dve", lambda e: e.tensor_scalar(out=g24, in0=g24, scalar1=1.0, scalar2=None, op0=ALU.add), r=["gate"], w=["gate"])
                            add("dve", lambda e: e.reciprocal(out=g24, in_=g24), r=["gate"], w=["gate"])
                            add("dve", lambda e, pvv=pvv: e.tensor_scalar(out=WS[:], in0=pvv[:, 472:476], scalar1=0.0, scalar2=2.0,
                                                                           op0=ALU.is_ge, op1=ALU.mult), r=[bkey], w=["WS"])
                            add("dve", lambda e: e.tensor_scalar(out=WS[:], in0=WS[:], scalar1=-1.0, scalar2=None, op0=ALU.add), r=["WS"], w=["WS"])
                            add("dve", lambda e, pvv=pvv: e.scalar_tensor_tensor(out=WA[:], in0=pvv[:, 472:476], scalar=IDX_SCALE, in1=WS[:],
                                                                                  op0=ALU.mult, op1=ALU.mult), r=[bkey, "WS"], w=["WA"])
                    rq2 = rq[:].rearrange("p h two j -> p (h two j)")
                    for p_ in range(8):
                        add("pe", lambda e, p_=p_: e.transpose(out=pbf(3)[:, p_ * 128:(p_ + 1) * 128], in_=rq2[:, p_ * 128:(p_ + 1) * 128],
                                                               identity=i4[:, 0:128]), r=["rq", "c_i4"], w=["pb3"])
                    add("act", lambda e: e.activation(out=qT[:, 0:8, :], in_=pbf(3)[:, :].rearrange("p (s q) -> p s q", s=8), func=AF.Copy),
                        r=["pb3"], w=["qT"])
                    for p_ in range(8, 14):
                        add("pe", lambda e, p_=p_: e.transpose(out=pbf(3)[:, (p_ - 8) * 128:(p_ - 7) * 128], in_=rq2[:, p_ * 128:(p_ + 1) * 128],
                                                               identity=i4[:, 0:128]), r=["rq", "c_i4"], w=["pb3"])
                    p3 = pbf(3)[:, 0:768].rearrange("p (s q) -> p s q", s=6)
                    add("dve", lambda e, p3=p3, tsl=tsl: e.tensor_copy(out=KT[0:64, 0:6, tsl], in_=p3[0:64, :, :]), r=["pb3"], w=[("KT", t)])
                    add("act", lambda e, p3=p3, tsl=tsl: e.activation(out=KT[64:128, 0:2, tsl], in_=p3[64:128, 0:2, :], func=AF.Copy),
                        r=["pb3"], w=[("KTd", t)])
                    add("act", lambda e, p3=p3: e.activation(out=qT[64:128, 10:14, :], in_=p3[64:128, 2:6, :], func=AF.Copy), r=["pb3"], w=["qT"])
                    add("pe", lambda e: e.transpose(out=pbf(0)[:, 0:128], in_=vct[:], identity=i4[:, 0:128]), r=["vct", "c_i4"], w=["pb0"])
                    add("dve", lambda e, tsl=tsl: e.tensor_copy(out=VCT[:, tsl], in_=pbf(0)[:, 0:128]), r=["pb0"], w=[("VCT", t)])

                    dump("G1", G1[:], ["G1"], t)
                    dump("cosT", cosT[:].rearrange("p t j -> p (t j)"), ["rope_tab"], t)
                    dump("sinT", sinT[:].rearrange("p t j -> p (t j)"), ["rope_tab"], t)
                    dump("hT", hT[:].rearrange("p k q -> p (k q)"), ["hT"], t)
                    dump("rq", rq[:].rearrange("p h two j -> p (h two j)"), ["rq"], t)
                    dump("qT", qT[:].rearrange("p s q -> p (s q)"), ["qT"], t)
                    dump("gate", gate[:].rearrange("p h c -> p (h c)"), ["gate"], t)
                    dump("WA", WA[:], ["WA"], t)
                    dump("WS", WS[:], ["WS"], t)
                    dump("VA", VA[:, t, :, 0:65], [("VA", t)], t)
                    dump("KT", KT[:, :, tsl], [("KT", t), ("KTd", t)], t)
                    cut("P", t)
                    n0 = 0 if t == 0 else 8 * t - 1
                    nb = 7 if t == 0 else 8
                    kkeys = [("KT", tt) for tt in range(max(0, t - 1), t + 1)]
                    vkeys_ = [("VCT", tt) for tt in range(max(0, t - 1), t + 1)]
                    tok0 = 16 * n0
                    xk = KT[0:64, 0:2, tok0:tok0 + 16 * (nb + 1)].rearrange("p g (n l) -> p g n l", l=16)
                    xv = VCT[:, tok0:tok0 + 16 * (nb + 1)].rearrange("p (n l) -> p n l", l=16)
                    for lhi in range(2):
                        add("pool", lambda e, lhi=lhi, xk=xk, nb=nb: e.tensor_copy(out=Ygk[:, :, lhi * 16:(lhi + 1) * 16, 0:nb],
                                                                                    in_=xk[:, :, lhi:lhi + nb, :].rearrange("p g n l -> p g l n")),
                            r=kkeys, w=["Ygk"])
                        add("pool", lambda e, lhi=lhi, xv=xv, nb=nb: e.tensor_copy(out=Ygv[:, lhi * 16:(lhi + 1) * 16, 0:nb],
                                                                                    in_=xv[:, lhi:lhi + nb, :].rearrange("p n l -> p l n")),
                            r=vkeys_, w=["Ygv"])
                    for kv in range(2):
                        for g in range(2):
                            jj = kv * 2 + g
                            for l in range(32):
                                if kv == 0:
                                    lhs = W1k[0:64, l, :]
                                    rhs = Ygk[:, g, l, 0:nb]
                                    rk = ["Ygk"]
                                else:
                                    lhs = W1v[g * 64:(g + 1) * 64, l, :]
                                    rhs = Ygv[g * 64:(g + 1) * 64, l, 0:nb]
                                    rk = ["Ygv"]
                                cb = 2 if jj == 3 else 1
                                add("pe", lambda e, lhs=lhs, rhs=rhs, jj=jj, l=l, nb=nb, cb=cb: e.matmul(pb[cb][:, jj * 8:jj * 8 + nb], lhsT=lhs, rhs=rhs,
                                                                                                          start=(l == 0), stop=(l == 31)),
                                    r=rk + ["weights"], w=["pb%d" % cb])
                    cut("C0", t)
                    pch = pb[1][:, 0:32].rearrange("p (a n) -> p a n", a=4)
                    pch2 = pb[2][:, 0:32].rearrange("p (a n) -> p a n", a=4)
                    add("dve", lambda e: e.tensor_scalar(out=hb[:, 0:2, :], in0=pch[:, 0:2, :], scalar1=bT[:, 0:1], scalar2=None, op0=ALU.add),
                        r=["pb1", "bT"], w=["hb"])
                    add("dve", lambda e: e.tensor_scalar(out=hb[:, 2:3, :], in0=pch[:, 2:3, :], scalar1=bT[:, 1:2], scalar2=None, op0=ALU.add),
                        r=["pb1", "bT"], w=["hb"])
                    add("dve", lambda e: e.tensor_scalar(out=hb[:, 3:4, :], in0=pch2[:, 3:4, :], scalar1=bT[:, 1:2], scalar2=None, op0=ALU.add),
                        r=["pb2", "bT"], w=["hb"])
                    add("dve", lambda e: e.tensor_tensor(out=hu[:], in0=hb[:], in1=hb[:], op=ALU.mult), r=["hb"], w=["hu"])
                    add("dve", lambda e: e.tensor_scalar(out=hu[:], in0=hu[:], scalar1=0.044715, scalar2=1.0, op0=ALU.mult, op1=ALU.add), r=["hu"], w=["hu"])
                    add("dve", lambda e: e.tensor_tensor(out=hu[:], in0=hu[:], in1=hb[:], op=ALU.mult), r=["hu", "hb"], w=["hu"])
                    add("act", lambda e: e.activation(out=hu[:], in_=hu[:], func=AF.Exp, scale=-2.0 * GELU_C), r=["hu"], w=["hu"])
                    add("dve", lambda e: e.tensor_scalar(out=hu[:], in0=hu[:], scalar1=1.0, scalar2=None, op0=ALU.add), r=["hu"], w=["hu"])
                    add("dve", lambda e: e.reciprocal(out=hu[:], in_=hu[:]), r=["hu"], w=["hu"])
                    hb4 = hb[:].rearrange("p (kv g) n -> p kv g n", kv=2)
                    hu4 = hu[:].rearrange("p (kv g) n -> p kv g n", kv=2)
                    add("dve", lambda e, n0=n0, nb=nb: e.tensor_tensor(out=GH[:, :, :, n0:n0 + nb], in0=hb4[:, :, :, 0:nb], in1=hu4[:, :, :, 0:nb], op=ALU.mult),
                        r=["hb", "hu"], w=["GH"])
                    dump("GH1", GH[:].rearrange("p a g n -> p (a g n)"), ["GH"], t)
                    cut("C1", t)
                    for g in range(2):
                        add("pe", lambda e, g=g: e.matmul(pb[2][0:64, g * 128:(g + 1) * 128], lhsT=W2[:, 0, :], rhs=GH[:, 0, g, :], start=True, stop=True),
                            r=["GH", "weights"], w=["pb2"])
                    add("act", lambda e: e.activation(out=KCT[:, :, :], in_=pb[2][0:64, 0:256].rearrange("p (g n) -> p g n", g=2), func=AF.Copy),
                        r=["pb2"], w=["KCT"])
                    for g in range(2):
                        add("pe", lambda e, g=g: e.matmul(pb[3][:, g * 64:(g + 1) * 64], lhsT=GH[:, 1, g, :], rhs=W2[:, 1, :], start=True, stop=True),
                            r=["GH", "weights"], w=["pb3"])
                    add("act", lambda e: e.activation(out=VCa[:, :, 0:64], in_=pb[3][:, 0:128].rearrange("p (g d) -> p g d", g=2), func=AF.Copy),
                        r=["pb3"], w=["VCa"])

                    dump("GH", GH[:].rearrange("p a g n -> p (a g n)"), ["GH"], t)
                    dump("KCT", KCT[:].rearrange("p g n -> p (g n)"), ["KCT"], t)
                    dump("VCa", VCa[:, :, 0:97], ["VCa"], t)
                    cut("C", t)
                    nkeys = (t + 1) * 128
                    if t >= 2:
                        for c0 in range(0, nkeys, 512):
                            cw = min(512, nkeys - c0)
                            kk = [("KTd", kt) for kt in range(c0 // 128, (c0 + cw) // 128)]
                            for h in range(4):
                                bk = 1 + (h % 2)
                                bkey = "pb%d" % bk
                                at = atmp[h % 2]
                                akey = "atmp%d" % (h % 2)
                                add("pe", lambda e, bk=bk, h=h, c0=c0, cw=cw: e.matmul(pb[bk][:, 0:cw], lhsT=qT[64:128, 10 + h, :], rhs=KT[64:128, 1, c0:c0 + cw],
                                                                                       start=True, stop=True), r=["qT"] + kk, w=[bkey])
                                add("act", lambda e, bk=bk, h=h, cw=cw, at=at: e.activation(out=at[:, 0:cw], in_=pb[bk][:, 0:cw], func=AF.Relu, scale=WA[:, h:h + 1]),
                                    r=[bkey, "WA"], w=[akey])
                                if h == 0:
                                    add("dve", lambda e, at=at, c0=c0, cw=cw: e.tensor_scalar(out=score[:, c0:c0 + cw], in0=at[:, 0:cw], scalar1=WS[:, 0:1], scalar2=None, op0=ALU.mult),
                                        r=[akey, "WS"], w=["score"])
                                else:
                                    add("dve", lambda e, at=at, c0=c0, cw=cw, h=h: e.scalar_tensor_tensor(out=score[:, c0:c0 + cw], in0=at[:, 0:cw], scalar=WS[:, h:h + 1],
                                                                                                            in1=score[:, c0:c0 + cw], op0=ALU.mult, op1=ALU.add),
                                        r=[akey, "WS"], w=["score"])
                        add("dve", lambda e, tsl=tsl: e.tensor_tensor(out=score[:, tsl], in0=score[:, tsl], in1=causalf[:], op=ALU.add), r=["c_causalf"], w=["score"])
                    for g in range(2):
                        sbk = sbank[sb_i[0] % 2]
                        sb_i[0] += 1
                        skey = "pb%d" % sbk
                        q_rhs = qT[0:64, 4 * g:4 * g + 4, :]
                        add("pe", lambda e, sbk=sbk, g=g, q_rhs=q_rhs: e.matmul(pb[sbk][:, :], lhsT=KCT[:, g, :], rhs=q_rhs, start=True, stop=False),
                            r=["KCT", "qT"], w=[skey])
                        add("pe", lambda e, sbk=sbk, t=t: e.matmul(pb[sbk][:, :], lhsT=cmpbias[:, t, :], rhs=i4[:, :], start=False, stop=True),
                            r=["c_cmpbias", "c_i4"], w=[skey])
                        add("act", lambda e, sbk=sbk: e.activation(out=PTc[:], in_=pb[sbk][:, :], func=AF.Exp, scale=0.125), r=[skey], w=["PTc"])
                        dump("PTc", PTc[:], ["PTc"], t)
                        cut("A1a", t)
                        ob = obank[ob_i[0] % 2]
                        ob_i[0] += 1
                        okey = "pb%d" % ob
                        for h in range(4):
                            add("pe", lambda e, ob=ob, h=h, g=g: e.matmul(pb[ob][:, h * 128:h * 128 + 97], lhsT=PTc[:, h * 128:(h + 1) * 128], rhs=VCa[:, g, 0:97],
                                                                          start=(h == 0), stop=False, skip_group_check=True), r=["PTc", "VCa"], w=[okey])
                        pv_evac(pb[ob], okey, 97, 256 * g, 0, g, True)
                        v = pb[ob][:, 0:512].rearrange("p (h w) -> p h w", h=4)
                        if t >= 8:
                            for h in range(4):
                                if h == 0:
                                    add("dve", lambda e, v=v, g=g: e.tensor_scalar(out=imp[:, g, :], in0=v[:, 0, 65:97], scalar1=rc[:, 0:1], scalar2=None, op0=ALU.mult),
                                        r=[okey, "rc"], w=[("imp", g)])
                                else:
                                    add("dve", lambda e, v=v, g=g, h=h: e.scalar_tensor_tensor(out=imp[:, g, :], in0=v[:, h, 65:97], scalar=rc[:, h:h + 1],
                                                                                               in1=imp[:, g, :], op0=ALU.mult, op1=ALU.add),
                                        r=[okey, "rc"], w=[("imp", g)])

                    dump("Ot_A1", Ot[:, 0:512], ["Ot"], t)
                    dump("imp", imp[:].rearrange("p g j -> p (g j)"), [("imp", 0), ("imp", 1)], t)
                    cut("A1", t)
                    if t >= 8:
                        for g in range(2):
                            add("dve", lambda e, g=g, t=t: e.tensor_tensor(out=tk[:, 0:32], in0=imp[:, g, :], in1=forceb[:, t, :], op=ALU.add),
                                r=[("imp", g), "c_forceb"], w=["tk"])
                            add("dve", lambda e: e.max(out=tk[:, 32:40], in_=tk[:, 0:32]), r=["tk"], w=["tk"])
                            add("dve", lambda e: e.match_replace(out=tk[:, 48:80], in_to_replace=tk[:, 32:40], in_values=tk[:, 0:32], imm_value=-1e9),
                                r=["tk"], w=["tk"])
                            add("dve", lambda e: e.max(out=tk[:, 40:48], in_=tk[:, 48:80]), r=["tk"], w=["tk"])
                            add("dve", lambda e, g=g: e.tensor_scalar(out=selb[:, g, :], in0=tk[:, 0:32], scalar1=tk[:, 47:48], scalar2=NEGB, op0=ALU.is_lt, op1=ALU.mult),
                                r=["tk"], w=[("selb", g)])
                    cut("SEL", t)
                    if t >= 2:
                        add("dve", lambda e, nkeys=nkeys: e.tensor_reduce(out=bis[:, 0:1], in_=score[:, 0:nkeys], axis=AX.X, op=ALU.max), r=["score"], w=["bis"])
                        add("dve", lambda e, t=t: e.tensor_reduce(out=bis[:, 1:2], in_=score[:, 0:t * 128], axis=AX.X, op=ALU.min), r=["score"], w=["bis"])
                        add("dve", lambda e: e.tensor_scalar(out=bis[:, 1:2], in0=bis[:, 1:2], scalar1=-1.0, scalar2=None, op0=ALU.add), r=["bis"], w=["bis"])
                        add("dve", lambda e: e.tensor_tensor(out=bis[:, 2:3], in0=bis[:, 0:1], in1=bis[:, 1:2], op=ALU.add), r=["bis"], w=["bis"])
                        add("dve", lambda e: e.tensor_scalar(out=bis[:, 2:3], in0=bis[:, 2:3], scalar1=0.5, scalar2=None, op0=ALU.mult), r=["bis"], w=["bis"])
                        add("dve", lambda e: e.tensor_tensor(out=bis[:, 3:4], in0=bis[:, 0:1], in1=bis[:, 1:2], op=ALU.subtract), r=["bis"], w=["bis"])
                        add("dve", lambda e: e.tensor_scalar(out=wtab[:], in0=misc[:, 33:33 + NBIS + 1], scalar1=bis[:, 3:4], scalar2=0.5, op0=ALU.mult, op1=ALU.mult),
                            r=["bis", "c_misc"], w=["wtab"])
                        for it in range(NBIS):
                            add("dve", lambda e: e.memset(bis[:, 4:5], 0.0), w=["bis"])
                            add("dve", lambda e, nkeys=nkeys: e.tensor_scalar(out=junk2[:, 0:nkeys], in0=score[:, 0:nkeys], scalar1=bis[:, 2:3], scalar2=0.0,
                                                                               op0=ALU.is_gt, op1=ALU.add, accum_out=bis[:, 4:5]), r=["score", "bis"], w=["junk2", "bis"])
                            add("dve", lambda e, it=it: e.tensor_scalar(out=bis[:, 5:6], in0=bis[:, 4:5], scalar1=255.5, scalar2=wtab[:, it:it + 1],
                                                                         op0=ALU.is_gt, op1=ALU.mult), r=["bis", "wtab"], w=["bis"])
                            add("dve", lambda e, it=it: e.scalar_tensor_tensor(out=bis[:, 2:3], in0=bis[:, 5:6], scalar=2.0, in1=bis[:, 2:3], op0=ALU.mult, op1=ALU.add),
                                r=["bis"], w=["bis"])
                            add("dve", lambda e, it=it: e.tensor_tensor(out=bis[:, 2:3], in0=bis[:, 2:3], in1=wtab[:, it:it + 1], op=ALU.subtract), r=["bis", "wtab"], w=["bis"])
                        add("dve", lambda e: e.tensor_tensor(out=bis[:, 6:7], in0=bis[:, 2:3], in1=wtab[:, NBIS - 1:NBIS], op=ALU.subtract), r=["bis", "wtab"], w=["bis"])
                        add("pool", lambda e, nkeys=nkeys: e.tensor_scalar(out=maskb[:, 0:nkeys], in0=score[:, 0:nkeys], scalar1=bis[:, 6:7], scalar2=NEGB,
                                                                            op0=ALU.is_le, op1=ALU.mult), r=["score", "bis"], w=["maskb"])

                        def dmask(kt):
                            return (maskb[:, kt * 128:(kt + 1) * 128], ["maskb"])
                    else:
                        def dmask(kt, t=t):
                            return (causal[:], ["c_causal"]) if kt == t else None
                    for g in range(2):
                        if t >= 8:
                            nkeys = (t + 1) * 128
                            add("pool", lambda e, nkeys=nkeys, g=g: e.tensor_copy(out=selx[:, 0:nkeys].rearrange("p (j k) -> p j k", k=64),
                                                                              in_=bc(selb[:, g, 0:nkeys // 64].unsqueeze(2), [128, nkeys // 64, 64])),
                                r=[("selb", g)], w=["selx"])
                            add("pool", lambda e, tsl=tsl: e.tensor_tensor(out=selx[:, tsl], in0=selx[:, tsl], in1=causal[:], op=ALU.add),
                                r=["c_causal"], w=["selx"])

                            def mask_fn(kt):
                                return (selx[:, kt * 128:(kt + 1) * 128], ["selx"])
                        else:
                            def mask_fn(kt, t=t):
                                return (causal[:], ["c_causal"]) if kt == t else None
                        bank, okey = attention(
                            t, list(range(t + 1)),
                            lambda kt, g=g: (KT[0:64, 2 + g, kt * 128:(kt + 1) * 128], [("KT", kt)]),
                            qT[0:64, 4 * g:4 * g + 4, :], mask_fn,
                            lambda kt, g=g: (VA[:, kt, g, 0:65], [("VA", kt)]))
                        pv_evac(bank, okey, 65, 256 * g, 1, g, False)

                    dump("Ot_A2", Ot[:, 0:512], ["Ot"], t)
                    dump("selx", selx[:, 0:(t + 1) * 128], ["selx"], t)
                    cut("A2", t)
                    for g in range(2):
                        def mask_fn(kt, t=t):
                            if kt == t:
                                return (causal[:], ["c_causal"])
                            if kt == t - 4:
                                return (band[:], ["c_band"])
                            return None
                        bank, okey = attention(
                            t, list(range(max(0, t - 4), t + 1)),
                            lambda kt, g=g: (KT[0:64, 4 + g, kt * 128:(kt + 1) * 128], [("KT", kt)]),
                            qT[0:64, 4 * g:4 * g + 4, :], mask_fn,
                            lambda kt, g=g: (VA[:, kt, 2 + g, 0:65], [("VA", kt)]))
                        pv_evac(bank, okey, 65, 256 * g, 2, g, False)

                    dump("Ot_A3", Ot[:, 0:512], ["Ot"], t)
                    cut("A3", t)
                    for hg in range(2):
                        bank, okey = attention(
                            t, list(range(t + 1)),
                            lambda kt: (KT[64:128, 0, kt * 128:(kt + 1) * 128], [("KTd", kt)]),
                            qT[64:128, 4 * hg:4 * hg + 4, :], dmask,
                            lambda kt: (VA[:, kt, 4, 0:65], [("VA", kt)]))
                        pv_evac(bank, okey, 65, 512 + 256 * hg, None, 0, True)

                    dump("Ot_A4", Ot[:], ["Ot"], t)
                    dump("score", score[:, 0:(t + 1) * 128], ["score"], t)
                    dump("maskb", maskb[:, 0:(t + 1) * 128], ["maskb"], t)
                    dump("bis", bis[:], ["bis"], t)
                    cut("A4", t)
                    add("act", lambda e: e.activation(out=Otb[:], in_=Ot[:], func=AF.Copy), r=["Ot"], w=["Otb"])
                    for k in range(8):
                        add("pe", lambda e, k=k: e.transpose(out=pbf(0)[:, k * 128:(k + 1) * 128], in_=Otb[:, k * 128:(k + 1) * 128], identity=i4[:, 0:128]),
                            r=["Otb", "c_i4"], w=["pb0"])
                    add("dve", lambda e: e.tensor_copy(out=oT[:].rearrange("p k q -> p (k q)"), in_=pbf(0)[:, :]), r=["pb0"], w=["oT"])
                    add("dve", lambda e: e.memset(st4[:, 4:6], 0.0), w=["ssy0", "ssy1"])
                    for half in range(2):
                        bk = 1 + half
                        bkey = "pb%d" % bk
                        for k in range(8):
                            add("pe", lambda e, k=k, bk=bk, half=half: e.matmul(pb[bk][:, :], lhsT=oT[:, k, :], rhs=Wout[:, k, half * 512:(half + 1) * 512],
                                                                                start=(k == 0), stop=(k == 7)), r=["oT", "weights"], w=[bkey])
                        add("act", lambda e, bk=bk, half=half: e.activation(out=junk2[:, half * 512:(half + 1) * 512], in_=pb[bk][:, :], func=AF.Square,
                                                                              accum_out=st4[:, 4 + half:5 + half]), r=[bkey], w=["junk2", "ssy%d" % half])
                    add("dve", lambda e: e.tensor_tensor(out=st4[:, 6:7], in0=st4[:, 4:5], in1=st4[:, 5:6], op=ALU.add), r=["ssy0", "ssy1"], w=["ssy"])
                    rstd_from_ss(st4[:, 6:7], st4[:, 7:8], ["ssy"], "rstdy")
                    for half in range(2):
                        bk = 1 + half
                        bkey = "pb%d" % bk
                        hs = slice(half * 512, (half + 1) * 512)
                        add("dve", lambda e, bk=bk, hs=hs: e.scalar_tensor_tensor(out=x1t[:, hs], in0=pb[bk][:, :], scalar=st4[:, 7:8], in1=G1[:, hs],
                                                                                   op0=ALU.mult, op1=ALU.mult), r=[bkey, "rstdy", "G1"], w=["x1t"])
                    add("pool", lambda e, xb=xb: e.tensor_tensor(out=x1t[:], in0=x1t[:], in1=xb[:], op=ALU.add), r=[xkey], w=["x1t"])
                    add("sp", lambda e, b=b, tsl=tsl: e.dma_start(out=out_d[b, tsl, :], in_=x1t[:]), r=["x1t"], w=[("out", b, t)], dma=True)
                    cut("O", t)
                    if debug and b == 0:
                        for name in debug:
                            if name == "Ot%d" % t:
                                add("sp", lambda e, name=name: e.dma_start(out=dbg_d[name], in_=Ot[:]), r=["Ot"], w=["dbg_" + name], dma=True)
                            if name == "qT%d" % t:
                                add("act", lambda e: e.activation(out=score[:, 0:1792], in_=qT[:].rearrange("p s q -> p (s q)"), func=AF.Copy), r=["qT"], w=["score"])
                                add("sp", lambda e, name=name: e.dma_start(out=dbg_d[name], in_=score[:, 0:1792]), r=["score"], w=["dbg_" + name], dma=True)
            S.barrier()
        cut("mixer")

        with ExitStack() as es2:
            Fz = es2
            Wup = sbuf(Fz, "Wup", [128, 8, DFF], BF16)
            Wdn = sbuf(Fz, "Wdn", [128, 32, D], BF16)
            with ExitStack() as es_w:
                wst = [sbuf(es_w, "wstf%d" % i, [128, 8, 512], F32) for i in range(2)]
                wup_v = wup_d.rearrange("(k p) n -> p k n", p=128)
                wdn_v = wdn_d.rearrange("(k p) n -> p k n", p=128)
                ci = 0
                for c0 in range(0, DFF, 512):
                    st = wst[ci % 2]
                    key = "wstf%d" % (ci % 2)
                    add("sp", lambda e, st=st, c0=c0: e.dma_start(out=st[:], in_=wup_v[:, :, c0:c0 + 512]), w=[key], dma=True)
                    add("pool" if ci % 2 else "dve", lambda e, st=st, c0=c0: e.tensor_copy(out=Wup[:, :, c0:c0 + 512], in_=st[:]), r=[key], w=["fw"])
                    ci += 1
                for k0 in range(0, 32, 4):
                    st = wst[ci % 2]
                    key = "wstf%d" % (ci % 2)
                    stv = st[:].rearrange("p a n -> p (a n)").rearrange("p (a n) -> p a n", a=4)
                    add("sp", lambda e, stv=stv, k0=k0: e.dma_start(out=stv, in_=wdn_v[:, k0:k0 + 4, :]), w=[key], dma=True)
                    add("pool" if ci % 2 else "dve", lambda e, stv=stv, k0=k0: e.tensor_copy(out=Wdn[:, k0:k0 + 4, :], in_=stv), r=[key], w=["fw"])
                    ci += 1
                S.barrier()
            NTC = 2
            NTOK = NTC * 128
            x1c = sbuf(Fz, "x1c", [128, NTC, D], F32)
            xn2 = sbuf(Fz, "xn2", [128, D], BF16)
            stf = sbuf(Fz, "stf", [128, 8], F32)
            h2T = sbuf(Fz, "h2T", [128, 8, NTOK], BF16)
            uT = sbuf(Fz, "uT", [128, 32, NTOK], BF16)
            rl = [sbuf(Fz, "rl%d" % i, [128, NTOK], F32) for i in range(2)]
            G2 = sbuf(Fz, "G2", [128, D], F32)
            yo = sbuf(Fz, "yo", [128, D], F32)
            jk = sbuf(Fz, "jk", [128, D], BF16)

            def rstd2(ss_ap, out_ap, rkeys, wkey):
                add("dve", lambda e: e.tensor_scalar(out=out_ap, in0=ss_ap, scalar1=1.0 / D, scalar2=EPS, op0=ALU.mult, op1=ALU.add), r=rkeys, w=[wkey])
                add("act", lambda e: e.activation(out=out_ap, in_=out_ap, func=AF.Ln), r=[wkey], w=[wkey])
                add("act", lambda e: e.activation(out=out_ap, in_=out_ap, func=AF.Exp, scale=-0.5), r=[wkey], w=[wkey])

            for b in range(nseq):
                add("sp", lambda e, b=b: e.dma_start(out=G2[:], in_=bc(gscr_d[b:b + 1, 1, :], [128, D])), r=["gscr"], w=["G2"], dma=True)
                for tc in range(NT // NTC):
                    for i in range(NTC):
                        t = tc * NTC + i
                        tsl = slice(t * 128, (t + 1) * 128)
                        add("sp", lambda e, b=b, tsl=tsl, i=i: e.dma_start(out=x1c[:, i, :], in_=out_d[b, tsl, :]), r=[("out", b, t)], w=[("x1c", i)], dma=True)
                        add("dve", lambda e: e.memset(stf[:, 0:1], 0.0), w=["fss"])
                        add("act", lambda e, i=i: e.activation(out=xn2[:], in_=x1c[:, i, :], func=AF.Square, accum_out=stf[:, 0:1]), r=[("x1c", i)], w=["xn2", "fss"])
                        rstd2(stf[:, 0:1], stf[:, 1:2], ["fss"], "frstd")
                        add("act", lambda e, i=i: e.activation(out=xn2[:], in_=x1c[:, i, :], func=AF.Copy, scale=stf[:, 1:2]), r=[("x1c", i), "frstd"], w=["xn2"])
                        for k in range(8):
                            add("pe", lambda e, k=k: e.transpose(out=pbf(0)[:, k * 128:(k + 1) * 128], in_=xn2[:, k * 128:(k + 1) * 128], identity=i4[:, 0:128]),
                                r=["xn2", "c_i4"], w=["pb0"])
                        for k in range(8):
                            add("dve", lambda e, k=k, b=b, i=i: e.tensor_scalar(out=h2T[:, k, i * 128:(i + 1) * 128], in0=pbf(0)[:, k * 128:(k + 1) * 128],
                                                                                 scalar1=AB[:, 2, b, k:k + 1], scalar2=AB[:, 3, b, k:k + 1], op0=ALU.mult, op1=ALU.add),
                                r=["pb0", "AB"], w=["h2T"])
                    for f in range(32):
                        bk = 1 + (f % 2)
                        bkey = "pb%d" % bk
                        r_ = rl[f % 2]
                        rkey = "rl%d" % (f % 2)
                        for k in range(8):
                            add("pe", lambda e, k=k, f=f, bk=bk: e.matmul(pb[bk][:, 0:NTOK], lhsT=Wup[:, k, f * 128:(f + 1) * 128], rhs=h2T[:, k, :],
                                                                          start=(k == 0), stop=(k == 7)), r=["h2T", "fw"], w=[bkey])
                        add("act", lambda e, bk=bk, r_=r_: e.activation(out=r_[:], in_=pb[bk][:, 0:NTOK], func=AF.Relu), r=[bkey], w=[rkey])
                        add("pool" if f % 2 else "dve", lambda e, f=f, r_=r_: e.tensor_tensor(out=uT[:, f, :], in0=r_[:], in1=r_[:], op=ALU.mult), r=[rkey], w=[("uT", f)])
                    for i in range(NTC):
                        t = tc * NTC + i
                        tsl = slice(t * 128, (t + 1) * 128)
                        add("dve", lambda e: e.memset(stf[:, 4:6], 0.0), w=["fssy0", "fssy1"])
                        for half in range(2):
                            bk = 3 + half
                            bkey = "pb%d" % bk
                            for f in range(32):
                                add("pe", lambda e, f=f, bk=bk, half=half, i=i: e.matmul(pb[bk][:, :], lhsT=uT[:, f, i * 128:(i + 1) * 128],
                                                                                        rhs=Wdn[:, f, half * 512:(half + 1) * 512], start=(f == 0), stop=(f == 31)),
                                    r=[("uT", f), "fw"], w=[bkey])
                            add("act", lambda e, bk=bk, half=half: e.activation(out=jk[:, half * 512:(half + 1) * 512], in_=pb[bk][:, :], func=AF.Square,
                                                                                  accum_out=stf[:, 4 + half:5 + half]), r=[bkey], w=["jk%d" % half, "fssy%d" % half])
                        add("dve", lambda e: e.tensor_tensor(out=stf[:, 6:7], in0=stf[:, 4:5], in1=stf[:, 5:6], op=ALU.add), r=["fssy0", "fssy1"], w=["fssy"])
                        rstd2(stf[:, 6:7], stf[:, 7:8], ["fssy"], "frstdy")
                        for half in range(2):
                            bk = 3 + half
                            bkey = "pb%d" % bk
                            hs = slice(half * 512, (half + 1) * 512)
                            add("dve", lambda e, bk=bk, hs=hs: e.scalar_tensor_tensor(out=yo[:, hs], in0=pb[bk][:, :], scalar=stf[:, 7:8], in1=G2[:, hs],
                                                                                       op0=ALU.mult, op1=ALU.mult), r=[bkey, "frstdy", "G2"], w=["yo"])
                        add("pool", lambda e, i=i: e.tensor_tensor(out=yo[:], in0=yo[:], in1=x1c[:, i, :], op=ALU.add), r=[("x1c", i)], w=["yo"])
                        add("sp", lambda e, b=b, tsl=tsl: e.dma_start(out=out_d[b, tsl, :], in_=yo[:]), r=["yo"], w=[("out", b, t)], dma=True)
            S.barrier()

    _build_body()
    S.stopped = False
    S.barrier()
    S.emit()
    top.close()
    return nc, S


def make_in_maps(inputs, cores=range(NCORES)):
    f32 = np.float32
    x = np.asarray(inputs["x"], f32)
    c = np.asarray(inputs["c"], f32)
    pos = np.asarray(inputs["positions"], np.int32)
    perm = _win_perm()
    w_in = np.ascontiguousarray(np.asarray(inputs["w_in"], f32)[0][:, perm])
    gcol = np.stack([np.asarray(inputs["g_pre_mix"], f32)[0].reshape(8, 128).T,
                     np.asarray(inputs["g_pre_ffn"], f32)[0].reshape(8, 128).T], axis=1)
    grow = np.stack([np.asarray(inputs["g_post_mix"], f32)[0], np.asarray(inputs["g_post_ffn"], f32)[0]], axis=0)[None]
    pek = np.asarray(inputs["cmp_pe_k"], f32)[0].T
    pev = np.asarray(inputs["cmp_pe_v"], f32)[0].T
    peT = np.zeros((128, 2, 32), f32)
    peT[0:64, 0] = pek
    peT[64:128, 0] = pek
    peT[0:64, 1] = pev
    peT[64:128, 1] = pev
    w1k = np.ascontiguousarray(np.asarray(inputs["cmp_w1_k"], f32)[0].reshape(32, 64, 128).transpose(1, 0, 2))
    w1v_ = np.asarray(inputs["cmp_w1_v"], f32)[0].reshape(32, 64, 128).transpose(1, 0, 2)
    w1v = np.ascontiguousarray(np.concatenate([w1v_, w1v_], axis=0))
    w2 = np.ascontiguousarray(np.stack([np.asarray(inputs["cmp_w2_k"], f32)[0], np.asarray(inputs["cmp_w2_v"], f32)[0]], axis=1))
    shared = dict(
        w_ada=np.ascontiguousarray(np.asarray(inputs["w_ada"], f32)[0]),
        b_ada=np.ascontiguousarray(np.asarray(inputs["b_ada"], f32)),
        b_adac=np.ascontiguousarray(np.asarray(inputs["b_ada"], f32)[0].reshape(48, 128).T),
        gcol=np.ascontiguousarray(gcol), grow=np.ascontiguousarray(grow), w_in=w_in, peT=peT, w1k=w1k, w1v=w1v, w2=w2,
        w_out=np.ascontiguousarray(np.asarray(inputs["w_out"], f32)[0]),
        w_up=np.ascontiguousarray(np.asarray(inputs["w_up"], f32)[0]),
        w_down=np.ascontiguousarray(np.asarray(inputs["w_down"], f32)[0]),
    )
    shared.update(_consts())
    maps = []
    for core in cores:
        b0 = core * SEQ_PER_CORE
        m = dict(shared)
        m["x"] = np.ascontiguousarray(x[b0:b0 + SEQ_PER_CORE])
        m["cT"] = np.ascontiguousarray(c[b0:b0 + SEQ_PER_CORE].T.reshape(8, 128, 4).transpose(1, 0, 2))
        m["posT"] = np.ascontiguousarray(pos[b0:b0 + SEQ_PER_CORE].reshape(4, NT, 128).transpose(2, 0, 1))
        maps.append(m)
    return maps


_PROG = {}


def kernel(**inputs):
    if "nc" not in _PROG:
        _PROG["nc"], _ = build_program()
    nc = _PROG["nc"]
    maps = make_in_maps(inputs)
    res = run_bass_kernel_spmd(nc, maps, core_ids=list(range(NCORES)))
    out = np.concatenate([np.asarray(r["out"], np.float32) for r in res.results], axis=0)
    return out
```

```python
import math
from contextlib import ExitStack

import numpy as np
import ml_dtypes

import concourse.bass as bass
import concourse.mybir as mybir
from concourse.bass_utils import run_bass_kernel_spmd

F32 = mybir.dt.float32
BF16 = mybir.dt.bfloat16
I32 = mybir.dt.int32
AF = mybir.ActivationFunctionType
ALU = mybir.AluOpType
AX = mybir.AxisListType

NCORES = 8
SEQ_PER_CORE = 4
T = 2048
D = 1024
NT = T // 128
DFF = 4096
D_IN = 2268
NEGB = -240000.0
EPS = 1e-6
IDX_SCALE = (4 ** -0.5) * (64 ** -0.5)
GELU_C = math.sqrt(2.0 / math.pi)
NBIS = 16


class Sched:
    COMPUTE = ("pe", "act", "dve", "pool")

    def __init__(self, nc, n_dma_sems=12):
        self.nc = nc
        self.ops = []
        self.lastw = {}
        self.readers = {}
        self.n_dma_sems = n_dma_sems
        self.dma_rr = {}
        self.dma_last = {}

    def add(self, eng, fn, r=(), w=(), dma=False, extra_deps=()):
        if getattr(self, "stopped", False):
            return -1
        idx = len(self.ops)
        deps = set(extra_deps)
        if eng != "pe":
            w = list(w) + [x for x in r if isinstance(x, str) and x.startswith("pb") and x not in w]
        pb_ = getattr(self, "pending_bar", None)
        if pb_ and eng in pb_:
            deps.add(pb_.pop(eng))
        for x in r:
            if x in self.lastw:
                deps.add(self.lastw[x])
        for x in w:
            if x in self.lastw:
                deps.add(self.lastw[x])
            for y in self.readers.get(x, ()):
                deps.add(y)
        for x in w:
            self.lastw[x] = idx
            self.readers[x] = []
        for x in r:
            if x not in w:
                self.readers.setdefault(x, []).append(idx)
        op = dict(eng=eng, fn=fn, deps=deps, dma=dma)
        if dma:
            k = self.dma_rr.get(eng, 0)
            self.dma_rr[eng] = k + 1
            slot = (eng, k % self.n_dma_sems)
            prev = self.dma_last.get(slot)
            if prev is not None:
                deps.add(prev)
            self.dma_last[slot] = idx
            op["slot"] = slot
        deps.discard(idx)
        self.ops.append(op)
        return idx

    def barrier(self):
        if getattr(self, "stopped", False):
            return
        live = set(self.lastw.values())
        for v in self.readers.values():
            live.update(v)
        for v in self.dma_last.values():
            live.add(v)
        b = self.add("sp", self.bar_fn, dma=True, extra_deps=live)
        self.lastw = {}
        self.readers = {}
        self.pending_bar = {e: b for e in self.COMPUTE}

    def emit(self):
        nc = self.nc
        ops = self.ops
        n = len(ops)
        es = ExitStack()
        sems = {}
        for e in self.COMPUTE + ("sp",):
            sems[e] = es.enter_context(nc.semaphore("s_" + e))
        dma_slots = sorted({op["slot"] for op in ops if op["dma"]})
        for s in dma_slots:
            sems[s] = es.enter_context(nc.semaphore("d_%s_%d" % s))

        def pe_pe(a, b):
            return a["eng"] == "pe" and b["eng"] == "pe" and not a["dma"] and not b["dma"]

        signaled = [False] * n
        for op in ops:
            for d in op["deps"]:
                if pe_pe(ops[d], op):
                    continue
                signaled[d] = True
        cnt = {}
        sig = [None] * n
        for i, op in enumerate(ops):
            if op["dma"]:
                key = op["slot"]
                cnt[key] = cnt.get(key, 0) + 16
                sig[i] = (key, cnt[key], 16)
            elif signaled[i]:
                key = op["eng"]
                cnt[key] = cnt.get(key, 0) + 1
                sig[i] = (key, cnt[key], 1)
        know = {}
        vc = [None] * n
        waits = [None] * n
        nw = 0
        for i, op in enumerate(ops):
            E = op["eng"]
            K = know.setdefault(E, {})
            best = {}
            for d in sorted(op["deps"], reverse=True):
                if pe_pe(ops[d], op):
                    continue
                key, val, _ = sig[d]
                if K.get(key, 0) >= val:
                    continue
                if best.get(key, 0) < val:
                    best[key] = val
                for k2, v2 in vc[d].items():
                    if K.get(k2, 0) < v2:
                        K[k2] = v2
            waits[i] = list(best.items())
            nw += len(waits[i])
            v = dict(K)
            if sig[i] is not None:
                key, val, _ = sig[i]
                v[key] = val
            vc[i] = v
        self.stats = dict(n_ops=n, n_waits=nw, n_sig=sum(1 for s in sig if s))
        per = {}
        for i, op in enumerate(ops):
            per.setdefault(op["eng"], []).append(i)
        self.stats["per_engine"] = {k: len(v) for k, v in per.items()}

        def run(engobj, name):
            for i in per.get(name, ()):
                op = ops[i]
                for key, val in waits[i]:
                    engobj.wait_ge(sems[key], val)
                ins = op["fn"](engobj)
                if sig[i] is not None:
                    key, val, inc = sig[i]
                    ins.then_inc(sems[key], inc)

        with nc.Block() as block:
            @block.sync
            def _(e):
                run(e, "sp")

            @block.tensor
            def _(e):
                run(e, "pe")

            @block.scalar
            def _(e):
                run(e, "act")

            @block.vector
            def _(e):
                run(e, "dve")

            @block.gpsimd
            def _(e):
                run(e, "pool")
        es.close()


def _win_perm():
    off = dict(q_n=0, kc=512, vc=640, ksl=768, vsl=896, kw=1024, vw=1152, gl=1280,
               q_d=1304, k_d=1816, v_d=1880, qi=1944, ki=2200, wi=2264)

    def head(name, i):
        return list(range(off[name] + 64 * i, off[name] + 64 * (i + 1)))

    nsa = [("q_n", i) for i in range(8)] + [("kc", 0), ("kc", 1), ("ksl", 0), ("ksl", 1), ("kw", 0), ("kw", 1)]
    dsa = [("q_d", i) for i in range(8)] + [("k_d", 0), ("ki", 0)] + [("qi", i) for i in range(4)]
    cols = []
    for p in range(14):
        cols += head(*nsa[p]) + head(*dsa[p])
    cols += head("vc", 0) + head("vc", 1) + head("vsl", 0) + head("vsl", 1) + head("vw", 0) + head("vw", 1)
    cols += head("v_d", 0)
    cols += list(range(off["gl"], off["gl"] + 24)) + list(range(off["wi"], off["wi"] + 4))
    assert len(cols) == D_IN and len(set(cols)) == D_IN
    return np.array(cols)


def _consts():
    bf = ml_dtypes.bfloat16
    c = {}
    eye = np.eye(128, dtype=np.float32)
    c["c_i4"] = np.tile(eye, (1, 4)).astype(bf)
    q = np.arange(128)[:, None]
    k = np.arange(128)[None, :]
    c["c_causal"] = np.where(k <= q, 0.0, NEGB).astype(bf)
    c["c_band"] = np.where(k > q, 0.0, NEGB).astype(bf)
    c["c_causalf"] = np.where(k <= q, 0.0, -1e30).astype(np.float32)
    t = (np.arange(NT)[None, :, None] * 128 + np.arange(128)[:, None, None])
    n = np.arange(128)[None, None, :]
    c["c_cmpbias"] = np.where((16 * n + 31 <= t) & (n < 127), 0.0, NEGB).astype(bf)
    j = np.arange(32)[None, None, :]
    cur = t // 64
    forced = (j == 0) | (j == cur) | (j == cur - 1)
    fb = np.where(j > cur, -100.0, np.where(forced, 100.0, 0.0))
    c["c_forceb"] = fb.astype(np.float32)
    cs = np.arange(128) * 16
    ce = cs + 31
    ss_ = np.arange(32) * 64
    se = ss_ + 63
    ov = ((cs[:, None] <= se[None, :]) & (ce[:, None] >= ss_[None, :]) & (np.arange(128)[:, None] < 127))
    vca = np.zeros((128, 2, 112), np.float32)
    vca[:, :, 64] = 1.0
    vca[:, :, 65:97] = ov[:, None, :]
    c["c_vca"] = vca.astype(bf)
    inv = (np.float32(10000.0) ** (-np.arange(32, dtype=np.float32) / np.float32(32))).astype(np.float32)
    misc = np.zeros((128, 64), np.float32)
    misc[:, 0:32] = inv[None, :]
    misc[:, 32] = -math.pi
    misc[:, 33:33 + NBIS + 1] = (2.0 ** -(np.arange(NBIS + 1) + 1.0))[None, :]
    misc[:, 50] = 1e-30
    c["c_misc"] = misc
    return c


class _Stop(Exception):
    pass


def build_program(nseq=SEQ_PER_CORE, debug=None, stop=None, stop_tile=0, dumps=()):
    nc = bass.Bass("TRN2", target_bir_lowering=False)
    S = Sched(nc)
    add = S.add

    def cut(name, tile=None):
        if stop == name and (tile is None or tile == stop_tile):
            S.stopped = True

    def dump(name, ap, keys, tile=None):
        if name in dumps and (tile is None or tile == stop_tile):
            shp = list(ap.shape)
            d_ = nc.dram_tensor("dbg_" + name, shp, ap.dtype, kind="ExternalOutput").ap()
            add("sp", lambda e: e.dma_start(out=d_, in_=ap), r=list(keys), w=["dbg_" + name], dma=True)

    def din(name, shape, dt=F32):
        return nc.dram_tensor(name, list(shape), dt, kind="ExternalInput").ap()

    x_d = din("x", [SEQ_PER_CORE, T, D])
    cT_d = din("cT", [128, 8, 4])
    pos_d = din("posT", [128, 4, NT], I32)
    wada_d = din("w_ada", [D, 6 * D])
    bada_d = din("b_ada", [1, 6 * D])
    badac_d = din("b_adac", [128, 48])
    gcol_d = din("gcol", [128, 2, 8])
    grow_d = din("grow", [1, 2, D])
    win_d = din("w_in", [D, D_IN])
    pe_d = din("peT", [128, 2, 32])
    w1k_d = din("w1k", [64, 32, 128])
    w1v_d = din("w1v", [128, 32, 128])
    w2_d = din("w2", [128, 2, 64])
    wout_d = din("w_out", [D, D])
    wup_d = din("w_up", [D, DFF])
    wdn_d = din("w_down", [DFF, D])
    consts = _consts()
    cd = {k: din(k, v.shape, BF16 if v.dtype == ml_dtypes.bfloat16 else F32) for k, v in consts.items()}
    out_d = nc.dram_tensor("out", [SEQ_PER_CORE, T, D], F32, kind="ExternalOutput").ap()
    S.bar_fn = lambda e: e.dma_start(out=bar_d[1:2, :], in_=cd["c_misc"][0:1, :])
    gscr_d = nc.dram_tensor("gscr", [4, 2, D], F32, kind="Internal").ap()
    bar_d = nc.dram_tensor("bar_scr", [2, 64], F32, kind="Internal").ap()
    dbg_d = {}
    if debug:
        for name, shape in debug.items():
            dbg_d[name] = nc.dram_tensor("dbg_" + name, list(shape), F32, kind="ExternalOutput").ap()

    top = ExitStack()

    def sbuf(es, name, shape, dt):
        return es.enter_context(nc.sbuf_tensor("s_" + name, list(shape), dt))

    pb = [top.enter_context(nc.psum_tensor("pb%d" % i, [128, 512], F32)) for i in range(8)]

    def pbf(i):
        return pb[i][:].bitcast(BF16)

    def bc(ap, shape):
        return ap.to_broadcast(list(shape))

    P = top
    i4 = sbuf(P, "i4", [128, 512], BF16)
    causal = sbuf(P, "causal", [128, 128], BF16)
    band = sbuf(P, "band", [128, 128], BF16)
    causalf = sbuf(P, "causalf", [128, 128], F32)
    misc = sbuf(P, "misc", [128, 64], F32)
    AB = sbuf(P, "AB", [128, 4, 4, 8], F32)
    for name, t_ in (("c_i4", i4), ("c_causal", causal), ("c_band", band), ("c_causalf", causalf),
                     ("c_misc", misc)):
        add("sp", lambda e, t_=t_, name=name: e.dma_start(out=t_[:], in_=cd[name]), w=[name], dma=True)
    CONST_R = ["c_i4", "c_causal", "c_band", "c_causalf", "c_misc"]
    invf = misc[:, 0:32]
    negpi = misc[:, 32:33]

    def _build_body():
        with ExitStack() as es0:
            cT = sbuf(es0, "cT", [128, 8, 4], F32)
            badar = sbuf(es0, "badar", [4, 6 * D], F32)
            badac = sbuf(es0, "badac", [128, 48], F32)
            gcol = sbuf(es0, "gcol", [128, 2, 8], F32)
            growb = sbuf(es0, "growb", [4, 2, D], F32)
            modT = sbuf(es0, "modT", [128, 4, 8, 4], F32)
            wst = [sbuf(es0, "wst%d" % i, [128, 8, 512], F32) for i in range(2)]
            add("sp", lambda e: e.dma_start(out=cT[:], in_=cT_d), w=["cT"], dma=True)
            add("sp", lambda e: e.dma_start(out=badar[:], in_=bc(bada_d, [4, 6 * D])), w=["badar"], dma=True)
            add("sp", lambda e: e.dma_start(out=badac[:], in_=badac_d), w=["badac"], dma=True)
            add("sp", lambda e: e.dma_start(out=gcol[:], in_=gcol_d), w=["gcol"], dma=True)
            add("sp", lambda e: e.dma_start(out=growb[:], in_=bc(grow_d, [4, 2, D])), w=["growb"], dma=True)
            wada_v = wada_d.rearrange("(k p) n -> p k n", p=128)
            colmap = {0: 0, 1: 0, 2: 1, 3: 1, 6: 2, 7: 2, 8: 3, 9: 3}
            rowmap = {4: (0, 0), 5: (0, 1), 10: (1, 0), 11: (1, 1)}
            for cc in range(12):
                st = wst[cc % 2]
                key = "wst%d" % (cc % 2)
                add("sp", lambda e, st=st, cc=cc: e.dma_start(out=st[:], in_=wada_v[:, :, cc * 512:(cc + 1) * 512]),
                    w=[key], dma=True)
                if cc in colmap:
                    for q in range(4):
                        jj = colmap[cc] * 8 + (cc % 2) * 4 + q
                        for k in range(8):
                            add("pe", lambda e, st=st, k=k, q=q, jj=jj: e.matmul(pb[2][:, jj * 4:(jj + 1) * 4], lhsT=st[:, k, q * 128:(q + 1) * 128],
                                                                                 rhs=cT[:, k, :], start=(k == 0), stop=(k == 7)),
                                r=["cT", key], w=["pb2"])
                else:
                    gi, half = rowmap[cc]
                    for k in range(8):
                        add("pe", lambda e, st=st, k=k: e.matmul(pb[0][0:4, :], lhsT=cT[:, k, :], rhs=st[:, k, :],
                                                                 start=(k == 0), stop=(k == 7)),
                            r=["cT", key], w=["pb0"])
                    hs_ = slice(half * 512, (half + 1) * 512)
                    add("dve", lambda e, cc=cc: e.tensor_tensor(out=badar[:, cc * 512:(cc + 1) * 512], in0=pb[0][0:4, :],
                                                                 in1=badar[:, cc * 512:(cc + 1) * 512], op=ALU.add),
                        r=["pb0", "badar"], w=["badar"])
                    add("dve", lambda e, cc=cc, gi=gi, hs_=hs_: e.tensor_tensor(out=growb[:, gi, hs_], in0=growb[:, gi, hs_],
                                                                                  in1=badar[:, cc * 512:(cc + 1) * 512], op=ALU.mult),
                        r=["badar", "growb"], w=["growb"])
            for a_, ch in enumerate((0, 1, 3, 4)):
                add("dve", lambda e, a_=a_, ch=ch: e.tensor_tensor(out=modT[:, a_, :, :], in0=pb[2][:, a_ * 32:(a_ + 1) * 32].rearrange("p (k b) -> p k b", b=4),
                                                                    in1=bc(badac[:, ch * 8:(ch + 1) * 8].unsqueeze(2), [128, 8, 4]), op=ALU.add),
                    r=["pb2", "badac"], w=["modT"])
            for b in range(4):
                for (dst, src, gi) in ((0, 1, 0), (2, 3, 1)):
                    add("dve", lambda e, b=b, dst=dst, src=src: e.tensor_scalar(out=AB[:, dst, b, :], in0=modT[:, src, :, b],
                                                                                 scalar1=1.0, scalar2=None, op0=ALU.add),
                        r=["modT"], w=["AB"])
                    add("dve", lambda e, b=b, dst=dst, gi=gi: e.tensor_tensor(out=AB[:, dst, b, :], in0=AB[:, dst, b, :],
                                                                               in1=gcol[:, gi, :], op=ALU.mult),
                        r=["gcol"], w=["AB"])
                for (dst, src) in ((1, 0), (3, 2)):
                    add("dve", lambda e, b=b, dst=dst, src=src: e.tensor_copy(out=AB[:, dst, b, :], in_=modT[:, src, :, b]),
                        r=["modT"], w=["AB"])
            add("sp", lambda e: e.dma_start(out=gscr_d, in_=growb[:]), r=["growb"], w=["gscr"], dma=True)
            dump("growb", growb[:].rearrange("p a d -> p (a d)"), ["growb"])
            dump("AB", AB[:].rearrange("p a b k -> p (a b k)"), ["AB"])
            S.barrier()
        cut("setup")

        with ExitStack() as es1:
            M = es1
            Win = sbuf(M, "Win", [128, 8, D_IN], BF16)
            Wout = sbuf(M, "Wout", [128, 8, D], BF16)
            W1k = sbuf(M, "W1k", [64, 32, 128], BF16)
            W1v = sbuf(M, "W1v", [128, 32, 128], BF16)
            W2 = sbuf(M, "W2", [128, 2, 64], BF16)
            peT = sbuf(M, "peT", [128, 2, 32], BF16)
            bT = sbuf(M, "bT", [128, 2], F32)
            cmpbias = sbuf(M, "cmpbias", [128, NT, 128], BF16)
            forceb = sbuf(M, "forceb", [128, NT, 32], F32)
            VCa = sbuf(M, "VCa", [128, 2, 112], BF16)
            add("sp", lambda e: e.dma_start(out=cmpbias[:], in_=cd["c_cmpbias"]), w=["c_cmpbias"], dma=True)
            add("sp", lambda e: e.dma_start(out=forceb[:], in_=cd["c_forceb"]), w=["c_forceb"], dma=True)
            add("sp", lambda e: e.dma_start(out=VCa[:], in_=cd["c_vca"]), w=["VCa"], dma=True)

            with ExitStack() as es_w:
                wst = [sbuf(es_w, "wstm%d" % i, [128, 8, 512], F32) for i in range(2)]
                cnt = [0]

                def load_cast(dst_fn, src_ap, shape, eng="pool"):
                    i = cnt[0] % 2
                    cnt[0] += 1
                    st = wst[i]
                    key = "wstm%d" % i
                    a, n_ = shape[1], shape[2]
                    view = st[0:shape[0], :, :].rearrange("p a n -> p (a n)")[:, 0:a * n_].rearrange("p (a n) -> p a n", a=a)
                    add("sp", lambda e: e.dma_start(out=view, in_=src_ap), w=[key], dma=True)
                    add(eng, lambda e: e.tensor_copy(out=dst_fn, in_=view), r=[key], w=["weights"])

                win_v = win_d.rearrange("(k p) n -> p k n", p=128)
                c0 = 0
                while c0 < D_IN:
                    cw = min(512, D_IN - c0)
                    load_cast(Win[:, :, c0:c0 + cw], win_v[:, :, c0:c0 + cw], [128, 8, cw], eng="pool" if (c0 // 512) % 2 else "dve")
                    c0 += cw
                wout_v = wout_d.rearrange("(k p) n -> p k n", p=128)
                for c0 in range(0, D, 512):
                    load_cast(Wout[:, :, c0:c0 + 512], wout_v[:, :, c0:c0 + 512], [128, 8, 512])
                for l0 in range(0, 32, 16):
                    load_cast(W1k[:, l0:l0 + 16, :], w1k_d[:, l0:l0 + 16, :], [64, 16, 128])
                    load_cast(W1v[:, l0:l0 + 16, :], w1v_d[:, l0:l0 + 16, :], [128, 16, 128])
                load_cast(W2[:], w2_d, [128, 2, 64])
                load_cast(peT[:], pe_d, [128, 2, 32])
                for l in range(32):
                    add("pe", lambda e, l=l: e.matmul(pb[0][:, 0:1], lhsT=W1k[0:64, l, :], rhs=peT[0:64, 0, l:l + 1],
                                                      start=(l == 0), stop=(l == 31)), r=["weights"], w=["pb0"])
                for l in range(32):
                    add("pe", lambda e, l=l: e.matmul(pb[1][:, 0:1], lhsT=W1v[0:64, l, :], rhs=peT[0:64, 1, l:l + 1],
                                                      start=(l == 0), stop=(l == 31)), r=["weights"], w=["pb1"])
                add("dve", lambda e: e.tensor_copy(out=bT[:, 0:1], in_=pb[0][:, 0:1]), r=["pb0"], w=["bT"])
                add("dve", lambda e: e.tensor_copy(out=bT[:, 1:2], in_=pb[1][:, 0:1]), r=["pb1"], w=["bT"])
                dump("bT", bT[:], ["bT"])
                S.barrier()
            cut("weights")

            KT = sbuf(M, "KT", [128, 6, T], BF16)
            VA = sbuf(M, "VA", [128, NT, 5, 80], BF16)
            VCT = sbuf(M, "VCT", [128, T], BF16)
            GH = sbuf(M, "GH", [128, 2, 2, 128], BF16)
            KCT = sbuf(M, "KCT", [64, 2, 128], BF16)
            cosT = sbuf(M, "cosT", [128, NT, 32], F32)
            sinT = sbuf(M, "sinT", [128, NT, 32], F32)
            posf = sbuf(M, "posf", [128, NT], F32)
            posi = sbuf(M, "posi", [128, 4, NT], I32)
            G1 = sbuf(M, "G1", [128, D], F32)
            xt = [sbuf(M, "xt%d" % i, [128, D], F32) for i in range(2)]
            xn = sbuf(M, "xn", [128, D], BF16)
            st4 = sbuf(M, "st4", [128, 8], F32)
            hT = sbuf(M, "hT", [128, 8, 128], BF16)
            rq = sbuf(M, "rq", [128, 28, 2, 32], BF16)
            rtmp = sbuf(M, "rtmp", [128, 4, 8, 32], F32)
            vct = sbuf(M, "vct", [128, 128], BF16)
            qT = sbuf(M, "qT", [128, 14, 128], BF16)
            gate = sbuf(M, "gate", [128, 8, 3], F32)
            WA = sbuf(M, "WA", [128, 4], F32)
            WS = sbuf(M, "WS", [128, 4], F32)
            hb = sbuf(M, "hb", [128, 4, 8], F32)
            Ygk = sbuf(M, "Ygk", [64, 2, 32, 8], BF16)
            Ygv = sbuf(M, "Ygv", [128, 32, 8], BF16)
            hu = sbuf(M, "hu", [128, 4, 8], F32)
            PT = sbuf(M, "PT", [128, 8, 512], BF16)
            PTc = sbuf(M, "PTc", [128, 512], BF16)
            rc = sbuf(M, "rc", [128, 8], F32)
            imp = sbuf(M, "imp", [128, 2, 32], F32)
            tk = sbuf(M, "tk", [128, 80], F32)
            selb = sbuf(M, "selb", [128, 2, 32], BF16)
            selx = sbuf(M, "selx", [128, T], BF16)
            score = sbuf(M, "score", [128, T], F32)
            junk2 = sbuf(M, "junk2", [128, T], BF16)
            atmp = [sbuf(M, "atmp%d" % i, [128, 512], F32) for i in range(2)]
            maskb = sbuf(M, "maskb", [128, T], BF16)
            bis = sbuf(M, "bis", [128, 32], F32)
            wtab = sbuf(M, "wtab", [128, NBIS + 1], F32)
            Ot = sbuf(M, "Ot", [128, D], F32)
            otmp = sbuf(M, "otmp", [128, 256], F32)
            accs = [sbuf(M, "accs%d" % i, [128, 4, 97], F32) for i in range(2)]
            Otb = sbuf(M, "Otb", [128, D], BF16)
            oT = sbuf(M, "oT", [128, 8, 128], BF16)
            x1t = sbuf(M, "x1t", [128, D], F32)

            add("sp", lambda e: e.dma_start(out=posi[:], in_=pos_d), w=["posi"], dma=True)
            add("pool", lambda e: e.memset(VA[:, :, :, 64:65], 1.0), w=[("VA", t_) for t_ in range(NT)])
            add("pool", lambda e: e.memset(qT[:], 0.0), w=["qT"])
            add("pool", lambda e: e.memset(KT[:], 0.0), w=[("KT", t_) for t_ in range(NT)] + [("KTd", t_) for t_ in range(NT)])

            def rstd_from_ss(ss_ap, out_ap, rkeys, wkey):
                add("dve", lambda e: e.tensor_scalar(out=out_ap, in0=ss_ap, scalar1=1.0 / D, scalar2=EPS,
                                                     op0=ALU.mult, op1=ALU.add), r=rkeys, w=[wkey])
                add("act", lambda e: e.activation(out=out_ap, in_=out_ap, func=AF.Ln), r=[wkey], w=[wkey])
                add("act", lambda e: e.activation(out=out_ap, in_=out_ap, func=AF.Exp, scale=-0.5), r=[wkey], w=[wkey])

            ev_i = [0]

            def pv_evac(bank, bkey, width, dst_cols, gate_br, g, first):
                ai = ev_i[0] % 2
                ev_i[0] += 1
                asb = accs[ai]
                akey = "accs%d" % ai
                v = bank[:, 0:512].rearrange("p (h w) -> p h w", h=4)
                add("act", lambda e: e.activation(out=asb[:, :, 0:width], in_=v[:, :, 0:width], func=AF.Copy), r=[bkey], w=[akey])
                add("act", lambda e: e.activation(out=rc[:, 0:4], in_=asb[:, :, 64], func=AF.Ln, bias=misc[:, 50:51], scale=1.0),
                    r=[akey, "c_misc"], w=["rc"])
                add("act", lambda e: e.activation(out=rc[:, 0:4], in_=rc[:, 0:4], func=AF.Exp, scale=-1.0), r=["rc"], w=["rc"])
                if gate_br is not None:
                    add("pool", lambda e: e.tensor_tensor(out=rc[:, 4:8], in0=rc[:, 0:4], in1=gate[:, 4 * g:4 * g + 4, gate_br],
                                                          op=ALU.mult), r=["rc", "gate"], w=["rcg"])
                    rcs, rkey = rc[:, 4:8], "rcg"
                else:
                    rcs, rkey = rc[:, 0:4], "rc"
                dst = Ot[:, dst_cols:dst_cols + 256].rearrange("p (h w) -> p h w", h=4)
                if first:
                    add("pool", lambda e: e.tensor_tensor(out=dst, in0=asb[:, :, 0:64], in1=bc(rcs.unsqueeze(2), [128, 4, 64]), op=ALU.mult),
                        r=[akey, rkey], w=["Ot"])
                else:
                    o3 = otmp[:].rearrange("p (h w) -> p h w", h=4)
                    add("pool", lambda e: e.tensor_tensor(out=o3, in0=asb[:, :, 0:64], in1=bc(rcs.unsqueeze(2), [128, 4, 64]), op=ALU.mult),
                        r=[akey, rkey], w=["otmp"])
                    add("pool", lambda e: e.tensor_tensor(out=dst, in0=dst, in1=o3, op=ALU.add), r=["otmp"], w=["Ot"])
                return asb, akey

            pt_i = [0]
            sbank = [4, 5]
            sb_i = [0]
            obank = [6, 7]
            ob_i = [0]

            def attention(t, kts, kslice_fn, q_rhs, mask_fn, v_fn, width=65):
                ob = obank[ob_i[0] % 2]
                ob_i[0] += 1
                okey = "pb%d" % ob
                first_pv = [True]
                for b0 in range(0, len(kts), 4):
                    blk = kts[b0:b0 + 4]
                    pt0 = (pt_i[0] % 2) * 4
                    pt_i[0] += 1
                    for j0, kt in enumerate(blk):
                        j = pt0 + j0
                        sbk = sbank[sb_i[0] % 2]
                        sb_i[0] += 1
                        skey = "pb%d" % sbk
                        lhs, kkeys = kslice_fn(kt)
                        m = mask_fn(kt)
                        add("pe", lambda e, sbk=sbk, lhs=lhs, m=m: e.matmul(pb[sbk][:, :], lhsT=lhs, rhs=q_rhs, start=True, stop=(m is None)),
                            r=kkeys + ["qT"], w=[skey])
                        if m is not None:
                            mlhs, mkeys = m
                            add("pe", lambda e, sbk=sbk, mlhs=mlhs: e.matmul(pb[sbk][:, :], lhsT=mlhs, rhs=i4[:, :], start=False, stop=True),
                                r=mkeys + ["c_i4"], w=[skey])
                        add("act", lambda e, sbk=sbk, j=j: e.activation(out=PT[:, j, :], in_=pb[sbk][:, :], func=AF.Exp, scale=0.125),
                            r=[skey], w=[("PT", j)])
                    for h in range(4):
                        for j0, kt in enumerate(blk):
                            j = pt0 + j0
                            rhs, vkeys = v_fn(kt)
                            st_flag = first_pv[0]
                            first_pv[0] = False
                            add("pe", lambda e, ob=ob, h=h, j=j, rhs=rhs, st_flag=st_flag: e.matmul(
                                pb[ob][:, h * 128:h * 128 + width], lhsT=PT[:, j, h * 128:(h + 1) * 128], rhs=rhs,
                                start=st_flag, stop=False, skip_group_check=True),
                                r=[("PT", j)] + vkeys, w=[okey])
                return pb[ob], okey

            for b in range(nseq):
                add("dve", lambda e, b=b: e.tensor_copy(out=posf[:], in_=posi[:, b, :]), r=["posi"], w=["posf"])
                ang = score[:, 0:NT * 32].rearrange("p (t j) -> p t j", t=NT)
                angk = score[:, 512:512 + NT * 32].rearrange("p (t j) -> p t j", t=NT)
                angi = junk2[:, 0:NT * 64].bitcast(I32).rearrange("p (t j) -> p t j", t=NT)
                for (dstT, shift) in ((sinT, 0.5), (cosT, 0.75)):
                    add("dve", lambda e: e.tensor_tensor(out=ang, in0=bc(invf.unsqueeze(1), [128, NT, 32]),
                                                         in1=bc(posf[:].unsqueeze(2), [128, NT, 32]), op=ALU.mult),
                        r=["posf", "c_misc"], w=["score"])
                    add("dve", lambda e, shift=shift: e.tensor_scalar(out=ang, in0=ang, scalar1=1.0 / (2.0 * math.pi), scalar2=shift,
                                                                       op0=ALU.mult, op1=ALU.add), r=["score"], w=["score"])
                    add("dve", lambda e: e.tensor_copy(out=angi, in_=ang), r=["score"], w=["junk2"])
                    add("dve", lambda e: e.tensor_copy(out=angk, in_=angi), r=["junk2"], w=["score"])
                    add("dve", lambda e: e.tensor_tensor(out=ang, in0=ang, in1=angk, op=ALU.subtract), r=["score"], w=["score"])
                    add("dve", lambda e: e.scalar_tensor_tensor(out=ang, in0=ang, scalar=0.0, in1=ang, op0=ALU.is_lt, op1=ALU.add),
                        r=["score"], w=["score"])
                    add("act", lambda e, dstT=dstT: e.activation(out=dstT[:], in_=ang, func=AF.Sin, bias=negpi, scale=2.0 * math.pi),
                        r=["score", "c_misc"], w=["rope_tab"])
                add("sp", lambda e, b=b: e.dma_start(out=G1[:], in_=bc(gscr_d[b:b + 1, 0, :], [128, D])), r=["gscr"], w=["G1"], dma=True)
                add("pool", lambda e: e.memset(GH[:], 0.0), w=["GH"])

                for t in range(NT):
                    xb = xt[t % 2]
                    xkey = "xt%d" % (t % 2)
                    tsl = slice(t * 128, (t + 1) * 128)
                    add("sp", lambda e, b=b, tsl=tsl, xb=xb: e.dma_start(out=xb[:], in_=x_d[b, tsl, :]), w=[xkey], dma=True)
                    add("dve", lambda e: e.memset(st4[:, 0:1], 0.0), w=["ss0"])
                    add("act", lambda e, xb=xb: e.activation(out=xn[:], in_=xb[:], func=AF.Square, accum_out=st4[:, 0:1]),
                        r=[xkey], w=["xn", "ss0"])
                    rstd_from_ss(st4[:, 0:1], st4[:, 1:2], ["ss0"], "rstd")
                    add("act", lambda e, xb=xb: e.activation(out=xn[:], in_=xb[:], func=AF.Copy, scale=st4[:, 1:2]),
                        r=[xkey, "rstd"], w=["xn"])
                    for k in range(8):
                        add("pe", lambda e, k=k: e.transpose(out=pbf(0)[:, k * 128:(k + 1) * 128], in_=xn[:, k * 128:(k + 1) * 128],
                                                             identity=i4[:, 0:128]), r=["xn", "c_i4"], w=["pb0"])
                    for k in range(8):
                        add("dve", lambda e, k=k, b=b: e.tensor_scalar(out=hT[:, k, :], in0=pbf(0)[:, k * 128:(k + 1) * 128],
                                                                        scalar1=AB[:, 0, b, k:k + 1], scalar2=AB[:, 1, b, k:k + 1],
                                                                        op0=ALU.mult, op1=ALU.add), r=["pb0", "AB"], w=["hT"])
                    chunks = [(0, 512), (512, 512), (1024, 512), (1536, 256), (1792, 476)]
                    for ci, (c0, cw) in enumerate(chunks):
                        bk = 1 + (ci % 2)
                        bkey = "pb%d" % bk
                        for k in range(8):
                            add("pe", lambda e, k=k, bk=bk, c0=c0, cw=cw: e.matmul(pb[bk][:, 0:cw], lhsT=hT[:, k, :], rhs=Win[:, k, c0:c0 + cw],
                                                                                   start=(k == 0), stop=(k == 7)), r=["hT", "weights"], w=[bkey])
                        if ci < 4:
                            nh = cw // 64
                            h0 = c0 // 64
                            pv = pb[bk][:, 0:cw].rearrange("p (h two j) -> p h two j", two=2, j=32)
                            cs_ = bc(cosT[:, t, :].unsqueeze(1), [128, nh, 32])
                            sn_ = bc(sinT[:, t, :].unsqueeze(1), [128, nh, 32])
                            add("dve", lambda e, pv=pv, cs_=cs_, nh=nh: e.tensor_tensor(out=rtmp[:, 0, 0:nh, :], in0=pv[:, :, 0, :], in1=cs_, op=ALU.mult),
                                r=[bkey, "rope_tab"], w=["rt0"])
                            add("dve", lambda e, pv=pv, sn_=sn_, nh=nh: e.tensor_tensor(out=rtmp[:, 1, 0:nh, :], in0=pv[:, :, 1, :], in1=sn_, op=ALU.mult),
                                r=[bkey, "rope_tab"], w=["rt1"])
                            add("dve", lambda e, pv=pv, cs_=cs_, nh=nh: e.tensor_tensor(out=rtmp[:, 2, 0:nh, :], in0=pv[:, :, 1, :], in1=cs_, op=ALU.mult),
                                r=[bkey, "rope_tab"], w=["rt2"])
                            add("dve", lambda e, pv=pv, sn_=sn_, nh=nh: e.tensor_tensor(out=rtmp[:, 3, 0:nh, :], in0=pv[:, :, 0, :], in1=sn_, op=ALU.mult),
                                r=[bkey, "rope_tab"], w=["rt3"])
                            add("pool", lambda e, nh=nh, h0=h0: e.tensor_tensor(out=rq[:, h0:h0 + nh, 0, :], in0=rtmp[:, 0, 0:nh, :], in1=rtmp[:, 1, 0:nh, :],
                                                                                 op=ALU.subtract), r=["rt0", "rt1"], w=["rq"])
                            add("pool", lambda e, nh=nh, h0=h0: e.tensor_tensor(out=rq[:, h0:h0 + nh, 1, :], in0=rtmp[:, 2, 0:nh, :], in1=rtmp[:, 3, 0:nh, :],
                                                                                 op=ALU.add), r=["rt2", "rt3"], w=["rq"])
                        else:
                            pvv = pb[bk]
                            add("act", lambda e, pvv=pvv: e.activation(out=vct[:], in_=pvv[:, 0:128], func=AF.Copy), r=[bkey], w=["vct"])
                            add("act", lambda e, pvv=pvv, t=t: e.activation(out=VA[:, t, :, 0:64], in_=pvv[:, 128:448].rearrange("p (s d) -> p s d", s=5),
                                                                             func=AF.Copy), r=[bkey], w=[("VA", t)])
                            g24 = gate[:].rearrange("p h c -> p (h c)")
                            add("act", lambda e, pvv=pvv: e.activation(out=g24, in_=pvv[:, 448:472], func=AF.Exp, scale=-1.0), r=[bkey], w=["gate"])
                            add("dve", lambda e: e.tensor_scalar(out=g24, in0=g24, scalar1=1.0, scalar2=None, op0=ALU.add), r=["gate"], w=["gate"])
                            add("dve", lambda e: e.reciprocal(out=g24, in_=g24), r=["gate"], w=["gate"])
                            add("dve", lambda e, pvv=pvv: e.tensor_scalar(out=WS[:], in0=pvv[:, 472:476], scalar1=0.0, scalar2=2.0,
                                                                           op0=ALU.is_ge, op1=ALU.mult), r=[bkey], w=["WS"])
                            add("dve", lambda e: e.tensor_scalar(out=WS[:], in0=WS[:], scalar1=-1.0, scalar2=None, op0=ALU.add), r=["WS"], w=["WS"])
                            add("dve", lambda e, pvv=pvv: e.scalar_tensor_tensor(out=WA[:], in0=pvv[:, 472:476], scalar=IDX_SCALE, in1=WS[:],
                                                                                  op0=ALU.mult, op1=ALU.mult), r=[bkey, "WS"], w=["WA"])
                    rq2 = rq[:].rearrange("p h two j -> p (h two j)")
                    for p_ in range(8):
                        add("pe", lambda e, p_=p_: e.transpose(out=pbf(3)[:, p_ * 128:(p_ + 1) * 128], in_=rq2[:, p_ * 128:(p_ + 1) * 128],
                                                               identity=i4[:, 0:128]), r=["rq", "c_i4"], w=["pb3"])
                    add("act", lambda e: e.activation(out=qT[:, 0:8, :], in_=pbf(3)[:, :].rearrange("p (s q) -> p s q", s=8), func=AF.Copy),
                        r=["pb3"], w=["qT"])
                    for p_ in range(8, 14):
                        add("pe", lambda e, p_=p_: e.transpose(out=pbf(3)[:, (p_ - 8) * 128:(p_ - 7) * 128], in_=rq2[:, p_ * 128:(p_ + 1) * 128],
                                                               identity=i4[:, 0:128]), r=["rq", "c_i4"], w=["pb3"])
                    p3 = pbf(3)[:, 0:768].rearrange("p (s q) -> p s q", s=6)
                    add("dve", lambda e, p3=p3, tsl=tsl: e.tensor_copy(out=KT[0:64, 0:6, tsl], in_=p3[0:64, :, :]), r=["pb3"], w=[("KT", t)])
                    add("act", lambda e, p3=p3, tsl=tsl: e.activation(out=KT[64:128, 0:2, tsl], in_=p3[64:128, 0:2, :], func=AF.Copy),
                        r=["pb3"], w=[("KTd", t)])
                    add("act", lambda e, p3=p3: e.activation(out=qT[64:128, 10:14, :], in_=p3[64:128, 2:6, :], func=AF.Copy), r=["pb3"], w=["qT"])
                    add("pe", lambda e: e.transpose(out=pbf(0)[:, 0:128], in_=vct[:], identity=i4[:, 0:128]), r=["vct", "c_i4"], w=["pb0"])
                    add("dve", lambda e, tsl=tsl: e.tensor_copy(out=VCT[:, tsl], in_=pbf(0)[:, 0:128]), r=["pb0"], w=[("VCT", t)])

                    dump("G1", G1[:], ["G1"], t)
                    dump("cosT", cosT[:].rearrange("p t j -> p (t j)"), ["rope_tab"], t)
                    dump("sinT", sinT[:].rearrange("p t j -> p (t j)"), ["rope_tab"], t)
                    dump("hT", hT[:].rearrange("p k q -> p (k q)"), ["hT"], t)
                    dump("rq", rq[:].rearrange("p h two j -> p (h two j)"), ["rq"], t)
                    dump("qT", qT[:].rearrange("p s q -> p (s q)"), ["qT"], t)
                    dump("gate", gate[:].rearrange("p h c -> p (h c)"), ["gate"], t)
                    dump("WA", WA[:], ["WA"], t)
                    dump("WS", WS[:], ["WS"], t)
                    dump("VA", VA[:, t, :, 0:65], [("VA", t)], t)
                    dump("KT", KT[:, :, tsl], [("KT", t), ("KTd", t)], t)
                    cut("P", t)
                    n0 = 0 if t == 0 else 8 * t - 1
                    nb = 7 if t == 0 else 8
                    kkeys = [("KT", tt) for tt in range(max(0, t - 1), t + 1)]
                    vkeys_ = [("VCT", tt) for tt in range(max(0, t - 1), t + 1)]
                    tok0 = 16 * n0
                    xk = KT[0:64, 0:2, tok0:tok0 + 16 * (nb + 1)].rearrange("p g (n l) -> p g n l", l=16)
                    xv = VCT[:, tok0:tok0 + 16 * (nb + 1)].rearrange("p (n l) -> p n l", l=16)
                    for lhi in range(2):
                        add("pool", lambda e, lhi=lhi, xk=xk, nb=nb: e.tensor_copy(out=Ygk[:, :, lhi * 16:(lhi + 1) * 16, 0:nb],
                                                                                    in_=xk[:, :, lhi:lhi + nb, :].rearrange("p g n l -> p g l n")),
                            r=kkeys, w=["Ygk"])
                        add("pool", lambda e, lhi=lhi, xv=xv, nb=nb: e.tensor_copy(out=Ygv[:, lhi * 16:(lhi + 1) * 16, 0:nb],
                                                                                    in_=xv[:, lhi:lhi + nb, :].rearrange("p n l -> p l n")),
                            r=vkeys_, w=["Ygv"])
                    for kv in range(2):
                        for g in range(2):
                            jj = kv * 2 + g
                            for l in range(32):
                                if kv == 0:
                                    lhs = W1k[0:64, l, :]
                                    rhs = Ygk[:, g, l, 0:nb]
                                    rk = ["Ygk"]
                                else:
                                    lhs = W1v[g * 64:(g + 1) * 64, l, :]
                                    rhs = Ygv[g * 64:(g + 1) * 64, l, 0:nb]
                                    rk = ["Ygv"]
                                cb = 2 if jj == 3 else 1
                                add("pe", lambda e, lhs=lhs, rhs=rhs, jj=jj, l=l, nb=nb, cb=cb: e.matmul(pb[cb][:, jj * 8:jj * 8 + nb], lhsT=lhs, rhs=rhs,
                                                                                                          start=(l == 0), stop=(l == 31)),
                                    r=rk + ["weights"], w=["pb%d" % cb])
                    cut("C0", t)
                    pch = pb[1][:, 0:32].rearrange("p (a n) -> p a n", a=4)
                    pch2 = pb[2][:, 0:32].rearrange("p (a n) -> p a n", a=4)
                    add("dve", lambda e: e.tensor_scalar(out=hb[:, 0:2, :], in0=pch[:, 0:2, :], scalar1=bT[:, 0:1], scalar2=None, op0=ALU.add),
                        r=["pb1", "bT"], w=["hb"])
                    add("dve", lambda e: e.tensor_scalar(out=hb[:, 2:3, :], in0=pch[:, 2:3, :], scalar1=bT[:, 1:2], scalar2=None, op0=ALU.add),
                        r=["pb1", "bT"], w=["hb"])
                    add("dve", lambda e: e.tensor_scalar(out=hb[:, 3:4, :], in0=pch2[:, 3:4, :], scalar1=bT[:, 1:2], scalar2=None, op0=ALU.add),
                        r=["pb2", "bT"], w=["hb"])
                    add("dve", lambda e: e.tensor_tensor(out=hu[:], in0=hb[:], in1=hb[:], op=ALU.mult), r=["hb"], w=["hu"])
                    add("dve", lambda e: e.tensor_scalar(out=hu[:], in0=hu[:], scalar1=0.044715, scalar2=1.0, op0=ALU.mult, op1=ALU.add), r=["hu"], w=["hu"])
                    add("dve", lambda e: e.tensor_tensor(out=hu[:], in0=hu[:], in1=hb[:], op=ALU.mult), r=["hu", "hb"], w=["hu"])
                    add("act", lambda e: e.activation(out=hu[:], in_=hu[:], func=AF.Exp, scale=-2.0 * GELU_C), r=["hu"], w=["hu"])
                    add("dve", lambda e: e.tensor_scalar(out=hu[:], in0=hu[:], scalar1=1.0, scalar2=None, op0=ALU.add), r=["hu"], w=["hu"])
                    add("dve", lambda e: e.reciprocal(out=hu[:], in_=hu[:]), r=["hu"], w=["hu"])
                    hb4 = hb[:].rearrange("p (kv g) n -> p kv g n", kv=2)
                    hu4 = hu[:].rearrange("p (kv g) n -> p kv g n", kv=2)
                    add("dve", lambda e, n0=n0, nb=nb: e.tensor_tensor(out=GH[:, :, :, n0:n0 + nb], in0=hb4[:, :, :, 0:nb], in1=hu4[:, :, :, 0:nb], op=ALU.mult),
                        r=["hb", "hu"], w=["GH"])
                    dump("GH1", GH[:].rearrange("p a g n -> p (a g n)"), ["GH"], t)
                    cut("C1", t)
                    for g in range(2):
                        add("pe", lambda e, g=g: e.matmul(pb[2][0:64, g * 128:(g + 1) * 128], lhsT=W2[:, 0, :], rhs=GH[:, 0, g, :], start=True, stop=True),
                            r=["GH", "weights"], w=["pb2"])
                    add("act", lambda e: e.activation(out=KCT[:, :, :], in_=pb[2][0:64, 0:256].rearrange("p (g n) -> p g n", g=2), func=AF.Copy),
                        r=["pb2"], w=["KCT"])
                    for g in range(2):
                        add("pe", lambda e, g=g: e.matmul(pb[3][:, g * 64:(g + 1) * 64], lhsT=GH[:, 1, g, :], rhs=W2[:, 1, :], start=True, stop=True),
                            r=["GH", "weights"], w=["pb3"])
                    add("act", lambda e: e.activation(out=VCa[:, :, 0:64], in_=pb[3][:, 0:128].rearrange("p (g d) -> p g d", g=2), func=AF.Copy),
                        r=["pb3"], w=["VCa"])

                    dump("GH", GH[:].rearrange("p a g n -> p (a g n)"), ["GH"], t)
                    dump("KCT", KCT[:].rearrange("p g n -> p (g n)"), ["KCT"], t)
                    dump("VCa", VCa[:, :, 0:97], ["VCa"], t)
                    cut("C", t)
                    nkeys = (t + 1) * 128
                    if t >= 2:
                        for c0 in range(0, nkeys, 512):
                            cw = min(512, nkeys - c0)
                            kk = [("KTd", kt) for kt in range(c0 // 128, (c0 + cw) // 128)]
                            for h in range(4):
                                bk = 1 + (h % 2)
                                bkey = "pb%d" % bk
                                at = atmp[h % 2]
                                akey = "atmp%d" % (h % 2)
                                add("pe", lambda e, bk=bk, h=h, c0=c0, cw=cw: e.matmul(pb[bk][:, 0:cw], lhsT=qT[64:128, 10 + h, :], rhs=KT[64:128, 1, c0:c0 + cw],
                                                                                       start=True, stop=True), r=["qT"] + kk, w=[bkey])
                                add("act", lambda e, bk=bk, h=h, cw=cw, at=at: e.activation(out=at[:, 0:cw], in_=pb[bk][:, 0:cw], func=AF.Relu, scale=WA[:, h:h + 1]),
                                    r=[bkey, "WA"], w=[akey])
                                if h == 0:
                                    add("dve", lambda e, at=at, c0=c0, cw=cw: e.tensor_scalar(out=score[:, c0:c0 + cw], in0=at[:, 0:cw], scalar1=WS[:, 0:1], scalar2=None, op0=ALU.mult),
                                        r=[akey, "WS"], w=["score"])
                                else:
                                    add("dve", lambda e, at=at, c0=c0, cw=cw, h=h: e.scalar_tensor_tensor(out=score[:, c0:c0 + cw], in0=at[:, 0:cw], scalar=WS[:, h:h + 1],
                                                                                                            in1=score[:, c0:c0 + cw], op0=ALU.mult, op1=ALU.add),
                                        r=[akey, "WS"], w=["score"])
                        add("dve", lambda e, tsl=tsl: e.tensor_tensor(out=score[:, tsl], in0=score[:, tsl], in1=causalf[:], op=ALU.add), r=["c_causalf"], w=["score"])
                    for g in range(2):
                        sbk = sbank[sb_i[0] % 2]
                        sb_i[0] += 1
                        skey = "pb%d" % sbk
                        q_rhs = qT[0:64, 4 * g:4 * g + 4, :]
                        add("pe", lambda e, sbk=sbk, g=g, q_rhs=q_rhs: e.matmul(pb[sbk][:, :], lhsT=KCT[:, g, :], rhs=q_rhs, start=True, stop=False),
                            r=["KCT", "qT"], w=[skey])
                        add("pe", lambda e, sbk=sbk, t=t: e.matmul(pb[sbk][:, :], lhsT=cmpbias[:, t, :], rhs=i4[:, :], start=False, stop=True),
                            r=["c_cmpbias", "c_i4"], w=[skey])
                        add("act", lambda e, sbk=sbk: e.activation(out=PTc[:], in_=pb[sbk][:, :], func=AF.Exp, scale=0.125), r=[skey], w=["PTc"])
                        dump("PTc", PTc[:], ["PTc"], t)
                        cut("A1a", t)
                        ob = obank[ob_i[0] % 2]
                        ob_i[0] += 1
                        okey = "pb%d" % ob
                        for h in range(4):
                            add("pe", lambda e, ob=ob, h=h, g=g: e.matmul(pb[ob][:, h * 128:h * 128 + 97], lhsT=PTc[:, h * 128:(h + 1) * 128], rhs=VCa[:, g, 0:97],
                                                                          start=(h == 0), stop=False, skip_group_check=True), r=["PTc", "VCa"], w=[okey])
                        v, vkey = pv_evac(pb[ob], okey, 97, 256 * g, 0, g, True)
                        if t >= 8:
                            for h in range(4):
                                if h == 0:
                                    add("dve", lambda e, v=v, g=g: e.tensor_scalar(out=imp[:, g, :], in0=v[:, 0, 65:97], scalar1=rc[:, 0:1], scalar2=None, op0=ALU.mult),
                                        r=[vkey, "rc"], w=[("imp", g)])
                                else:
                                    add("dve", lambda e, v=v, g=g, h=h: e.scalar_tensor_tensor(out=imp[:, g, :], in0=v[:, h, 65:97], scalar=rc[:, h:h + 1],
                                                                                               in1=imp[:, g, :], op0=ALU.mult, op1=ALU.add),
                                        r=[vkey, "rc"], w=[("imp", g)])

                    dump("Ot_A1", Ot[:, 0:512], ["Ot"], t)
                    dump("imp", imp[:].rearrange("p g j -> p (g j)"), [("imp", 0), ("imp", 1)], t)
                    cut("A1", t)
                    if t >= 8:
                        for g in range(2):
                            add("dve", lambda e, g=g, t=t: e.tensor_tensor(out=tk[:, 0:32], in0=imp[:, g, :], in1=forceb[:, t, :], op=ALU.add),
                                r=[("imp", g), "c_forceb"], w=["tk"])
                            add("dve", lambda e: e.max(out=tk[:, 32:40], in_=tk[:, 0:32]), r=["tk"], w=["tk"])
                            add("dve", lambda e: e.match_replace(out=tk[:, 48:80], in_to_replace=tk[:, 32:40], in_values=tk[:, 0:32], imm_value=-1e9),
                                r=["tk"], w=["tk"])
                            add("dve", lambda e: e.max(out=tk[:, 40:48], in_=tk[:, 48:80]), r=["tk"], w=["tk"])
                            add("dve", lambda e, g=g: e.tensor_scalar(out=selb[:, g, :], in0=tk[:, 0:32], scalar1=tk[:, 47:48], scalar2=NEGB, op0=ALU.is_lt, op1=ALU.mult),
                                r=["tk"], w=[("selb", g)])
                    cut("SEL", t)
                    if t >= 2:
                        add("dve", lambda e, nkeys=nkeys: e.tensor_reduce(out=bis[:, 0:1], in_=score[:, 0:nkeys], axis=AX.X, op=ALU.max), r=["score"], w=["bis"])
                        add("dve", lambda e, t=t: e.tensor_reduce(out=bis[:, 1:2], in_=score[:, 0:t * 128], axis=AX.X, op=ALU.min), r=["score"], w=["bis"])
                        add("dve", lambda e: e.tensor_scalar(out=bis[:, 1:2], in0=bis[:, 1:2], scalar1=-1.0, scalar2=None, op0=ALU.add), r=["bis"], w=["bis"])
                        add("dve", lambda e: e.tensor_tensor(out=bis[:, 2:3], in0=bis[:, 0:1], in1=bis[:, 1:2], op=ALU.add), r=["bis"], w=["bis"])
                        add("dve", lambda e: e.tensor_scalar(out=bis[:, 2:3], in0=bis[:, 2:3], scalar1=0.5, scalar2=None, op0=ALU.mult), r=["bis"], w=["bis"])
                        add("dve", lambda e: e.tensor_tensor(out=bis[:, 3:4], in0=bis[:, 0:1], in1=bis[:, 1:2], op=ALU.subtract), r=["bis"], w=["bis"])
                        add("dve", lambda e: e.tensor_scalar(out=wtab[:], in0=misc[:, 33:33 + NBIS + 1], scalar1=bis[:, 3:4], scalar2=0.5, op0=ALU.mult, op1=ALU.mult),
                            r=["bis", "c_misc"], w=["wtab"])
                        for it in range(NBIS):
                            add("dve", lambda e: e.memset(bis[:, 4:5], 0.0), w=["bis"])
                            add("dve", lambda e, nkeys=nkeys: e.tensor_scalar(out=junk2[:, 0:nkeys], in0=score[:, 0:nkeys], scalar1=bis[:, 2:3], scalar2=0.0,
                                                                               op0=ALU.is_gt, op1=ALU.add, accum_out=bis[:, 4:5]), r=["score", "bis"], w=["junk2", "bis"])
                            add("dve", lambda e, it=it: e.tensor_scalar(out=bis[:, 5:6], in0=bis[:, 4:5], scalar1=255.5, scalar2=wtab[:, it:it + 1],
                                                                         op0=ALU.is_gt, op1=ALU.mult), r=["bis", "wtab"], w=["bis"])
                            add("dve", lambda e, it=it: e.scalar_tensor_tensor(out=bis[:, 2:3], in0=bis[:, 5:6], scalar=2.0, in1=bis[:, 2:3], op0=ALU.mult, op1=ALU.add),
                                r=["bis"], w=["bis"])
                            add("dve", lambda e, it=it: e.tensor_tensor(out=bis[:, 2:3], in0=bis[:, 2:3], in1=wtab[:, it:it + 1], op=ALU.subtract), r=["bis", "wtab"], w=["bis"])
                        add("dve", lambda e: e.tensor_tensor(out=bis[:, 6:7], in0=bis[:, 2:3], in1=wtab[:, NBIS - 1:NBIS], op=ALU.subtract), r=["bis", "wtab"], w=["bis"])
                        add("dve", lambda e, nkeys=nkeys: e.tensor_scalar(out=maskb[:, 0:nkeys], in0=score[:, 0:nkeys], scalar1=bis[:, 6:7], scalar2=NEGB,
                                                                            op0=ALU.is_le, op1=ALU.mult), r=["score", "bis"], w=["maskb"])

                        def dmask(kt):
                            return (maskb[:, kt * 128:(kt + 1) * 128], ["maskb"])
                    else:
                        def dmask(kt, t=t):
                            return (causal[:], ["c_causal"]) if kt == t else None
                    for g in range(2):
                        if t >= 8:
                            nkeys = (t + 1) * 128
                            add("pool", lambda e, nkeys=nkeys, g=g: e.tensor_copy(out=selx[:, 0:nkeys].rearrange("p (j k) -> p j k", k=64),
                                                                              in_=bc(selb[:, g, 0:nkeys // 64].unsqueeze(2), [128, nkeys // 64, 64])),
                                r=[("selb", g)], w=["selx"])
                            add("pool", lambda e, tsl=tsl: e.tensor_tensor(out=selx[:, tsl], in0=selx[:, tsl], in1=causal[:], op=ALU.add),
                                r=["c_causal"], w=["selx"])

                            def mask_fn(kt):
                                return (selx[:, kt * 128:(kt + 1) * 128], ["selx"])
                        else:
                            def mask_fn(kt, t=t):
                                return (causal[:], ["c_causal"]) if kt == t else None
                        bank, okey = attention(
                            t, list(range(t + 1)),
                            lambda kt, g=g: (KT[0:64, 2 + g, kt * 128:(kt + 1) * 128], [("KT", kt)]),
                            qT[0:64, 4 * g:4 * g + 4, :], mask_fn,
                            lambda kt, g=g: (VA[:, kt, g, 0:65], [("VA", kt)]))
                        pv_evac(bank, okey, 65, 256 * g, 1, g, False)

                    dump("Ot_A2", Ot[:, 0:512], ["Ot"], t)
                    dump("selx", selx[:, 0:(t + 1) * 128], ["selx"], t)
                    cut("A2", t)
                    for g in range(2):
                        def mask_fn(kt, t=t):
                            if kt == t:
                                return (causal[:], ["c_causal"])
                            if kt == t - 4:
                                return (band[:], ["c_band"])
                            return None
                        bank, okey = attention(
                            t, list(range(max(0, t - 4), t + 1)),
                            lambda kt, g=g: (KT[0:64, 4 + g, kt * 128:(kt + 1) * 128], [("KT", kt)]),
                            qT[0:64, 4 * g:4 * g + 4, :], mask_fn,
                            lambda kt, g=g: (VA[:, kt, 2 + g, 0:65], [("VA", kt)]))
                        pv_evac(bank, okey, 65, 256 * g, 2, g, False)

                    dump("Ot_A3", Ot[:, 0:512], ["Ot"], t)
                    cut("A3", t)
                    for hg in range(2):
                        bank, okey = attention(
                            t, list(range(t + 1)),
                            lambda kt: (KT[64:128, 0, kt * 128:(kt + 1) * 128], [("KTd", kt)]),
                            qT[64:128, 4 * hg:4 * hg + 4, :], dmask,
                            lambda kt: (VA[:, kt, 4, 0:65], [("VA", kt)]))
                        pv_evac(bank, okey, 65, 512 + 256 * hg, None, 0, True)

                    dump("Ot_A4", Ot[:], ["Ot"], t)
                    dump("score", score[:, 0:(t + 1) * 128], ["score"], t)
                    dump("maskb", maskb[:, 0:(t + 1) * 128], ["maskb"], t)
                    dump("bis", bis[:], ["bis"], t)
                    cut("A4", t)
                    add("act", lambda e: e.activation(out=Otb[:], in_=Ot[:], func=AF.Copy), r=["Ot"], w=["Otb"])
                    for k in range(8):
                        add("pe", lambda e, k=k: e.transpose(out=pbf(0)[:, k * 128:(k + 1) * 128], in_=Otb[:, k * 128:(k + 1) * 128], identity=i4[:, 0:128]),
                            r=["Otb", "c_i4"], w=["pb0"])
                    add("dve", lambda e: e.tensor_copy(out=oT[:].rearrange("p k q -> p (k q)"), in_=pbf(0)[:, :]), r=["pb0"], w=["oT"])
                    add("dve", lambda e: e.memset(st4[:, 4:6], 0.0), w=["ssy0", "ssy1"])
                    for half in range(2):
                        bk = 1 + half
                        bkey = "pb%d" % bk
                        for k in range(8):
                            add("pe", lambda e, k=k, bk=bk, half=half: e.matmul(pb[bk][:, :], lhsT=oT[:, k, :], rhs=Wout[:, k, half * 512:(half + 1) * 512],
                                                                                start=(k == 0), stop=(k == 7)), r=["oT", "weights"], w=[bkey])
                        add("act", lambda e, bk=bk, half=half: e.activation(out=junk2[:, half * 512:(half + 1) * 512], in_=pb[bk][:, :], func=AF.Square,
                                                                              accum_out=st4[:, 4 + half:5 + half]), r=[bkey], w=["junk2", "ssy%d" % half])
                    add("dve", lambda e: e.tensor_tensor(out=st4[:, 6:7], in0=st4[:, 4:5], in1=st4[:, 5:6], op=ALU.add), r=["ssy0", "ssy1"], w=["ssy"])
                    rstd_from_ss(st4[:, 6:7], st4[:, 7:8], ["ssy"], "rstdy")
                    for half in range(2):
                        bk = 1 + half
                        bkey = "pb%d" % bk
                        hs = slice(half * 512, (half + 1) * 512)
                        add("dve", lambda e, bk=bk, hs=hs: e.scalar_tensor_tensor(out=x1t[:, hs], in0=pb[bk][:, :], scalar=st4[:, 7:8], in1=G1[:, hs],
                                                                                   op0=ALU.mult, op1=ALU.mult), r=[bkey, "rstdy", "G1"], w=["x1t"])
                    add("pool", lambda e, xb=xb: e.tensor_tensor(out=x1t[:], in0=x1t[:], in1=xb[:], op=ALU.add), r=[xkey], w=["x1t"])
                    add("sp", lambda e, b=b, tsl=tsl: e.dma_start(out=out_d[b, tsl, :], in_=x1t[:]), r=["x1t"], w=[("out", b, t)], dma=True)
                    cut("O", t)
                    if debug and b == 0:
                        for name in debug:
                            if name == "Ot%d" % t:
                                add("sp", lambda e, name=name: e.dma_start(out=dbg_d[name], in_=Ot[:]), r=["Ot"], w=["dbg_" + name], dma=True)
                            if name == "qT%d" % t:
                                add("act", lambda e: e.activation(out=score[:, 0:1792], in_=qT[:].rearrange("p s q -> p (s q)"), func=AF.Copy), r=["qT"], w=["score"])
                                add("sp", lambda e, name=name: e.dma_start(out=dbg_d[name], in_=score[:, 0:1792]), r=["score"], w=["dbg_" + name], dma=True)
            S.barrier()
        cut("mixer")

        with ExitStack() as es2:
            Fz = es2
            Wup = sbuf(Fz, "Wup", [128, 8, DFF], BF16)
            Wdn = sbuf(Fz, "Wdn", [128, 32, D], BF16)
            with ExitStack() as es_w:
                wst = [sbuf(es_w, "wstf%d" % i, [128, 8, 512], F32) for i in range(2)]
                wup_v = wup_d.rearrange("(k p) n -> p k n", p=128)
                wdn_v = wdn_d.rearrange("(k p) n -> p k n", p=128)
                ci = 0
                for c0 in range(0, DFF, 512):
                    st = wst[ci % 2]
                    key = "wstf%d" % (ci % 2)
                    add("sp", lambda e, st=st, c0=c0: e.dma_start(out=st[:], in_=wup_v[:, :, c0:c0 + 512]), w=[key], dma=True)
                    add("pool" if ci % 2 else "dve", lambda e, st=st, c0=c0: e.tensor_copy(out=Wup[:, :, c0:c0 + 512], in_=st[:]), r=[key], w=["fw"])
                    ci += 1
                for k0 in range(0, 32, 4):
                    st = wst[ci % 2]
                    key = "wstf%d" % (ci % 2)
                    stv = st[:].rearrange("p a n -> p (a n)").rearrange("p (a n) -> p a n", a=4)
                    add("sp", lambda e, stv=stv, k0=k0: e.dma_start(out=stv, in_=wdn_v[:, k0:k0 + 4, :]), w=[key], dma=True)
                    add("pool" if ci % 2 else "dve", lambda e, stv=stv, k0=k0: e.tensor_copy(out=Wdn[:, k0:k0 + 4, :], in_=stv), r=[key], w=["fw"])
                    ci += 1
                S.barrier()
            NTC = 2
            NTOK = NTC * 128
            x1c = sbuf(Fz, "x1c", [128, NTC, D], F32)
            xn2 = sbuf(Fz, "xn2", [128, D], BF16)
            stf = sbuf(Fz, "stf", [128, 8], F32)
            h2T = sbuf(Fz, "h2T", [128, 8, NTOK], BF16)
            uT = sbuf(Fz, "uT", [128, 32, NTOK], BF16)
            rl = [sbuf(Fz, "rl%d" % i, [128, NTOK], F32) for i in range(2)]
            G2 = sbuf(Fz, "G2", [128, D], F32)
            yo = sbuf(Fz, "yo", [128, D], F32)
            jk = sbuf(Fz, "jk", [128, D], BF16)

            def rstd2(ss_ap, out_ap, rkeys, wkey):
                add("dve", lambda e: e.tensor_scalar(out=out_ap, in0=ss_ap, scalar1=1.0 / D, scalar2=EPS, op0=ALU.mult, op1=ALU.add), r=rkeys, w=[wkey])
                add("act", lambda e: e.activation(out=out_ap, in_=out_ap, func=AF.Ln), r=[wkey], w=[wkey])
                add("act", lambda e: e.activation(out=out_ap, in_=out_ap, func=AF.Exp, scale=-0.5), r=[wkey], w=[wkey])

            for b in range(nseq):
                add("sp", lambda e, b=b: e.dma_start(out=G2[:], in_=bc(gscr_d[b:b + 1, 1, :], [128, D])), r=["gscr"], w=["G2"], dma=True)
                for tc in range(NT // NTC):
                    for i in range(NTC):
                        t = tc * NTC + i
                        tsl = slice(t * 128, (t + 1) * 128)
                        add("sp", lambda e, b=b, tsl=tsl, i=i: e.dma_start(out=x1c[:, i, :], in_=out_d[b, tsl, :]), r=[("out", b, t)], w=[("x1c", i)], dma=True)
                        add("dve", lambda e: e.memset(stf[:, 0:1], 0.0), w=["fss"])
                        add("act", lambda e, i=i: e.activation(out=xn2[:], in_=x1c[:, i, :], func=AF.Square, accum_out=stf[:, 0:1]), r=[("x1c", i)], w=["xn2", "fss"])
                        rstd2(stf[:, 0:1], stf[:, 1:2], ["fss"], "frstd")
                        add("act", lambda e, i=i: e.activation(out=xn2[:], in_=x1c[:, i, :], func=AF.Copy, scale=stf[:, 1:2]), r=[("x1c", i), "frstd"], w=["xn2"])
                        for k in range(8):
                            add("pe", lambda e, k=k: e.transpose(out=pbf(0)[:, k * 128:(k + 1) * 128], in_=xn2[:, k * 128:(k + 1) * 128], identity=i4[:, 0:128]),
                                r=["xn2", "c_i4"], w=["pb0"])
                        for k in range(8):
                            add("dve", lambda e, k=k, b=b, i=i: e.tensor_scalar(out=h2T[:, k, i * 128:(i + 1) * 128], in0=pbf(0)[:, k * 128:(k + 1) * 128],
                                                                                 scalar1=AB[:, 2, b, k:k + 1], scalar2=AB[:, 3, b, k:k + 1], op0=ALU.mult, op1=ALU.add),
                                r=["pb0", "AB"], w=["h2T"])
                    for f in range(32):
                        bk = 1 + (f % 2)
                        bkey = "pb%d" % bk
                        r_ = rl[f % 2]
                        rkey = "rl%d" % (f % 2)
                        for k in range(8):
                            add("pe", lambda e, k=k, f=f, bk=bk: e.matmul(pb[bk][:, 0:NTOK], lhsT=Wup[:, k, f * 128:(f + 1) * 128], rhs=h2T[:, k, :],
                                                                          start=(k == 0), stop=(k == 7)), r=["h2T", "fw"], w=[bkey])
                        add("act", lambda e, bk=bk, r_=r_: e.activation(out=r_[:], in_=pb[bk][:, 0:NTOK], func=AF.Relu), r=[bkey], w=[rkey])
                        add("pool" if f % 2 else "dve", lambda e, f=f, r_=r_: e.tensor_tensor(out=uT[:, f, :], in0=r_[:], in1=r_[:], op=ALU.mult), r=[rkey], w=[("uT", f)])
                    for i in range(NTC):
                        t = tc * NTC + i
                        tsl = slice(t * 128, (t + 1) * 128)
                        add("dve", lambda e: e.memset(stf[:, 4:6], 0.0), w=["fssy0", "fssy1"])
                        for half in range(2):
                            bk = 3 + half
                            bkey = "pb%d" % bk
                            for f in range(32):
                                add("pe", lambda e, f=f, bk=bk, half=half, i=i: e.matmul(pb[bk][:, :], lhsT=uT[:, f, i * 128:(i + 1) * 128],
                                                                                        rhs=Wdn[:, f, half * 512:(half + 1) * 512], start=(f == 0), stop=(f == 31)),
                                    r=[("uT", f), "fw"], w=[bkey])
                            add("act", lambda e, bk=bk, half=half: e.activation(out=jk[:, half * 512:(half + 1) * 512], in_=pb[bk][:, :], func=AF.Square,
                                                                                  accum_out=stf[:, 4 + half:5 + half]), r=[bkey], w=["jk%d" % half, "fssy%d" % half])
                        add("dve", lambda e: e.tensor_tensor(out=stf[:, 6:7], in0=stf[:, 4:5], in1=stf[:, 5:6], op=ALU.add), r=["fssy0", "fssy1"], w=["fssy"])
                        rstd2(stf[:, 6:7], stf[:, 7:8], ["fssy"], "frstdy")
                        for half in range(2):
                            bk = 3 + half
                            bkey = "pb%d" % bk
                            hs = slice(half * 512, (half + 1) * 512)
                            add("dve", lambda e, bk=bk, hs=hs: e.scalar_tensor_tensor(out=yo[:, hs], in0=pb[bk][:, :], scalar=stf[:, 7:8], in1=G2[:, hs],
                                                                                       op0=ALU.mult, op1=ALU.mult), r=[bkey, "frstdy", "G2"], w=["yo"])
                        add("pool", lambda e, i=i: e.tensor_tensor(out=yo[:], in0=yo[:], in1=x1c[:, i, :], op=ALU.add), r=[("x1c", i)], w=["yo"])
                        add("sp", lambda e, b=b, tsl=tsl: e.dma_start(out=out_d[b, tsl, :], in_=yo[:]), r=["yo"], w=[("out", b, t)], dma=True)
            S.barrier()

    _build_body()
    S.stopped = False
    S.barrier()
    S.emit()
    top.close()
    return nc, S


def make_in_maps(inputs, cores=range(NCORES)):
    f32 = np.float32
    x = np.asarray(inputs["x"], f32)
    c = np.asarray(inputs["c"], f32)
    pos = np.asarray(inputs["positions"], np.int32)
    perm = _win_perm()
    w_in = np.ascontiguousarray(np.asarray(inputs["w_in"], f32)[0][:, perm])
    gcol = np.stack([np.asarray(inputs["g_pre_mix"], f32)[0].reshape(8, 128).T,
                     np.asarray(inputs["g_pre_ffn"], f32)[0].reshape(8, 128).T], axis=1)
    grow = np.stack([np.asarray(inputs["g_post_mix"], f32)[0], np.asarray(inputs["g_post_ffn"], f32)[0]], axis=0)[None]
    pek = np.asarray(inputs["cmp_pe_k"], f32)[0].T
    pev = np.asarray(inputs["cmp_pe_v"], f32)[0].T
    peT = np.zeros((128, 2, 32), f32)
    peT[0:64, 0] = pek
    peT[64:128, 0] = pek
    peT[0:64, 1] = pev
    peT[64:128, 1] = pev
    w1k = np.ascontiguousarray(np.asarray(inputs["cmp_w1_k"], f32)[0].reshape(32, 64, 128).transpose(1, 0, 2))
    w1v_ = np.asarray(inputs["cmp_w1_v"], f32)[0].reshape(32, 64, 128).transpose(1, 0, 2)
    w1v = np.ascontiguousarray(np.concatenate([w1v_, w1v_], axis=0))
    w2 = np.ascontiguousarray(np.stack([np.asarray(inputs["cmp_w2_k"], f32)[0], np.asarray(inputs["cmp_w2_v"], f32)[0]], axis=1))
    shared = dict(
        w_ada=np.ascontiguousarray(np.asarray(inputs["w_ada"], f32)[0]),
        b_ada=np.ascontiguousarray(np.asarray(inputs["b_ada"], f32)),
        b_adac=np.ascontiguousarray(np.asarray(inputs["b_ada"], f32)[0].reshape(48, 128).T),
        gcol=np.ascontiguousarray(gcol), grow=np.ascontiguousarray(grow), w_in=w_in, peT=peT, w1k=w1k, w1v=w1v, w2=w2,
        w_out=np.ascontiguousarray(np.asarray(inputs["w_out"], f32)[0]),
        w_up=np.ascontiguousarray(np.asarray(inputs["w_up"], f32)[0]),
        w_down=np.ascontiguousarray(np.asarray(inputs["w_down"], f32)[0]),
    )
    shared.update(_consts())
    maps = []
    for core in cores:
        b0 = core * SEQ_PER_CORE
        m = dict(shared)
        m["x"] = np.ascontiguousarray(x[b0:b0 + SEQ_PER_CORE])
        m["cT"] = np.ascontiguousarray(c[b0:b0 + SEQ_PER_CORE].T.reshape(8, 128, 4).transpose(1, 0, 2))
        m["posT"] = np.ascontiguousarray(pos[b0:b0 + SEQ_PER_CORE].reshape(4, NT, 128).transpose(2, 0, 1))
        maps.append(m)
    return maps


_PROG = {}


def kernel(**inputs):
    if "nc" not in _PROG:
        _PROG["nc"], _ = build_program()
    nc = _PROG["nc"]
    maps = make_in_maps(inputs)
    res = run_bass_kernel_spmd(nc, maps, core_ids=list(range(NCORES)))
    out = np.concatenate([np.asarray(r["out"], np.float32) for r in res.results], axis=0)
    return out
```

```python
import math
from contextlib import ExitStack

import numpy as np
import ml_dtypes

import concourse.bass as bass
import concourse.mybir as mybir
from concourse.bass_utils import run_bass_kernel_spmd

F32 = mybir.dt.float32
BF16 = mybir.dt.bfloat16
I32 = mybir.dt.int32
AF = mybir.ActivationFunctionType
ALU = mybir.AluOpType
AX = mybir.AxisListType

NCORES = 8
SEQ_PER_CORE = 4
T = 2048
D = 1024
NT = T // 128
DFF = 4096
D_IN = 2268
NEGB = -240000.0
EPS = 1e-6
IDX_SCALE = (4 ** -0.5) * (64 ** -0.5)
GELU_C = math.sqrt(2.0 / math.pi)
NBIS = 16


class Sched:
    COMPUTE = ("pe", "act", "dve", "pool")

    def __init__(self, nc, n_dma_sems=12):
        self.nc = nc
        self.ops = []
        self.lastw = {}
        self.readers = {}
        self.n_dma_sems = n_dma_sems
        self.dma_rr = {}
        self.dma_last = {}

    def add(self, eng, fn, r=(), w=(), dma=False, extra_deps=()):
        if getattr(self, "stopped", False):
            return -1
        idx = len(self.ops)
        deps = set(extra_deps)
        if eng != "pe":
            w = list(w) + [x for x in r if isinstance(x, str) and x.startswith("pb") and x not in w]
        pb_ = getattr(self, "pending_bar", None)
        if pb_ and eng in pb_:
            deps.add(pb_.pop(eng))
        for x in r:
            if x in self.lastw:
                deps.add(self.lastw[x])
        for x in w:
            if x in self.lastw:
                deps.add(self.lastw[x])
            for y in self.readers.get(x, ()):
                deps.add(y)
        for x in w:
            self.lastw[x] = idx
            self.readers[x] = []
        for x in r:
            if x not in w:
                self.readers.setdefault(x, []).append(idx)
        op = dict(eng=eng, fn=fn, deps=deps, dma=dma)
        if dma:
            k = self.dma_rr.get(eng, 0)
            self.dma_rr[eng] = k + 1
            slot = (eng, k % self.n_dma_sems)
            prev = self.dma_last.get(slot)
            if prev is not None:
                deps.add(prev)
            self.dma_last[slot] = idx
            op["slot"] = slot
        deps.discard(idx)
        self.ops.append(op)
        return idx

    def barrier(self):
        if getattr(self, "stopped", False):
            return
        live = set(self.lastw.values())
        for v in self.readers.values():
            live.update(v)
        for v in self.dma_last.values():
            live.add(v)
        b = self.add("sp", self.bar_fn, dma=True, extra_deps=live)
        self.lastw = {}
        self.readers = {}
        self.pending_bar = {e: b for e in self.COMPUTE}

    def emit(self):
        nc = self.nc
        ops = self.ops
        n = len(ops)
        es = ExitStack()
        sems = {}
        for e in self.COMPUTE + ("sp",):
            sems[e] = es.enter_context(nc.semaphore("s_" + e))
        dma_slots = sorted({op["slot"] for op in ops if op["dma"]})
        for s in dma_slots:
            sems[s] = es.enter_context(nc.semaphore("d_%s_%d" % s))

        def pe_pe(a, b):
            return a["eng"] == "pe" and b["eng"] == "pe" and not a["dma"] and not b["dma"]

        signaled = [False] * n
        for op in ops:
            for d in op["deps"]:
                if pe_pe(ops[d], op):
                    continue
                signaled[d] = True
        cnt = {}
        sig = [None] * n
        for i, op in enumerate(ops):
            if op["dma"]:
                key = op["slot"]
                cnt[key] = cnt.get(key, 0) + 16
                sig[i] = (key, cnt[key], 16)
            elif signaled[i]:
                key = op["eng"]
                cnt[key] = cnt.get(key, 0) + 1
                sig[i] = (key, cnt[key], 1)
        know = {}
        vc = [None] * n
        waits = [None] * n
        nw = 0
        for i, op in enumerate(ops):
            E = op["eng"]
            K = know.setdefault(E, {})
            best = {}
            for d in sorted(op["deps"], reverse=True):
                if pe_pe(ops[d], op):
                    continue
                key, val, _ = sig[d]
                if K.get(key, 0) >= val:
                    continue
                if best.get(key, 0) < val:
                    best[key] = val
                for k2, v2 in vc[d].items():
                    if K.get(k2, 0) < v2:
                        K[k2] = v2
            waits[i] = list(best.items())
            nw += len(waits[i])
            v = dict(K)
            if sig[i] is not None:
                key, val, _ = sig[i]
                v[key] = val
            vc[i] = v
        self.stats = dict(n_ops=n, n_waits=nw, n_sig=sum(1 for s in sig if s))
        per = {}
        for i, op in enumerate(ops):
            per.setdefault(op["eng"], []).append(i)
        self.stats["per_engine"] = {k: len(v) for k, v in per.items()}

        def run(engobj, name):
            for i in per.get(name, ()):
                op = ops[i]
                for key, val in waits[i]:
                    engobj.wait_ge(sems[key], val)
                ins = op["fn"](engobj)
                if sig[i] is not None:
                    key, val, inc = sig[i]
                    ins.then_inc(sems[key], inc)

        with nc.Block() as block:
            @block.sync
            def _(e):
                run(e, "sp")

            @block.tensor
            def _(e):
                run(e, "pe")

            @block.scalar
            def _(e):
                run(e, "act")

            @block.vector
            def _(e):
                run(e, "dve")

            @block.gpsimd
            def _(e):
                run(e, "pool")
        es.close()


def _win_perm():
    off = dict(q_n=0, kc=512, vc=640, ksl=768, vsl=896, kw=1024, vw=1152, gl=1280,
               q_d=1304, k_d=1816, v_d=1880, qi=1944, ki=2200, wi=2264)

    def head(name, i):
        return list(range(off[name] + 64 * i, off[name] + 64 * (i + 1)))

    nsa = [("q_n", i) for i in range(8)] + [("kc", 0), ("kc", 1), ("ksl", 0), ("ksl", 1), ("kw", 0), ("kw", 1)]
    dsa = [("q_d", i) for i in range(8)] + [("k_d", 0), ("ki", 0)] + [("qi", i) for i in range(4)]
    cols = []
    for p in range(14):
        cols += head(*nsa[p]) + head(*dsa[p])
    cols += head("vc", 0) + head("vc", 1) + head("vsl", 0) + head("vsl", 1) + head("vw", 0) + head("vw", 1)
    cols += head("v_d", 0)
    cols += list(range(off["gl"], off["gl"] + 24)) + list(range(off["wi"], off["wi"] + 4))
    assert len(cols) == D_IN and len(set(cols)) == D_IN
    return np.array(cols)


def _consts():
    bf = ml_dtypes.bfloat16
    c = {}
    eye = np.eye(128, dtype=np.float32)
    c["c_i4"] = np.tile(eye, (1, 4)).astype(bf)
    q = np.arange(128)[:, None]
    k = np.arange(128)[None, :]
    c["c_causal"] = np.where(k <= q, 0.0, NEGB).astype(bf)
    c["c_band"] = np.where(k > q, 0.0, NEGB).astype(bf)
    c["c_causalf"] = np.where(k <= q, 0.0, -1e30).astype(np.float32)
    t = (np.arange(NT)[None, :, None] * 128 + np.arange(128)[:, None, None])
    n = np.arange(128)[None, None, :]
    c["c_cmpbias"] = np.where((16 * n + 31 <= t) & (n < 127), 0.0, NEGB).astype(bf)
    j = np.arange(32)[None, None, :]
    cur = t // 64
    forced = (j == 0) | (j == cur) | (j == cur - 1)
    fb = np.where(j > cur, -100.0, np.where(forced, 100.0, 0.0))
    c["c_forceb"] = fb.astype(np.float32)
    cs = np.arange(128) * 16
    ce = cs + 31
    ss_ = np.arange(32) * 64
    se = ss_ + 63
    ov = ((cs[:, None] <= se[None, :]) & (ce[:, None] >= ss_[None, :]) & (np.arange(128)[:, None] < 127))
    vca = np.zeros((128, 2, 112), np.float32)
    vca[:, :, 64] = 1.0
    vca[:, :, 65:97] = ov[:, None, :]
    c["c_vca"] = vca.astype(bf)
    inv = (np.float32(10000.0) ** (-np.arange(32, dtype=np.float32) / np.float32(32))).astype(np.float32)
    misc = np.zeros((128, 64), np.float32)
    misc[:, 0:32] = inv[None, :]
    misc[:, 32] = -math.pi
    misc[:, 33:33 + NBIS + 1] = (2.0 ** -(np.arange(NBIS + 1) + 1.0))[None, :]
    misc[:, 50] = 1e-30
    c["c_misc"] = misc
    return c


class _Stop(Exception):
    pass


def build_program(nseq=SEQ_PER_CORE, debug=None, stop=None, stop_tile=0, dumps=()):
    nc = bass.Bass("TRN2", target_bir_lowering=False)
    S = Sched(nc)
    add = S.add

    def cut(name, tile=None):
        if stop == name and (tile is None or tile == stop_tile):
            S.stopped = True

    def dump(name, ap, keys, tile=None):
        if name in dumps and (tile is None or tile == stop_tile):
            shp = list(ap.shape)
            d_ = nc.dram_tensor("dbg_" + name, shp, ap.dtype, kind="ExternalOutput").ap()
            add("sp", lambda e: e.dma_start(out=d_, in_=ap), r=list(keys), w=["dbg_" + name], dma=True)

    def din(name, shape, dt=F32):
        return nc.dram_tensor(name, list(shape), dt, kind="ExternalInput").ap()

    x_d = din("x", [SEQ_PER_CORE, T, D])
    cT_d = din("cT", [128, 8, 4])
    pos_d = din("posT", [128, 4, NT], I32)
    wada_d = din("w_ada", [D, 6 * D])
    bada_d = din("b_ada", [1, 6 * D])
    badac_d = din("b_adac", [128, 48])
    gcol_d = din("gcol", [128, 2, 8])
    grow_d = din("grow", [1, 2, D])
    win_d = din("w_in", [D, D_IN])
    pe_d = din("peT", [128, 2, 32])
    w1k_d = din("w1k", [64, 32, 128])
    w1v_d = din("w1v", [128, 32, 128])
    w2_d = din("w2", [128, 2, 64])
    wout_d = din("w_out", [D, D])
    wup_d = din("w_up", [D, DFF])
    wdn_d = din("w_down", [DFF, D])
    consts = _consts()
    cd = {k: din(k, v.shape, BF16 if v.dtype == ml_dtypes.bfloat16 else F32) for k, v in consts.items()}
    out_d = nc.dram_tensor("out", [SEQ_PER_CORE, T, D], F32, kind="ExternalOutput").ap()
    S.bar_fn = lambda e: e.dma_start(out=bar_d[1:2, :], in_=cd["c_misc"][0:1, :])
    gscr_d = nc.dram_tensor("gscr", [4, 2, D], F32, kind="Internal").ap()
    bar_d = nc.dram_tensor("bar_scr", [2, 64], F32, kind="Internal").ap()
    dbg_d = {}
    if debug:
        for name, shape in debug.items():
            dbg_d[name] = nc.dram_tensor("dbg_" + name, list(shape), F32, kind="ExternalOutput").ap()

    top = ExitStack()

    def sbuf(es, name, shape, dt):
        return es.enter_context(nc.sbuf_tensor("s_" + name, list(shape), dt))

    pb = [top.enter_context(nc.psum_tensor("pb%d" % i, [128, 512], F32)) for i in range(8)]

    def pbf(i):
        return pb[i][:].bitcast(BF16)

    def bc(ap, shape):
        return ap.to_broadcast(list(shape))

    P = top
    i4 = sbuf(P, "i4", [128, 512], BF16)
    causal = sbuf(P, "causal", [128, 128], BF16)
    band = sbuf(P, "band", [128, 128], BF16)
    causalf = sbuf(P, "causalf", [128, 128], F32)
    misc = sbuf(P, "misc", [128, 64], F32)
    AB = sbuf(P, "AB", [128, 4, 4, 8], F32)
    for name, t_ in (("c_i4", i4), ("c_causal", causal), ("c_band", band), ("c_causalf", causalf),
                     ("c_misc", misc)):
        add("sp", lambda e, t_=t_, name=name: e.dma_start(out=t_[:], in_=cd[name]), w=[name], dma=True)
    CONST_R = ["c_i4", "c_causal", "c_band", "c_causalf", "c_misc"]
    invf = misc[:, 0:32]
    negpi = misc[:, 32:33]

    def _build_body():
        with ExitStack() as es0:
            cT = sbuf(es0, "cT", [128, 8, 4], F32)
            badar = sbuf(es0, "badar", [4, 6 * D], F32)
            badac = sbuf(es0, "badac", [128, 48], F32)
            gcol = sbuf(es0, "gcol", [128, 2, 8], F32)
            growb = sbuf(es0, "growb", [4, 2, D], F32)
            modT = sbuf(es0, "modT", [128, 4, 8, 4], F32)
            wst = [sbuf(es0, "wst%d" % i, [128, 8, 512], F32) for i in range(2)]
            add("sp", lambda e: e.dma_start(out=cT[:], in_=cT_d), w=["cT"], dma=True)
            add("sp", lambda e: e.dma_start(out=badar[:], in_=bc(bada_d, [4, 6 * D])), w=["badar"], dma=True)
            add("sp", lambda e: e.dma_start(out=badac[:], in_=badac_d), w=["badac"], dma=True)
            add("sp", lambda e: e.dma_start(out=gcol[:], in_=gcol_d), w=["gcol"], dma=True)
            add("sp", lambda e: e.dma_start(out=growb[:], in_=bc(grow_d, [4, 2, D])), w=["growb"], dma=True)
            wada_v = wada_d.rearrange("(k p) n -> p k n", p=128)
            colmap = {0: 0, 1: 0, 2: 1, 3: 1, 6: 2, 7: 2, 8: 3, 9: 3}
            rowmap = {4: (0, 0), 5: (0, 1), 10: (1, 0), 11: (1, 1)}
            for cc in range(12):
                st = wst[cc % 2]
                key = "wst%d" % (cc % 2)
                add("sp", lambda e, st=st, cc=cc: e.dma_start(out=st[:], in_=wada_v[:, :, cc * 512:(cc + 1) * 512]),
                    w=[key], dma=True)
                if cc in colmap:
                    for q in range(4):
                        jj = colmap[cc] * 8 + (cc % 2) * 4 + q
                        for k in range(8):
                            add("pe", lambda e, st=st, k=k, q=q, jj=jj: e.matmul(pb[2][:, jj * 4:(jj + 1) * 4], lhsT=st[:, k, q * 128:(q + 1) * 128],
                                                                                 rhs=cT[:, k, :], start=(k == 0), stop=(k == 7)),
                                r=["cT", key], w=["pb2"])
                else:
                    gi, half = rowmap[cc]
                    for k in range(8):
                        add("pe", lambda e, st=st, k=k: e.matmul(pb[0][0:4, :], lhsT=cT[:, k, :], rhs=st[:, k, :],
                                                                 start=(k == 0), stop=(k == 7)),
                            r=["cT", key], w=["pb0"])
                    hs_ = slice(half * 512, (half + 1) * 512)
                    add("dve", lambda e, cc=cc: e.tensor_tensor(out=badar[:, cc * 512:(cc + 1) * 512], in0=pb[0][0:4, :],
                                                                 in1=badar[:, cc * 512:(cc + 1) * 512], op=ALU.add),
                        r=["pb0", "badar"], w=["badar"])
                    add("dve", lambda e, cc=cc, gi=gi, hs_=hs_: e.tensor_tensor(out=growb[:, gi, hs_], in0=growb[:, gi, hs_],
                                                                                  in1=badar[:, cc * 512:(cc + 1) * 512], op=ALU.mult),
                        r=["badar", "growb"], w=["growb"])
            for a_, ch in enumerate((0, 1, 3, 4)):
                add("dve", lambda e, a_=a_, ch=ch: e.tensor_tensor(out=modT[:, a_, :, :], in0=pb[2][:, a_ * 32:(a_ + 1) * 32].rearrange("p (k b) -> p k b", b=4),
                                                                    in1=bc(badac[:, ch * 8:(ch + 1) * 8].unsqueeze(2), [128, 8, 4]), op=ALU.add),
                    r=["pb2", "badac"], w=["modT"])
            for b in range(4):
                for (dst, src, gi) in ((0, 1, 0), (2, 3, 1)):
                    add("dve", lambda e, b=b, dst=dst, src=src: e.tensor_scalar(out=AB[:, dst, b, :], in0=modT[:, src, :, b],
                                                                                 scalar1=1.0, scalar2=None, op0=ALU.add),
                        r=["modT"], w=["AB"])
                    add("dve", lambda e, b=b, dst=dst, gi=gi: e.tensor_tensor(out=AB[:, dst, b, :], in0=AB[:, dst, b, :],
                                                                               in1=gcol[:, gi, :], op=ALU.mult),
                        r=["gcol"], w=["AB"])
                for (dst, src) in ((1, 0), (3, 2)):
                    add("dve", lambda e, b=b, dst=dst, src=src: e.tensor_copy(out=AB[:, dst, b, :], in_=modT[:, src, :, b]),
                        r=["modT"], w=["AB"])
            add("sp", lambda e: e.dma_start(out=gscr_d, in_=growb[:]), r=["growb"], w=["gscr"], dma=True)
            dump("growb", growb[:].rearrange("p a d -> p (a d)"), ["growb"])
            dump("AB", AB[:].rearrange("p a b k -> p (a b k)"), ["AB"])
            S.barrier()
        cut("setup")

        with ExitStack() as es1:
            M = es1
            Win = sbuf(M, "Win", [128, 8, D_IN], BF16)
            Wout = sbuf(M, "Wout", [128, 8, D], BF16)
            W1k = sbuf(M, "W1k", [64, 32, 128], BF16)
            W1v = sbuf(M, "W1v", [128, 32, 128], BF16)
            W2 = sbuf(M, "W2", [128, 2, 64], BF16)
            peT = sbuf(M, "peT", [128, 2, 32], BF16)
            bT = sbuf(M, "bT", [128, 2], F32)
            cmpbias = sbuf(M, "cmpbias", [128, NT, 128], BF16)
            forceb = sbuf(M, "forceb", [128, NT, 32], F32)
            VCa = sbuf(M, "VCa", [128, 2, 112], BF16)
            add("sp", lambda e: e.dma_start(out=cmpbias[:], in_=cd["c_cmpbias"]), w=["c_cmpbias"], dma=True)
            add("sp", lambda e: e.dma_start(out=forceb[:], in_=cd["c_forceb"]), w=["c_forceb"], dma=True)
            add("sp", lambda e: e.dma_start(out=VCa[:], in_=cd["c_vca"]), w=["VCa"], dma=True)

            with ExitStack() as es_w:
                wst = [sbuf(es_w, "wstm%d" % i, [128, 8, 512], F32) for i in range(2)]
                cnt = [0]

                def load_cast(dst_fn, src_ap, shape, eng="pool"):
                    i = cnt[0] % 2
                    cnt[0] += 1
                    st = wst[i]
                    key = "wstm%d" % i
                    a, n_ = shape[1], shape[2]
                    view = st[0:shape[0], :, :].rearrange("p a n -> p (a n)")[:, 0:a * n_].rearrange("p (a n) -> p a n", a=a)
                    add("sp", lambda e: e.dma_start(out=view, in_=src_ap), w=[key], dma=True)
                    add(eng, lambda e: e.tensor_copy(out=dst_fn, in_=view), r=[key], w=["weights"])

                win_v = win_d.rearrange("(k p) n -> p k n", p=128)
                c0 = 0
                while c0 < D_IN:
                    cw = min(512, D_IN - c0)
                    load_cast(Win[:, :, c0:c0 + cw], win_v[:, :, c0:c0 + cw], [128, 8, cw], eng="pool" if (c0 // 512) % 2 else "dve")
                    c0 += cw
                wout_v = wout_d.rearrange("(k p) n -> p k n", p=128)
                for c0 in range(0, D, 512):
                    load_cast(Wout[:, :, c0:c0 + 512], wout_v[:, :, c0:c0 + 512], [128, 8, 512])
                for l0 in range(0, 32, 16):
                    load_cast(W1k[:, l0:l0 + 16, :], w1k_d[:, l0:l0 + 16, :], [64, 16, 128])
                    load_cast(W1v[:, l0:l0 + 16, :], w1v_d[:, l0:l0 + 16, :], [128, 16, 128])
                load_cast(W2[:], w2_d, [128, 2, 64])
                load_cast(peT[:], pe_d, [128, 2, 32])
                for l in range(32):
                    add("pe", lambda e, l=l: e.matmul(pb[0][:, 0:1], lhsT=W1k[0:64, l, :], rhs=peT[0:64, 0, l:l + 1],
                                                      start=(l == 0), stop=(l == 31)), r=["weights"], w=["pb0"])
                for l in range(32):
                    add("pe", lambda e, l=l: e.matmul(pb[1][:, 0:1], lhsT=W1v[0:64, l, :], rhs=peT[0:64, 1, l:l + 1],
                                                      start=(l == 0), stop=(l == 31)), r=["weights"], w=["pb1"])
                add("dve", lambda e: e.tensor_copy(out=bT[:, 0:1], in_=pb[0][:, 0:1]), r=["pb0"], w=["bT"])
                add("dve", lambda e: e.tensor_copy(out=bT[:, 1:2], in_=pb[1][:, 0:1]), r=["pb1"], w=["bT"])
                dump("bT", bT[:], ["bT"])
                S.barrier()
            cut("weights")

            KT = sbuf(M, "KT", [128, 6, T], BF16)
            VA = sbuf(M, "VA", [128, NT, 5, 80], BF16)
            VCT = sbuf(M, "VCT", [128, T], BF16)
            GH = sbuf(M, "GH", [128, 2, 2, 128], BF16)
            KCT = sbuf(M, "KCT", [64, 2, 128], BF16)
            cosT = sbuf(M, "cosT", [128, NT, 32], F32)
            sinT = sbuf(M, "sinT", [128, NT, 32], F32)
            posf = sbuf(M, "posf", [128, NT], F32)
            posi = sbuf(M, "posi", [128, 4, NT], I32)
            G1 = sbuf(M, "G1", [128, D], F32)
            xt = [sbuf(M, "xt%d" % i, [128, D], F32) for i in range(2)]
            xn = sbuf(M, "xn", [128, D], BF16)
            st4 = sbuf(M, "st4", [128, 8], F32)
            hT = sbuf(M, "hT", [128, 8, 128], BF16)
            rq = sbuf(M, "rq", [128, 28, 2, 32], BF16)
            rtmp = sbuf(M, "rtmp", [128, 4, 8, 32], F32)
            vct = sbuf(M, "vct", [128, 128], BF16)
            qT = sbuf(M, "qT", [128, 14, 128], BF16)
            gate = sbuf(M, "gate", [128, 8, 3], F32)
            WA = sbuf(M, "WA", [128, 4], F32)
            WS = sbuf(M, "WS", [128, 4], F32)
            hb = sbuf(M, "hb", [128, 4, 8], F32)
            Ygk = sbuf(M, "Ygk", [64, 2, 32, 8], BF16)
            Ygv = sbuf(M, "Ygv", [128, 32, 8], BF16)
            hu = sbuf(M, "hu", [128, 4, 8], F32)
            PT = sbuf(M, "PT", [128, 8, 512], BF16)
            PTc = sbuf(M, "PTc", [128, 512], BF16)
            rc = sbuf(M, "rc", [128, 8], F32)
            imp = sbuf(M, "imp", [128, 2, 32], F32)
            tk = sbuf(M, "tk", [128, 80], F32)
            selb = sbuf(M, "selb", [128, 2, 32], BF16)
            selx = sbuf(M, "selx", [128, T], BF16)
            score = sbuf(M, "score", [128, T], F32)
            junk2 = sbuf(M, "junk2", [128, T], BF16)
            atmp = [sbuf(M, "atmp%d" % i, [128, 512], F32) for i in range(2)]
            maskb = sbuf(M, "maskb", [128, T], BF16)
            bis = sbuf(M, "bis", [128, 32], F32)
            wtab = sbuf(M, "wtab", [128, NBIS + 1], F32)
            Ot = sbuf(M, "Ot", [128, D], F32)
            otmp = sbuf(M, "otmp", [128, 256], F32)
            accs = [sbuf(M, "accs%d" % i, [128, 4, 97], F32) for i in range(2)]
            Otb = sbuf(M, "Otb", [128, D], BF16)
            oT = sbuf(M, "oT", [128, 8, 128], BF16)
            x1t = sbuf(M, "x1t", [128, D], F32)

            add("sp", lambda e: e.dma_start(out=posi[:], in_=pos_d), w=["posi"], dma=True)
            add("pool", lambda e: e.memset(VA[:, :, :, 64:65], 1.0), w=[("VA", t_) for t_ in range(NT)])
            add("pool", lambda e: e.memset(qT[:], 0.0), w=["qT"])
            add("pool", lambda e: e.memset(KT[:], 0.0), w=[("KT", t_) for t_ in range(NT)] + [("KTd", t_) for t_ in range(NT)])

            def rstd_from_ss(ss_ap, out_ap, rkeys, wkey):
                add("dve", lambda e: e.tensor_scalar(out=out_ap, in0=ss_ap, scalar1=1.0 / D, scalar2=EPS,
                                                     op0=ALU.mult, op1=ALU.add), r=rkeys, w=[wkey])
                add("act", lambda e: e.activation(out=out_ap, in_=out_ap, func=AF.Ln), r=[wkey], w=[wkey])
                add("act", lambda e: e.activation(out=out_ap, in_=out_ap, func=AF.Exp, scale=-0.5), r=[wkey], w=[wkey])

            ev_i = [0]

            def pv_evac(bank, bkey, width, dst_cols, gate_br, g, first):
                ai = ev_i[0] % 2
                ev_i[0] += 1
                asb = accs[ai]
                akey = "accs%d" % ai
                v = bank[:, 0:512].rearrange("p (h w) -> p h w", h=4)
                add("act", lambda e: e.activation(out=asb[:, :, 0:width], in_=v[:, :, 0:width], func=AF.Copy), r=[bkey], w=[akey])
                add("act", lambda e: e.activation(out=rc[:, 0:4], in_=asb[:, :, 64], func=AF.Ln, bias=misc[:, 50:51], scale=1.0),
                    r=[akey, "c_misc"], w=["rc"])
                add("act", lambda e: e.activation(out=rc[:, 0:4], in_=rc[:, 0:4], func=AF.Exp, scale=-1.0), r=["rc"], w=["rc"])
                if gate_br is not None:
                    add("pool", lambda e: e.tensor_tensor(out=rc[:, 4:8], in0=rc[:, 0:4], in1=gate[:, 4 * g:4 * g + 4, gate_br],
                                                          op=ALU.mult), r=["rc", "gate"], w=["rcg"])
                    rcs, rkey = rc[:, 4:8], "rcg"
                else:
                    rcs, rkey = rc[:, 0:4], "rc"
                dst = Ot[:, dst_cols:dst_cols + 256].rearrange("p (h w) -> p h w", h=4)
                if first:
                    add("pool", lambda e: e.tensor_tensor(out=dst, in0=asb[:, :, 0:64], in1=bc(rcs.unsqueeze(2), [128, 4, 64]), op=ALU.mult),
                        r=[akey, rkey], w=["Ot"])
                else:
                    o3 = otmp[:].rearrange("p (h w) -> p h w", h=4)
                    add("pool", lambda e: e.tensor_tensor(out=o3, in0=asb[:, :, 0:64], in1=bc(rcs.unsqueeze(2), [128, 4, 64]), op=ALU.mult),
                        r=[akey, rkey], w=["otmp"])
                    add("pool", lambda e: e.tensor_tensor(out=dst, in0=dst, in1=o3, op=ALU.add), r=["otmp"], w=["Ot"])
                return asb, akey

            pt_i = [0]
            sbank = [4, 5]
            sb_i = [0]
            obank = [6, 7]
            ob_i = [0]

            def attention(t, kts, kslice_fn, q_rhs, mask_fn, v_fn, width=65):
                ob = obank[ob_i[0] % 2]
                ob_i[0] += 1
                okey = "pb%d" % ob
                first_pv = [True]

                def emit_pv(idx, kt):
                    j = idx % 8
                    rhs, vkeys = v_fn(kt)
                    for h in range(4):
                        st_flag = first_pv[0]
                        first_pv[0] = False
                        add("pe", lambda e, ob=ob, h=h, j=j, rhs=rhs, st_flag=st_flag: e.matmul(
                            pb[ob][:, h * 128:h * 128 + width], lhsT=PT[:, j, h * 128:(h + 1) * 128], rhs=rhs,
                            start=st_flag, stop=False, skip_group_check=True),
                            r=[("PT", j)] + vkeys, w=[okey])

                for idx, kt in enumerate(kts):
                    j = idx % 8
                    sbk = sbank[sb_i[0] % 2]
                    sb_i[0] += 1
                    skey = "pb%d" % sbk
                    lhs, kkeys = kslice_fn(kt)
                    m = mask_fn(kt)
                    add("pe", lambda e, sbk=sbk, lhs=lhs, m=m: e.matmul(pb[sbk][:, :], lhsT=lhs, rhs=q_rhs, start=True, stop=(m is None)),
                        r=kkeys + ["qT"], w=[skey])
                    if m is not None:
                        mlhs, mkeys = m
                        add("pe", lambda e, sbk=sbk, mlhs=mlhs: e.matmul(pb[sbk][:, :], lhsT=mlhs, rhs=i4[:, :], start=False, stop=True),
                            r=mkeys + ["c_i4"], w=[skey])
                    add("act", lambda e, sbk=sbk, j=j: e.activation(out=PT[:, j, :], in_=pb[sbk][:, :], func=AF.Exp, scale=0.125),
                        r=[skey], w=[("PT", j)])
                    if idx >= 1:
                        emit_pv(idx - 1, kts[idx - 1])
                emit_pv(len(kts) - 1, kts[-1])
                return pb[ob], okey

            for b in range(nseq):
                add("dve", lambda e, b=b: e.tensor_copy(out=posf[:], in_=posi[:, b, :]), r=["posi"], w=["posf"])
                ang = score[:, 0:NT * 32].rearrange("p (t j) -> p t j", t=NT)
                angk = score[:, 512:512 + NT * 32].rearrange("p (t j) -> p t j", t=NT)
                angi = junk2[:, 0:NT * 64].bitcast(I32).rearrange("p (t j) -> p t j", t=NT)
                for (dstT, shift) in ((sinT, 0.5), (cosT, 0.75)):
                    add("dve", lambda e: e.tensor_tensor(out=ang, in0=bc(invf.unsqueeze(1), [128, NT, 32]),
                                                         in1=bc(posf[:].unsqueeze(2), [128, NT, 32]), op=ALU.mult),
                        r=["posf", "c_misc"], w=["score"])
                    add("dve", lambda e, shift=shift: e.tensor_scalar(out=ang, in0=ang, scalar1=1.0 / (2.0 * math.pi), scalar2=shift,
                                                                       op0=ALU.mult, op1=ALU.add), r=["score"], w=["score"])
                    add("dve", lambda e: e.tensor_copy(out=angi, in_=ang), r=["score"], w=["junk2"])
                    add("dve", lambda e: e.tensor_copy(out=angk, in_=angi), r=["junk2"], w=["score"])
                    add("dve", lambda e: e.tensor_tensor(out=ang, in0=ang, in1=angk, op=ALU.subtract), r=["score"], w=["score"])
                    add("dve", lambda e: e.scalar_tensor_tensor(out=ang, in0=ang, scalar=0.0, in1=ang, op0=ALU.is_lt, op1=ALU.add),
                        r=["score"], w=["score"])
                    add("act", lambda e, dstT=dstT: e.activation(out=dstT[:], in_=ang, func=AF.Sin, bias=negpi, scale=2.0 * math.pi),
                        r=["score", "c_misc"], w=["rope_tab"])
                add("sp", lambda e, b=b: e.dma_start(out=G1[:], in_=bc(gscr_d[b:b + 1, 0, :], [128, D])), r=["gscr"], w=["G1"], dma=True)
                add("pool", lambda e: e.memset(GH[:], 0.0), w=["GH"])

                for t in range(NT):
                    xb = xt[t % 2]
                    xkey = "xt%d" % (t % 2)
                    tsl = slice(t * 128, (t + 1) * 128)
                    add("sp", lambda e, b=b, tsl=tsl, xb=xb: e.dma_start(out=xb[:], in_=x_d[b, tsl, :]), w=[xkey], dma=True)
                    add("dve", lambda e: e.memset(st4[:, 0:1], 0.0), w=["ss0"])
                    add("act", lambda e, xb=xb: e.activation(out=xn[:], in_=xb[:], func=AF.Square, accum_out=st4[:, 0:1]),
                        r=[xkey], w=["xn", "ss0"])
                    rstd_from_ss(st4[:, 0:1], st4[:, 1:2], ["ss0"], "rstd")
                    add("act", lambda e, xb=xb: e.activation(out=xn[:], in_=xb[:], func=AF.Copy, scale=st4[:, 1:2]),
                        r=[xkey, "rstd"], w=["xn"])
                    for k in range(8):
                        add("pe", lambda e, k=k: e.transpose(out=pbf(0)[:, k * 128:(k + 1) * 128], in_=xn[:, k * 128:(k + 1) * 128],
                                                             identity=i4[:, 0:128]), r=["xn", "c_i4"], w=["pb0"])
                    for k in range(8):
                        add("dve", lambda e, k=k, b=b: e.tensor_scalar(out=hT[:, k, :], in0=pbf(0)[:, k * 128:(k + 1) * 128],
                                                                        scalar1=AB[:, 0, b, k:k + 1], scalar2=AB[:, 1, b, k:k + 1],
                                                                        op0=ALU.mult, op1=ALU.add), r=["pb0", "AB"], w=["hT"])
                    chunks = [(0, 512), (512, 512), (1024, 512), (1536, 256), (1792, 476)]
                    for ci, (c0, cw) in enumerate(chunks):
                        bk = 1 + (ci % 2)
                        bkey = "pb%d" % bk
                        for k in range(8):
                            add("pe", lambda e, k=k, bk=bk, c0=c0, cw=cw: e.matmul(pb[bk][:, 0:cw], lhsT=hT[:, k, :], rhs=Win[:, k, c0:c0 + cw],
                                                                                   start=(k == 0), stop=(k == 7)), r=["hT", "weights"], w=[bkey])
                        if ci < 4:
                            nh = cw // 64
                            h0 = c0 // 64
                            pv = pb[bk][:, 0:cw].rearrange("p (h two j) -> p h two j", two=2, j=32)
                            cs_ = bc(cosT[:, t, :].unsqueeze(1), [128, nh, 32])
                            sn_ = bc(sinT[:, t, :].unsqueeze(1), [128, nh, 32])
                            add("dve", lambda e, pv=pv, cs_=cs_, nh=nh: e.tensor_tensor(out=rtmp[:, 0, 0:nh, :], in0=pv[:, :, 0, :], in1=cs_, op=ALU.mult),
                                r=[bkey, "rope_tab"], w=["rt0"])
                            add("dve", lambda e, pv=pv, sn_=sn_, nh=nh: e.tensor_tensor(out=rtmp[:, 1, 0:nh, :], in0=pv[:, :, 1, :], in1=sn_, op=ALU.mult),
                                r=[bkey, "rope_tab"], w=["rt1"])
                            add("dve", lambda e, pv=pv, cs_=cs_, nh=nh: e.tensor_tensor(out=rtmp[:, 2, 0:nh, :], in0=pv[:, :, 1, :], in1=cs_, op=ALU.mult),
                                r=[bkey, "rope_tab"], w=["rt2"])
                            add("dve", lambda e, pv=pv, sn_=sn_, nh=nh: e.tensor_tensor(out=rtmp[:, 3, 0:nh, :], in0=pv[:, :, 0, :], in1=sn_, op=ALU.mult),
                                r=[bkey, "rope_tab"], w=["rt3"])
                            add("pool", lambda e, nh=nh, h0=h0: e.tensor_tensor(out=rq[:, h0:h0 + nh, 0, :], in0=rtmp[:, 0, 0:nh, :], in1=rtmp[:, 1, 0:nh, :],
                                                                                 op=ALU.subtract), r=["rt0", "rt1"], w=["rq"])
                            add("pool", lambda e, nh=nh, h0=h0: e.tensor_tensor(out=rq[:, h0:h0 + nh, 1, :], in0=rtmp[:, 2, 0:nh, :], in1=rtmp[:, 3, 0:nh, :],
                                                                                 op=ALU.add), r=["rt2", "rt3"], w=["rq"])
                        else:
                            pvv = pb[bk]
                            add("act", lambda e, pvv=pvv: e.activation(out=vct[:], in_=pvv[:, 0:128], func=AF.Copy), r=[bkey], w=["vct"])
                            add("act", lambda e, pvv=pvv, t=t: e.activation(out=VA[:, t, :, 0:64], in_=pvv[:, 128:448].rearrange("p (s d) -> p s d", s=5),
                                                                             func=AF.Copy), r=[bkey], w=[("VA", t)])
                            g24 = gate[:].rearrange("p h c -> p (h c)")
                            add("act", lambda e, pvv=pvv: e.activation(out=g24, in_=pvv[:, 448:472], func=AF.Exp, scale=-1.0), r=[bkey], w=["gate"])
                            add("dve", lambda e: e.tensor_scalar(out=g24, in0=g24, scalar1=1.0, scalar2=None, op0=ALU.add), r=["gate"], w=["gate"])
                            add("dve", lambda e: e.reciprocal(out=g24, in_=g24), r=["gate"], w=["gate"])
                            add("dve", lambda e, pvv=pvv: e.tensor_scalar(out=WS[:], in0=pvv[:, 472:476], scalar1=0.0, scalar2=2.0,
                                                                           op0=ALU.is_ge, op1=ALU.mult), r=[bkey], w=["WS"])
                            add("dve", lambda e: e.tensor_scalar(out=WS[:], in0=WS[:], scalar1=-1.0, scalar2=None, op0=ALU.add), r=["WS"], w=["WS"])
                            add("dve", lambda e, pvv=pvv: e.scalar_tensor_tensor(out=WA[:], in0=pvv[:, 472:476], scalar=IDX_SCALE, in1=WS[:],
                                                                                  op0=ALU.mult, op1=ALU.mult), r=[bkey, "WS"], w=["WA"])
                    rq2 = rq[:].rearrange("p h two j -> p (h two j)")
                    for p_ in range(8):
                        add("pe", lambda e, p_=p_: e.transpose(out=pbf(3)[:, p_ * 128:(p_ + 1) * 128], in_=rq2[:, p_ * 128:(p_ + 1) * 128],
                                                               identity=i4[:, 0:128]), r=["rq", "c_i4"], w=["pb3"])
                    add("act", lambda e: e.activation(out=qT[:, 0:8, :], in_=pbf(3)[:, :].rearrange("p (s q) -> p s q", s=8), func=AF.Copy),
                        r=["pb3"], w=["qT"])
                    for p_ in range(8, 14):
                        add("pe", lambda e, p_=p_: e.transpose(out=pbf(3)[:, (p_ - 8) * 128:(p_ - 7) * 128], in_=rq2[:, p_ * 128:(p_ + 1) * 128],
                                                               identity=i4[:, 0:128]), r=["rq", "c_i4"], w=["pb3"])
                    p3 = pbf(3)[:, 0:768].rearrange("p (s q) -> p s q", s=6)
                    add("dve", lambda e, p3=p3, tsl=tsl: e.tensor_copy(out=KT[0:64, 0:6, tsl], in_=p3[0:64, :, :]), r=["pb3"], w=[("KT", t)])
                    add("act", lambda e, p3=p3, tsl=tsl: e.activation(out=KT[64:128, 0:2, tsl], in_=p3[64:128, 0:2, :], func=AF.Copy),
                        r=["pb3"], w=[("KTd", t)])
                    add("act", lambda e, p3=p3: e.activation(out=qT[64:128, 10:14, :], in_=p3[64:128, 2:6, :], func=AF.Copy), r=["pb3"], w=["qT"])
                    add("pe", lambda e: e.transpose(out=pbf(0)[:, 0:128], in_=vct[:], identity=i4[:, 0:128]), r=["vct", "c_i4"], w=["pb0"])
                    add("dve", lambda e, tsl=tsl: e.tensor_copy(out=VCT[:, tsl], in_=pbf(0)[:, 0:128]), r=["pb0"], w=[("VCT", t)])

                    dump("G1", G1[:], ["G1"], t)
                    dump("cosT", cosT[:].rearrange("p t j -> p (t j)"), ["rope_tab"], t)
                    dump("sinT", sinT[:].rearrange("p t j -> p (t j)"), ["rope_tab"], t)
                    dump("hT", hT[:].rearrange("p k q -> p (k q)"), ["hT"], t)
                    dump("rq", rq[:].rearrange("p h two j -> p (h two j)"), ["rq"], t)
                    dump("qT", qT[:].rearrange("p s q -> p (s q)"), ["qT"], t)
                    dump("gate", gate[:].rearrange("p h c -> p (h c)"), ["gate"], t)
                    dump("WA", WA[:], ["WA"], t)
                    dump("WS", WS[:], ["WS"], t)
                    dump("VA", VA[:, t, :, 0:65], [("VA", t)], t)
                    dump("KT", KT[:, :, tsl], [("KT", t), ("KTd", t)], t)
                    cut("P", t)
                    n0 = 0 if t == 0 else 8 * t - 1
                    nb = 7 if t == 0 else 8
                    kkeys = [("KT", tt) for tt in range(max(0, t - 1), t + 1)]
                    vkeys_ = [("VCT", tt) for tt in range(max(0, t - 1), t + 1)]
                    tok0 = 16 * n0
                    xk = KT[0:64, 0:2, tok0:tok0 + 16 * (nb + 1)].rearrange("p g (n l) -> p g n l", l=16)
                    xv = VCT[:, tok0:tok0 + 16 * (nb + 1)].rearrange("p (n l) -> p n l", l=16)
                    for lhi in range(2):
                        add("pool", lambda e, lhi=lhi, xk=xk, nb=nb: e.tensor_copy(out=Ygk[:, :, lhi * 16:(lhi + 1) * 16, 0:nb],
                                                                                    in_=xk[:, :, lhi:lhi + nb, :].rearrange("p g n l -> p g l n")),
                            r=kkeys, w=["Ygk"])
                        add("pool", lambda e, lhi=lhi, xv=xv, nb=nb: e.tensor_copy(out=Ygv[:, lhi * 16:(lhi + 1) * 16, 0:nb],
                                                                                    in_=xv[:, lhi:lhi + nb, :].rearrange("p n l -> p l n")),
                            r=vkeys_, w=["Ygv"])
                    for kv in range(2):
                        for g in range(2):
                            jj = kv * 2 + g
                            for l in range(32):
                                if kv == 0:
                                    lhs = W1k[0:64, l, :]
                                    rhs = Ygk[:, g, l, 0:nb]
                                    rk = ["Ygk"]
                                else:
                                    lhs = W1v[g * 64:(g + 1) * 64, l, :]
                                    rhs = Ygv[g * 64:(g + 1) * 64, l, 0:nb]
                                    rk = ["Ygv"]
                                cb = 2 if jj == 3 else 1
                                add("pe", lambda e, lhs=lhs, rhs=rhs, jj=jj, l=l, nb=nb, cb=cb: e.matmul(pb[cb][:, jj * 8:jj * 8 + nb], lhsT=lhs, rhs=rhs,
                                                                                                          start=(l == 0), stop=(l == 31)),
                                    r=rk + ["weights"], w=["pb%d" % cb])
                    cut("C0", t)
                    pch = pb[1][:, 0:32].rearrange("p (a n) -> p a n", a=4)
                    pch2 = pb[2][:, 0:32].rearrange("p (a n) -> p a n", a=4)
                    add("dve", lambda e: e.tensor_scalar(out=hb[:, 0:2, :], in0=pch[:, 0:2, :], scalar1=bT[:, 0:1], scalar2=None, op0=ALU.add),
                        r=["pb1", "bT"], w=["hb"])
                    add("dve", lambda e: e.tensor_scalar(out=hb[:, 2:3, :], in0=pch[:, 2:3, :], scalar1=bT[:, 1:2], scalar2=None, op0=ALU.add),
                        r=["pb1", "bT"], w=["hb"])
                    add("dve", lambda e: e.tensor_scalar(out=hb[:, 3:4, :], in0=pch2[:, 3:4, :], scalar1=bT[:, 1:2], scalar2=None, op0=ALU.add),
                        r=["pb2", "bT"], w=["hb"])
                    add("dve", lambda e: e.tensor_tensor(out=hu[:], in0=hb[:], in1=hb[:], op=ALU.mult), r=["hb"], w=["hu"])
                    add("dve", lambda e: e.tensor_scalar(out=hu[:], in0=hu[:], scalar1=0.044715, scalar2=1.0, op0=ALU.mult, op1=ALU.add), r=["hu"], w=["hu"])
                    add("dve", lambda e: e.tensor_tensor(out=hu[:], in0=hu[:], in1=hb[:], op=ALU.mult), r=["hu", "hb"], w=["hu"])
                    add("act", lambda e: e.activation(out=hu[:], in_=hu[:], func=AF.Exp, scale=-2.0 * GELU_C), r=["hu"], w=["hu"])
                    add("dve", lambda e: e.tensor_scalar(out=hu[:], in0=hu[:], scalar1=1.0, scalar2=None, op0=ALU.add), r=["hu"], w=["hu"])
                    add("dve", lambda e: e.reciprocal(out=hu[:], in_=hu[:]), r=["hu"], w=["hu"])
                    hb4 = hb[:].rearrange("p (kv g) n -> p kv g n", kv=2)
                    hu4 = hu[:].rearrange("p (kv g) n -> p kv g n", kv=2)
                    add("dve", lambda e, n0=n0, nb=nb: e.tensor_tensor(out=GH[:, :, :, n0:n0 + nb], in0=hb4[:, :, :, 0:nb], in1=hu4[:, :, :, 0:nb], op=ALU.mult),
                        r=["hb", "hu"], w=["GH"])
                    dump("GH1", GH[:].rearrange("p a g n -> p (a g n)"), ["GH"], t)
                    cut("C1", t)
                    for g in range(2):
                        add("pe", lambda e, g=g: e.matmul(pb[2][0:64, g * 128:(g + 1) * 128], lhsT=W2[:, 0, :], rhs=GH[:, 0, g, :], start=True, stop=True),
                            r=["GH", "weights"], w=["pb2"])
                    add("act", lambda e: e.activation(out=KCT[:, :, :], in_=pb[2][0:64, 0:256].rearrange("p (g n) -> p g n", g=2), func=AF.Copy),
                        r=["pb2"], w=["KCT"])
                    for g in range(2):
                        add("pe", lambda e, g=g: e.matmul(pb[3][:, g * 64:(g + 1) * 64], lhsT=GH[:, 1, g, :], rhs=W2[:, 1, :], start=True, stop=True),
                            r=["GH", "weights"], w=["pb3"])
                    add("act", lambda e: e.activation(out=VCa[:, :, 0:64], in_=pb[3][:, 0:128].rearrange("p (g d) -> p g d", g=2), func=AF.Copy),
                        r=["pb3"], w=["VCa"])

                    dump("GH", GH[:].rearrange("p a g n -> p (a g n)"), ["GH"], t)
                    dump("KCT", KCT[:].rearrange("p g n -> p (g n)"), ["KCT"], t)
                    dump("VCa", VCa[:, :, 0:97], ["VCa"], t)
                    cut("C", t)
                    nkeys = (t + 1) * 128
                    if t >= 2:
                        for c0 in range(0, nkeys, 512):
                            cw = min(512, nkeys - c0)
                            kk = [("KTd", kt) for kt in range(c0 // 128, (c0 + cw) // 128)]
                            for h in range(4):
                                bk = 1 + (h % 2)
                                bkey = "pb%d" % bk
                                at = atmp[h % 2]
                                akey = "atmp%d" % (h % 2)
                                add("pe", lambda e, bk=bk, h=h, c0=c0, cw=cw: e.matmul(pb[bk][:, 0:cw], lhsT=qT[64:128, 10 + h, :], rhs=KT[64:128, 1, c0:c0 + cw],
                                                                                       start=True, stop=True), r=["qT"] + kk, w=[bkey])
                                add("act", lambda e, bk=bk, h=h, cw=cw, at=at: e.activation(out=at[:, 0:cw], in_=pb[bk][:, 0:cw], func=AF.Relu, scale=WA[:, h:h + 1]),
                                    r=[bkey, "WA"], w=[akey])
                                if h == 0:
                                    add("dve", lambda e, at=at, c0=c0, cw=cw: e.tensor_scalar(out=score[:, c0:c0 + cw], in0=at[:, 0:cw], scalar1=WS[:, 0:1], scalar2=None, op0=ALU.mult),
                                        r=[akey, "WS"], w=["score"])
                                else:
                                    add("dve", lambda e, at=at, c0=c0, cw=cw, h=h: e.scalar_tensor_tensor(out=score[:, c0:c0 + cw], in0=at[:, 0:cw], scalar=WS[:, h:h + 1],
                                                                                                            in1=score[:, c0:c0 + cw], op0=ALU.mult, op1=ALU.add),
                                        r=[akey, "WS"], w=["score"])
                        add("dve", lambda e, tsl=tsl: e.tensor_tensor(out=score[:, tsl], in0=score[:, tsl], in1=causalf[:], op=ALU.add), r=["c_causalf"], w=["score"])
                    for g in range(2):
                        sbk = sbank[sb_i[0] % 2]
                        sb_i[0] += 1
                        skey = "pb%d" % sbk
                        q_rhs = qT[0:64, 4 * g:4 * g + 4, :]
                        add("pe", lambda e, sbk=sbk, g=g, q_rhs=q_rhs: e.matmul(pb[sbk][:, :], lhsT=KCT[:, g, :], rhs=q_rhs, start=True, stop=False),
                            r=["KCT", "qT"], w=[skey])
                        add("pe", lambda e, sbk=sbk, t=t: e.matmul(pb[sbk][:, :], lhsT=cmpbias[:, t, :], rhs=i4[:, :], start=False, stop=True),
                            r=["c_cmpbias", "c_i4"], w=[skey])
                        add("act", lambda e, sbk=sbk: e.activation(out=PTc[:], in_=pb[sbk][:, :], func=AF.Exp, scale=0.125), r=[skey], w=["PTc"])
                        dump("PTc", PTc[:], ["PTc"], t)
                        cut("A1a", t)
                        ob = obank[ob_i[0] % 2]
                        ob_i[0] += 1
                        okey = "pb%d" % ob
                        for h in range(4):
                            add("pe", lambda e, ob=ob, h=h, g=g: e.matmul(pb[ob][:, h * 128:h * 128 + 97], lhsT=PTc[:, h * 128:(h + 1) * 128], rhs=VCa[:, g, 0:97],
                                                                          start=(h == 0), stop=False, skip_group_check=True), r=["PTc", "VCa"], w=[okey])
                        v, vkey = pv_evac(pb[ob], okey, 97, 256 * g, 0, g, True)
                        if t >= 8:
                            for h in range(4):
                                if h == 0:
                                    add("dve", lambda e, v=v, g=g: e.tensor_scalar(out=imp[:, g, :], in0=v[:, 0, 65:97], scalar1=rc[:, 0:1], scalar2=None, op0=ALU.mult),
                                        r=[vkey, "rc"], w=[("imp", g)])
                                else:
                                    add("dve", lambda e, v=v, g=g, h=h: e.scalar_tensor_tensor(out=imp[:, g, :], in0=v[:, h, 65:97], scalar=rc[:, h:h + 1],
                                                                                               in1=imp[:, g, :], op0=ALU.mult, op1=ALU.add),
                                        r=[vkey, "rc"], w=[("imp", g)])

                    dump("Ot_A1", Ot[:, 0:512], ["Ot"], t)
                    dump("imp", imp[:].rearrange("p g j -> p (g j)"), [("imp", 0), ("imp", 1)], t)
                    cut("A1", t)
                    if t >= 8:
                        for g in range(2):
                            add("dve", lambda e, g=g, t=t: e.tensor_tensor(out=tk[:, 0:32], in0=imp[:, g, :], in1=forceb[:, t, :], op=ALU.add),
                                r=[("imp", g), "c_forceb"], w=["tk"])
                            add("dve", lambda e: e.max(out=tk[:, 32:40], in_=tk[:, 0:32]), r=["tk"], w=["tk"])
                            add("dve", lambda e: e.match_replace(out=tk[:, 48:80], in_to_replace=tk[:, 32:40], in_values=tk[:, 0:32], imm_value=-1e9),
                                r=["tk"], w=["tk"])
                            add("dve", lambda e: e.max(out=tk[:, 40:48], in_=tk[:, 48:80]), r=["tk"], w=["tk"])
                            add("dve", lambda e, g=g: e.tensor_scalar(out=selb[:, g, :], in0=tk[:, 0:32], scalar1=tk[:, 47:48], scalar2=NEGB, op0=ALU.is_lt, op1=ALU.mult),
                                r=["tk"], w=[("selb", g)])
                    cut("SEL", t)
                    if t >= 2:
                        add("dve", lambda e, nkeys=nkeys: e.tensor_reduce(out=bis[:, 0:1], in_=score[:, 0:nkeys], axis=AX.X, op=ALU.max), r=["score"], w=["bis"])
                        add("dve", lambda e, t=t: e.tensor_reduce(out=bis[:, 1:2], in_=score[:, 0:t * 128], axis=AX.X, op=ALU.min), r=["score"], w=["bis"])
                        add("dve", lambda e: e.tensor_scalar(out=bis[:, 1:2], in0=bis[:, 1:2], scalar1=-1.0, scalar2=None, op0=ALU.add), r=["bis"], w=["bis"])
                        add("dve", lambda e: e.tensor_tensor(out=bis[:, 2:3], in0=bis[:, 0:1], in1=bis[:, 1:2], op=ALU.add), r=["bis"], w=["bis"])
                        add("dve", lambda e: e.tensor_scalar(out=bis[:, 2:3], in0=bis[:, 2:3], scalar1=0.5, scalar2=None, op0=ALU.mult), r=["bis"], w=["bis"])
                        add("dve", lambda e: e.tensor_tensor(out=bis[:, 3:4], in0=bis[:, 0:1], in1=bis[:, 1:2], op=ALU.subtract), r=["bis"], w=["bis"])
                        add("dve", lambda e: e.tensor_scalar(out=wtab[:], in0=misc[:, 33:33 + NBIS + 1], scalar1=bis[:, 3:4], scalar2=0.5, op0=ALU.mult, op1=ALU.mult),
                            r=["bis", "c_misc"], w=["wtab"])
                        for it in range(NBIS):
                            add("dve", lambda e: e.memset(bis[:, 4:5], 0.0), w=["bis"])
                            add("dve", lambda e, nkeys=nkeys: e.tensor_scalar(out=junk2[:, 0:nkeys], in0=score[:, 0:nkeys], scalar1=bis[:, 2:3], scalar2=0.0,
                                                                               op0=ALU.is_gt, op1=ALU.add, accum_out=bis[:, 4:5]), r=["score", "bis"], w=["junk2", "bis"])
                            add("dve", lambda e, it=it: e.tensor_scalar(out=bis[:, 5:6], in0=bis[:, 4:5], scalar1=255.5, scalar2=wtab[:, it:it + 1],
                                                                         op0=ALU.is_gt, op1=ALU.mult), r=["bis", "wtab"], w=["bis"])
                            add("dve", lambda e, it=it: e.scalar_tensor_tensor(out=bis[:, 2:3], in0=bis[:, 5:6], scalar=2.0, in1=bis[:, 2:3], op0=ALU.mult, op1=ALU.add),
                                r=["bis"], w=["bis"])
                            add("dve", lambda e, it=it: e.tensor_tensor(out=bis[:, 2:3], in0=bis[:, 2:3], in1=wtab[:, it:it + 1], op=ALU.subtract), r=["bis", "wtab"], w=["bis"])
                        add("dve", lambda e: e.tensor_tensor(out=bis[:, 6:7], in0=bis[:, 2:3], in1=wtab[:, NBIS - 1:NBIS], op=ALU.subtract), r=["bis", "wtab"], w=["bis"])
                        add("dve", lambda e, nkeys=nkeys: e.tensor_scalar(out=maskb[:, 0:nkeys], in0=score[:, 0:nkeys], scalar1=bis[:, 6:7], scalar2=NEGB,
                                                                            op0=ALU.is_le, op1=ALU.mult), r=["score", "bis"], w=["maskb"])

                        def dmask(kt):
                            return (maskb[:, kt * 128:(kt + 1) * 128], ["maskb"])
                    else:
                        def dmask(kt, t=t):
                            return (causal[:], ["c_causal"]) if kt == t else None
                    for g in range(2):
                        if t >= 8:
                            nkeys = (t + 1) * 128
                            add("pool", lambda e, nkeys=nkeys, g=g: e.tensor_copy(out=selx[:, 0:nkeys].rearrange("p (j k) -> p j k", k=64),
                                                                              in_=bc(selb[:, g, 0:nkeys // 64].unsqueeze(2), [128, nkeys // 64, 64])),
                                r=[("selb", g)], w=["selx"])
                            add("pool", lambda e, tsl=tsl: e.tensor_tensor(out=selx[:, tsl], in0=selx[:, tsl], in1=causal[:], op=ALU.add),
                                r=["c_causal"], w=["selx"])

                            def mask_fn(kt):
                                return (selx[:, kt * 128:(kt + 1) * 128], ["selx"])
                        else:
                            def mask_fn(kt, t=t):
                                return (causal[:], ["c_causal"]) if kt == t else None
                        bank, okey = attention(
                            t, list(range(t + 1)),
                            lambda kt, g=g: (KT[0:64, 2 + g, kt * 128:(kt + 1) * 128], [("KT", kt)]),
                            qT[0:64, 4 * g:4 * g + 4, :], mask_fn,
                            lambda kt, g=g: (VA[:, kt, g, 0:65], [("VA", kt)]))
                        pv_evac(bank, okey, 65, 256 * g, 1, g, False)

                    dump("Ot_A2", Ot[:, 0:512], ["Ot"], t)
                    dump("selx", selx[:, 0:(t + 1) * 128], ["selx"], t)
                    cut("A2", t)
                    for g in range(2):
                        def mask_fn(kt, t=t):
                            if kt == t:
                                return (causal[:], ["c_causal"])
                            if kt == t - 4:
                                return (band[:], ["c_band"])
                            return None
                        bank, okey = attention(
                            t, list(range(max(0, t - 4), t + 1)),
                            lambda kt, g=g: (KT[0:64, 4 + g, kt * 128:(kt + 1) * 128], [("KT", kt)]),
                            qT[0:64, 4 * g:4 * g + 4, :], mask_fn,
                            lambda kt, g=g: (VA[:, kt, 2 + g, 0:65], [("VA", kt)]))
                        pv_evac(bank, okey, 65, 256 * g, 2, g, False)

                    dump("Ot_A3", Ot[:, 0:512], ["Ot"], t)
                    cut("A3", t)
                    for hg in range(2):
                        bank, okey = attention(
                            t, list(range(t + 1)),
                            lambda kt: (KT[64:128, 0, kt * 128:(kt + 1) * 128], [("KTd", kt)]),
                            qT[64:128, 4 * hg:4 * hg + 4, :], dmask,
                            lambda kt: (VA[:, kt, 4, 0:65], [("VA", kt)]))
                        pv_evac(bank, okey, 65, 512 + 256 * hg, None, 0, True)

                    dump("Ot_A4", Ot[:], ["Ot"], t)
                    dump("score", score[:, 0:(t + 1) * 128], ["score"], t)
                    dump("maskb", maskb[:, 0:(t + 1) * 128], ["maskb"], t)
                    dump("bis", bis[:], ["bis"], t)
                    cut("A4", t)
                    add("act", lambda e: e.activation(out=Otb[:], in_=Ot[:], func=AF.Copy), r=["Ot"], w=["Otb"])
                    for k in range(8):
                        add("pe", lambda e, k=k: e.transpose(out=pbf(0)[:, k * 128:(k + 1) * 128], in_=Otb[:, k * 128:(k + 1) * 128], identity=i4[:, 0:128]),
                            r=["Otb", "c_i4"], w=["pb0"])
                    add("dve", lambda e: e.tensor_copy(out=oT[:].rearrange("p k q -> p (k q)"), in_=pbf(0)[:, :]), r=["pb0"], w=["oT"])
                    add("dve", lambda e: e.memset(st4[:, 4:6], 0.0), w=["ssy0", "ssy1"])
                    for half in range(2):
                        bk = 1 + half
                        bkey = "pb%d" % bk
                        for k in range(8):
                            add("pe", lambda e, k=k, bk=bk, half=half: e.matmul(pb[bk][:, :], lhsT=oT[:, k, :], rhs=Wout[:, k, half * 512:(half + 1) * 512],
                                                                                start=(k == 0), stop=(k == 7)), r=["oT", "weights"], w=[bkey])
                        add("act", lambda e, bk=bk, half=half: e.activation(out=junk2[:, half * 512:(half + 1) * 512], in_=pb[bk][:, :], func=AF.Square,
                                                                              accum_out=st4[:, 4 + half:5 + half]), r=[bkey], w=["junk2", "ssy%d" % half])
                    add("dve", lambda e: e.tensor_tensor(out=st4[:, 6:7], in0=st4[:, 4:5], in1=st4[:, 5:6], op=ALU.add), r=["ssy0", "ssy1"], w=["ssy"])
                    rstd_from_ss(st4[:, 6:7], st4[:, 7:8], ["ssy"], "rstdy")
                    for half in range(2):
                        bk = 1 + half
                        bkey = "pb%d" % bk
                        hs = slice(half * 512, (half + 1) * 512)
                        add("dve", lambda e, bk=bk, hs=hs: e.scalar_tensor_tensor(out=x1t[:, hs], in0=pb[bk][:, :], scalar=st4[:, 7:8], in1=G1[:, hs],
                                                                                   op0=ALU.mult, op1=ALU.mult), r=[bkey, "rstdy", "G1"], w=["x1t"])
                    add("pool", lambda e, xb=xb: e.tensor_tensor(out=x1t[:], in0=x1t[:], in1=xb[:], op=ALU.add), r=[xkey], w=["x1t"])
                    add("sp", lambda e, b=b, tsl=tsl: e.dma_start(out=out_d[b, tsl, :], in_=x1t[:]), r=["x1t"], w=[("out", b, t)], dma=True)
                    cut("O", t)
                    if debug and b == 0:
                        for name in debug:
                            if name == "Ot%d" % t:
                                add("sp", lambda e, name=name: e.dma_start(out=dbg_d[name], in_=Ot[:]), r=["Ot"], w=["dbg_" + name], dma=True)
                            if name == "qT%d" % t:
                                add("act", lambda e: e.activation(out=score[:, 0:1792], in_=qT[:].rearrange("p s q -> p (s q)"), func=AF.Copy), r=["qT"], w=["score"])
                                add("sp", lambda e, name=name: e.dma_start(out=dbg_d[name], in_=score[:, 0:1792]), r=["score"], w=["dbg_" + name], dma=True)
            S.barrier()
        cut("mixer")

        with ExitStack() as es2:
            Fz = es2
            Wup = sbuf(Fz, "Wup", [128, 8, DFF], BF16)
            Wdn = sbuf(Fz, "Wdn", [128, 32, D], BF16)
            with ExitStack() as es_w:
                wst = [sbuf(es_w, "wstf%d" % i, [128, 8, 512], F32) for i in range(2)]
                wup_v = wup_d.rearrange("(k p) n -> p k n", p=128)
                wdn_v = wdn_d.rearrange("(k p) n -> p k n", p=128)
                ci = 0
                for c0 in range(0, DFF, 512):
                    st = wst[ci % 2]
                    key = "wstf%d" % (ci % 2)
                    add("sp", lambda e, st=st, c0=c0: e.dma_start(out=st[:], in_=wup_v[:, :, c0:c0 + 512]), w=[key], dma=True)
                    add("pool" if ci % 2 else "dve", lambda e, st=st, c0=c0: e.tensor_copy(out=Wup[:, :, c0:c0 + 512], in_=st[:]), r=[key], w=["fw"])
                    ci += 1
                for k0 in range(0, 32, 4):
                    st = wst[ci % 2]
                    key = "wstf%d" % (ci % 2)
                    stv = st[:].rearrange("p a n -> p (a n)").rearrange("p (a n) -> p a n", a=4)
                    add("sp", lambda e, stv=stv, k0=k0: e.dma_start(out=stv, in_=wdn_v[:, k0:k0 + 4, :]), w=[key], dma=True)
                    add("pool" if ci % 2 else "dve", lambda e, stv=stv, k0=k0: e.tensor_copy(out=Wdn[:, k0:k0 + 4, :], in_=stv), r=[key], w=["fw"])
                    ci += 1
                S.barrier()
            NTC = 2
            NTOK = NTC * 128
            x1c = sbuf(Fz, "x1c", [128, NTC, D], F32)
            xn2 = sbuf(Fz, "xn2", [128, D], BF16)
            stf = sbuf(Fz, "stf", [128, 8], F32)
            h2T = sbuf(Fz, "h2T", [128, 8, NTOK], BF16)
            uT = sbuf(Fz, "uT", [128, 32, NTOK], BF16)
            rl = [sbuf(Fz, "rl%d" % i, [128, NTOK], F32) for i in range(2)]
            G2 = sbuf(Fz, "G2", [128, D], F32)
            yo = sbuf(Fz, "yo", [128, D], F32)
            jk = sbuf(Fz, "jk", [128, D], BF16)

            def rstd2(ss_ap, out_ap, rkeys, wkey):
                add("dve", lambda e: e.tensor_scalar(out=out_ap, in0=ss_ap, scalar1=1.0 / D, scalar2=EPS, op0=ALU.mult, op1=ALU.add), r=rkeys, w=[wkey])
                add("act", lambda e: e.activation(out=out_ap, in_=out_ap, func=AF.Ln), r=[wkey], w=[wkey])
                add("act", lambda e: e.activation(out=out_ap, in_=out_ap, func=AF.Exp, scale=-0.5), r=[wkey], w=[wkey])

            for b in range(nseq):
                add("sp", lambda e, b=b: e.dma_start(out=G2[:], in_=bc(gscr_d[b:b + 1, 1, :], [128, D])), r=["gscr"], w=["G2"], dma=True)
                for tc in range(NT // NTC):
                    for i in range(NTC):
                        t = tc * NTC + i
                        tsl = slice(t * 128, (t + 1) * 128)
                        add("sp", lambda e, b=b, tsl=tsl, i=i: e.dma_start(out=x1c[:, i, :], in_=out_d[b, tsl, :]), r=[("out", b, t)], w=[("x1c", i)], dma=True)
                        add("dve", lambda e: e.memset(stf[:, 0:1], 0.0), w=["fss"])
                        add("act", lambda e, i=i: e.activation(out=xn2[:], in_=x1c[:, i, :], func=AF.Square, accum_out=stf[:, 0:1]), r=[("x1c", i)], w=["xn2", "fss"])
                        rstd2(stf[:, 0:1], stf[:, 1:2], ["fss"], "frstd")
                        add("act", lambda e, i=i: e.activation(out=xn2[:], in_=x1c[:, i, :], func=AF.Copy, scale=stf[:, 1:2]), r=[("x1c", i), "frstd"], w=["xn2"])
                        for k in range(8):
                            add("pe", lambda e, k=k: e.transpose(out=pbf(0)[:, k * 128:(k + 1) * 128], in_=xn2[:, k * 128:(k + 1) * 128], identity=i4[:, 0:128]),
                                r=["xn2", "c_i4"], w=["pb0"])
                        for k in range(8):
                            add("dve", lambda e, k=k, b=b, i=i: e.tensor_scalar(out=h2T[:, k, i * 128:(i + 1) * 128], in0=pbf(0)[:, k * 128:(k + 1) * 128],
                                                                                 scalar1=AB[:, 2, b, k:k + 1], scalar2=AB[:, 3, b, k:k + 1], op0=ALU.mult, op1=ALU.add),
                                r=["pb0", "AB"], w=["h2T"])
                    for f in range(32):
                        bk = 1 + (f % 2)
                        bkey = "pb%d" % bk
                        r_ = rl[f % 2]
                        rkey = "rl%d" % (f % 2)
                        for k in range(8):
                            add("pe", lambda e, k=k, f=f, bk=bk: e.matmul(pb[bk][:, 0:NTOK], lhsT=Wup[:, k, f * 128:(f + 1) * 128], rhs=h2T[:, k, :],
                                                                          start=(k == 0), stop=(k == 7)), r=["h2T", "fw"], w=[bkey])
                        add("act", lambda e, bk=bk, r_=r_: e.activation(out=r_[:], in_=pb[bk][:, 0:NTOK], func=AF.Relu), r=[bkey], w=[rkey])
                        add("pool" if f % 2 else "dve", lambda e, f=f, r_=r_: e.tensor_tensor(out=uT[:, f, :], in0=r_[:], in1=r_[:], op=ALU.mult), r=[rkey], w=[("uT", f)])
                    for i in range(NTC):
                        t = tc * NTC + i
                        tsl = slice(t * 128, (t + 1) * 128)
                        add("dve", lambda e: e.memset(stf[:, 4:6], 0.0), w=["fssy0", "fssy1"])
                        for half in range(2):
                            bk = 3 + half
                            bkey = "pb%d" % bk
                            for f in range(32):
                                add("pe", lambda e, f=f, bk=bk, half=half, i=i: e.matmul(pb[bk][:, :], lhsT=uT[:, f, i * 128:(i + 1) * 128],
                                                                                        rhs=Wdn[:, f, half * 512:(half + 1) * 512], start=(f == 0), stop=(f == 31)),
                                    r=[("uT", f), "fw"], w=[bkey])
                            add("act", lambda e, bk=bk, half=half: e.activation(out=jk[:, half * 512:(half + 1) * 512], in_=pb[bk][:, :], func=AF.Square,
                                                                                  accum_out=stf[:, 4 + half:5 + half]), r=[bkey], w=["jk%d" % half, "fssy%d" % half])
                        add("dve", lambda e: e.tensor_tensor(out=stf[:, 6:7], in0=stf[:, 4:5], in1=stf[:, 5:6], op=ALU.add), r=["fssy0", "fssy1"], w=["fssy"])
                        rstd2(stf[:, 6:7], stf[:, 7:8], ["fssy"], "frstdy")
                        for half in range(2):
                            bk = 3 + half
                            bkey = "pb%d" % bk
                            hs = slice(half * 512, (half + 1) * 512)
                            add("dve", lambda e, bk=bk, hs=hs: e.scalar_tensor_tensor(out=yo[:, hs], in0=pb[bk][:, :], scalar=stf[:, 7:8], in1=G2[:, hs],
                                                                                       op0=ALU.mult, op1=ALU.mult), r=[bkey, "frstdy", "G2"], w=["yo"])
                        add("pool", lambda e, i=i: e.tensor_tensor(out=yo[:], in0=yo[:], in1=x1c[:, i, :], op=ALU.add), r=[("x1c", i)], w=["yo"])
                        add("sp", lambda e, b=b, tsl=tsl: e.dma_start(out=out_d[b, tsl, :], in_=yo[:]), r=["yo"], w=[("out", b, t)], dma=True)
            S.barrier()

    _build_body()
    S.stopped = False
    S.barrier()
    S.emit()
    top.close()
    return nc, S


def make_in_maps(inputs, cores=range(NCORES)):
    f32 = np.float32
    x = np.asarray(inputs["x"], f32)
    c = np.asarray(inputs["c"], f32)
    pos = np.asarray(inputs["positions"], np.int32)
    perm = _win_perm()
    w_in = np.ascontiguousarray(np.asarray(inputs["w_in"], f32)[0][:, perm])
    gcol = np.stack([np.asarray(inputs["g_pre_mix"], f32)[0].reshape(8, 128).T,
                     np.asarray(inputs["g_pre_ffn"], f32)[0].reshape(8, 128).T], axis=1)
    grow = np.stack([np.asarray(inputs["g_post_mix"], f32)[0], np.asarray(inputs["g_post_ffn"], f32)[0]], axis=0)[None]
    pek = np.asarray(inputs["cmp_pe_k"], f32)[0].T
    pev = np.asarray(inputs["cmp_pe_v"], f32)[0].T
    peT = np.zeros((128, 2, 32), f32)
    peT[0:64, 0] = pek
    peT[64:128, 0] = pek
    peT[0:64, 1] = pev
    peT[64:128, 1] = pev
    w1k = np.ascontiguousarray(np.asarray(inputs["cmp_w1_k"], f32)[0].reshape(32, 64, 128).transpose(1, 0, 2))
    w1v_ = np.asarray(inputs["cmp_w1_v"], f32)[0].reshape(32, 64, 128).transpose(1, 0, 2)
    w1v = np.ascontiguousarray(np.concatenate([w1v_, w1v_], axis=0))
    w2 = np.ascontiguousarray(np.stack([np.asarray(inputs["cmp_w2_k"], f32)[0], np.asarray(inputs["cmp_w2_v"], f32)[0]], axis=1))
    shared = dict(
        w_ada=np.ascontiguousarray(np.asarray(inputs["w_ada"], f32)[0]),
        b_ada=np.ascontiguousarray(np.asarray(inputs["b_ada"], f32)),
        b_adac=np.ascontiguousarray(np.asarray(inputs["b_ada"], f32)[0].reshape(48, 128).T),
        gcol=np.ascontiguousarray(gcol), grow=np.ascontiguousarray(grow), w_in=w_in, peT=peT, w1k=w1k, w1v=w1v, w2=w2,
        w_out=np.ascontiguousarray(np.asarray(inputs["w_out"], f32)[0]),
        w_up=np.ascontiguousarray(np.asarray(inputs["w_up"], f32)[0]),
        w_down=np.ascontiguousarray(np.asarray(inputs["w_down"], f32)[0]),
    )
    shared.update(_consts())
    maps = []
    for core in cores:
        b0 = core * SEQ_PER_CORE
        m = dict(shared)
        m["x"] = np.ascontiguousarray(x[b0:b0 + SEQ_PER_CORE])
        m["cT"] = np.ascontiguousarray(c[b0:b0 + SEQ_PER_CORE].T.reshape(8, 128, 4).transpose(1, 0, 2))
        m["posT"] = np.ascontiguousarray(pos[b0:b0 + SEQ_PER_CORE].reshape(4, NT, 128).transpose(2, 0, 1))
        maps.append(m)
    return maps


_PROG = {}


def kernel(**inputs):
    if "nc" not in _PROG:
        _PROG["nc"], _ = build_program()
    nc = _PROG["nc"]
    maps = make_in_maps(inputs)
    res = run_bass_kernel_spmd(nc, maps, core_ids=list(range(NCORES)))
    out = np.concatenate([np.asarray(r["out"], np.float32) for r in res.results], axis=0)
    return out
```
